# Optimizing a Trainium2 kernel written in Bass

```python
import jax, jax.numpy as jnp
from jax import lax

D_MODEL = 4096
BATCH = 1
SEQ = 8192
DEPTH = 1

HEAD_DIM = 128
ROPE_THETA = 10000.0
NORM_EPS = 1e-6
NEG_INF = -1e30
QBLK = 128

DIL_WINDOWS = (128, 512, 2048)
DIL_RATES = (1, 4, 16)
N_DIL = 3
DIL_HEADS = 4
DIL_NKEYS = DIL_WINDOWS[0] // DIL_RATES[0] + 1
A_HEADS = N_DIL * DIL_HEADS
A_W = A_HEADS * HEAD_DIM
A_OUT_W = DIL_HEADS * HEAD_DIM

NSA_HEADS = 16
NSA_KV = 4
NSA_QPG = NSA_HEADS // NSA_KV
B_W = NSA_HEADS * HEAD_DIM
KV_W = NSA_KV * HEAD_DIM
CMP_LEN = 32
CMP_STRIDE = 16
CMP_HID = HEAD_DIM
SEL_LEN = 64
SEL_TOP = 16
WIN = 512
FORCE_BONUS = 1000.0
N_NSA_GATES = 3

D_FF = -(-8 * D_MODEL // (3 * 256)) * 256
PLE_DIM = 256

IN_WIDTHS = (A_W, A_W, A_W, B_W, KV_W, KV_W, KV_W, KV_W, KV_W, KV_W, NSA_HEADS * N_NSA_GATES, 2 * D_MODEL)
IN_W = sum(IN_WIDTHS)

kernel_name = "hybrid_dilated_nsa_gated_block"


def rms_norm(x, g):
    xf = x.astype(jnp.float32)
    y = xf * lax.rsqrt(jnp.mean(xf * xf, axis=-1, keepdims=True) + NORM_EPS)
    return (y * g.astype(jnp.float32)).astype(x.dtype)


def rope(x, pos):
    half = HEAD_DIM // 2
    inv = ROPE_THETA ** (-jnp.arange(half, dtype=jnp.float32) / half)
    ang = pos.astype(jnp.float32)[:, None] * inv[None, :]
    shape = (pos.shape[0],) + (1,) * (x.ndim - 3) + (half,)
    cos = jnp.cos(ang).reshape(shape)
    sin = jnp.sin(ang).reshape(shape)
    xf = x.astype(jnp.float32)
    x1, x2 = xf[..., :half], xf[..., half:]
    return jnp.concatenate([x1 * cos - x2 * sin, x2 * cos + x1 * sin], axis=-1).astype(x.dtype)


def masked_softmax(logits, mask):
    return jax.nn.softmax(jnp.where(mask, logits.astype(jnp.float32), NEG_INF), axis=-1)


def dilated_attention(q, k, v):
    B, S = q.shape[0], q.shape[1]
    scale = HEAD_DIM ** -0.5
    rates = jnp.array(DIL_RATES, dtype=jnp.int32)
    offs = rates[:, None] * jnp.arange(DIL_NKEYS, dtype=jnp.int32)[None, :]
    g_ix = jnp.arange(N_DIL)[None, :, None]

    def block(c):
        start = c * QBLK
        pos = start + jnp.arange(QBLK)
        qb = lax.dynamic_slice_in_dim(q, start, QBLK, axis=1)
        kp = pos[:, None, None] - offs[None]
        valid = kp >= 0
        kpc = jnp.maximum(kp, 0)
        kb = k[:, kpc, g_ix]
        vb = v[:, kpc, g_ix]
        logit = jnp.einsum('bqghd,bqgkhd->bqghk', qb, kb).astype(jnp.float32) * scale
        logit = jnp.where(valid[None, :, :, None, :], logit, NEG_INF)
        m = jnp.max(logit, axis=-1, keepdims=True)
        e = jnp.exp(logit - m)
        den = jnp.sum(e, axis=-1, keepdims=True)
        o = jnp.einsum('bqghk,bqgkhd->bqghd', (e / den).astype(v.dtype), vb)
        log_den = (m + jnp.log(den))[..., 0]
        wgt = jax.nn.softmax(log_den, axis=2)
        return jnp.einsum('bqgh,bqghd->bqhd', wgt.astype(o.dtype), o)

    out = lax.map(block, jnp.arange(S // QBLK))
    return jnp.moveaxis(out, 0, 1).reshape(B, S, A_OUT_W)


def compress(kv, pe, w1, w2):
    B, S = kv.shape[0], kv.shape[1]
    nc = (S - CMP_LEN) // CMP_STRIDE + 1
    idx = jnp.arange(nc)[:, None] * CMP_STRIDE + jnp.arange(CMP_LEN)[None, :]
    blk = kv[:, idx] + pe[None, None, :, None, :]
    blk = blk.transpose(0, 1, 3, 2, 4).reshape(B, nc, NSA_KV, CMP_LEN * HEAD_DIM)
    return jax.nn.gelu(blk @ w1) @ w2


def overlap_matrix(nc, ns):
    cs = jnp.arange(nc)[:, None] * CMP_STRIDE
    ss = jnp.arange(ns)[None, :] * SEL_LEN
    ov = jnp.clip(jnp.minimum(cs + CMP_LEN, ss + SEL_LEN) - jnp.maximum(cs, ss), 0, None)
    return ov.astype(jnp.float32) / CMP_LEN


def native_sparse_attention(q_raw, q_rot, k_c, v_c, k_s, v_s, k_w, v_w, gates):
    B, S = q_raw.shape[0], q_raw.shape[1]
    nc = k_c.shape[1]
    ns = S // SEL_LEN
    n_top = min(SEL_TOP, ns)
    scale = HEAD_DIM ** -0.5
    ov = overlap_matrix(nc, ns)
    cmp_end = jnp.arange(nc) * CMP_STRIDE + CMP_LEN - 1
    sblk = jnp.arange(ns)
    ks_blk = k_s.reshape(B, ns, SEL_LEN, NSA_KV, HEAD_DIM).transpose(0, 3, 1, 2, 4)
    vs_blk = v_s.reshape(B, ns, SEL_LEN, NSA_KV, HEAD_DIM).transpose(0, 3, 1, 2, 4)
    pad = ((0, 0), (WIN, 0), (0, 0), (0, 0))
    k_wp = jnp.pad(k_w, pad)
    v_wp = jnp.pad(v_w, pad)
    b_ix = jnp.arange(B)[:, None, None, None]
    g_ix = jnp.arange(NSA_KV)[None, None, :, None]

    def block(c):
        start = c * QBLK
        pos = start + jnp.arange(QBLK)
        qr = lax.dynamic_slice_in_dim(q_raw, start, QBLK, axis=1)
        qo = lax.dynamic_slice_in_dim(q_rot, start, QBLK, axis=1)
        gb = lax.dynamic_slice_in_dim(gates, start, QBLK, axis=1)
        cl = jnp.einsum('bqgjd,bngd->bqgjn', qr, k_c) * scale
        cvalid = cmp_end[None, :] <= pos[:, None]
        has_any = (pos >= CMP_LEN - 1).astype(jnp.float32)
        p_c = masked_softmax(cl, cvalid[None, :, None, None, :]) * has_any[None, :, None, None, None]
        o_c = jnp.einsum('bqgjn,bngd->bqgjd', p_c.astype(v_c.dtype), v_c)
        imp = jnp.einsum('bqgjn,nm->bqgm', p_c, ov)
        cur = pos // SEL_LEN
        forced = ((sblk[None] == 0) | (sblk[None] == cur[:, None]) | (sblk[None] == cur[:, None] - 1)).astype(jnp.float32)
        svalid = sblk[None] * SEL_LEN <= pos[:, None]
        score = jnp.where(svalid[None, :, None, :], imp + FORCE_BONUS * forced[None, :, None, :], NEG_INF)
        _, sel = lax.top_k(score, n_top)
        ksb = ks_blk[b_ix, g_ix, sel]
        vsb = vs_blk[b_ix, g_ix, sel]
        kpos = sel[..., None] * SEL_LEN + jnp.arange(SEL_LEN)
        smask = (kpos <= pos[None, :, None, None, None]).reshape(B, QBLK, NSA_KV, 1, n_top * SEL_LEN)
        sl = jnp.einsum('bqgjd,bqgkld->bqgjkl', qo, ksb) * scale
        sl = sl.reshape(B, QBLK, NSA_KV, NSA_QPG, n_top * SEL_LEN)
        p_s = masked_softmax(sl, smask).reshape(B, QBLK, NSA_KV, NSA_QPG, n_top, SEL_LEN)
        o_s = jnp.einsum('bqgjkl,bqgkld->bqgjd', p_s.astype(vsb.dtype), vsb)
        kwb = lax.dynamic_slice_in_dim(k_wp, start, QBLK + WIN, axis=1)
        vwb = lax.dynamic_slice_in_dim(v_wp, start, QBLK + WIN, axis=1)
        wpos = start - WIN + jnp.arange(QBLK + WIN)
        wmask = (wpos[None] <= pos[:, None]) & (pos[:, None] - wpos[None] < WIN) & (wpos[None] >= 0)
        wl = jnp.einsum('bqgjd,bkgd->bqgjk', qo, kwb) * scale
        p_w = masked_softmax(wl, wmask[None, :, None, None, :])
        o_w = jnp.einsum('bqgjk,bkgd->bqgjd', p_w.astype(vwb.dtype), vwb)
        return gb[..., 0:1] * o_c + gb[..., 1:2] * o_s + gb[..., 2:3] * o_w

    out = lax.map(block, jnp.arange(S // QBLK))
    return jnp.moveaxis(out, 0, 1).reshape(B, S, B_W)


def setup_inputs(seed: int = 0) -> dict:
    key = jax.random.key(seed)
    ks = jax.random.split(key, 22)
    f32 = jnp.float32

    def w(k, shape, fan_in):
        return jax.random.normal(k, shape, f32) * fan_in ** -0.5

    def gain(k):
        return 1.0 + 0.1 * jax.random.normal(k, (DEPTH, D_MODEL), f32)

    return {
        "x": jax.random.normal(ks[0], (BATCH, SEQ, D_MODEL), f32),
        "p": jax.random.normal(ks[1], (DEPTH, BATCH, SEQ, PLE_DIM), f32),
        "g_mix_pre": gain(ks[2]),
        "w_in": w(ks[3], (DEPTH, D_MODEL, IN_W), D_MODEL),
        "pe_ck": 0.5 * jax.random.normal(ks[4], (DEPTH, CMP_LEN, HEAD_DIM), f32),
        "w_ck1": w(ks[5], (DEPTH, CMP_LEN * HEAD_DIM, CMP_HID), CMP_LEN * HEAD_DIM),
        "w_ck2": w(ks[6], (DEPTH, CMP_HID, HEAD_DIM), CMP_HID),
        "pe_cv": 0.5 * jax.random.normal(ks[7], (DEPTH, CMP_LEN, HEAD_DIM), f32),
        "w_cv1": w(ks[8], (DEPTH, CMP_LEN * HEAD_DIM, CMP_HID), CMP_LEN * HEAD_DIM),
        "w_cv2": w(ks[9], (DEPTH, CMP_HID, HEAD_DIM), CMP_HID),
        "w_a": w(ks[10], (DEPTH, A_OUT_W, D_MODEL), A_OUT_W),
        "w_b": w(ks[11], (DEPTH, B_W, D_MODEL), B_W),
        "w_out": w(ks[12], (DEPTH, D_MODEL, D_MODEL), D_MODEL),
        "g_mix_post": gain(ks[13]),
        "g_ffn_pre": gain(ks[14]),
        "w_gu": w(ks[15], (DEPTH, D_MODEL, 2 * D_FF), D_MODEL),
        "w_down": w(ks[16], (DEPTH, D_FF, D_MODEL), D_FF),
        "g_ffn_post": gain(ks[17]),
        "g_ple_pre": gain(ks[18]),
        "w_ple_gate": w(ks[19], (DEPTH, D_MODEL, D_MODEL), D_MODEL),
        "w_ple": w(ks[20], (DEPTH, PLE_DIM, D_MODEL), PLE_DIM),
        "g_ple_post": gain(ks[21]),
    }


def reference(x, p, g_mix_pre, w_in, pe_ck, w_ck1, w_ck2, pe_cv, w_cv1, w_cv2, w_a, w_b, w_out,
              g_mix_post, g_ffn_pre, w_gu, w_down, g_ffn_post, g_ple_pre, w_ple_gate, w_ple, g_ple_post):
    B, S, D = x.shape
    pos = jnp.arange(S)
    splits = []
    acc = 0
    for wd in IN_WIDTHS[:-1]:
        acc += wd
        splits.append(acc)
    for i in range(DEPTH):
        h = rms_norm(x, g_mix_pre[i])
        proj = h @ w_in[i]
        (qa, ka, va, qb, kc, vc, ksl, vsl, kwn, vwn, g_nsa, g_mix) = jnp.split(proj, splits, axis=-1)
        a_shape = (B, S, N_DIL, DIL_HEADS, HEAD_DIM)
        y_a = dilated_attention(rope(qa.reshape(a_shape), pos), rope(ka.reshape(a_shape), pos),
                                va.reshape(a_shape)) @ w_a[i]
        kv_shape = (B, S, NSA_KV, HEAD_DIM)
        q_raw = qb.reshape(B, S, NSA_KV, NSA_QPG, HEAD_DIM)
        k_c = compress(kc.reshape(kv_shape), pe_ck[i], w_ck1[i], w_ck2[i])
        v_c = compress(vc.reshape(kv_shape), pe_cv[i], w_cv1[i], w_cv2[i])
        gates = jax.nn.sigmoid(g_nsa.reshape(B, S, NSA_KV, NSA_QPG, N_NSA_GATES))
        y_b = native_sparse_attention(q_raw, rope(q_raw, pos), k_c, v_c,
                                      rope(ksl.reshape(kv_shape), pos), vsl.reshape(kv_shape),
                                      rope(kwn.reshape(kv_shape), pos), vwn.reshape(kv_shape),
                                      gates) @ w_b[i]
        gate_a, gate_b = jnp.split(jax.nn.sigmoid(g_mix), 2, axis=-1)
        mixed = (gate_a * y_a + gate_b * y_b) @ w_out[i]
        x = x + rms_norm(mixed, g_mix_post[i])
        h = rms_norm(x, g_ffn_pre[i])
        gt, up = jnp.split(h @ w_gu[i], 2, axis=-1)
        x = x + rms_norm((jax.nn.silu(gt) * up) @ w_down[i], g_ffn_post[i])
        ple = (p[i] @ w_ple[i]) * jax.nn.sigmoid(rms_norm(x, g_ple_pre[i]) @ w_ple_gate[i])
        x = x + rms_norm(ple, g_ple_post[i])
    return x
```

```python
import numpy as np
import ml_dtypes
from contextlib import ExitStack
import concourse.bass as bass
import concourse.mybir as mybir
from concourse.bass_utils import run_bass_kernel_spmd

F32 = mybir.dt.float32
BF16 = mybir.dt.bfloat16
AF = mybir.ActivationFunctionType
ALU = mybir.AluOpType

NCORES = 8
S_ALL = 8192
D = 4096
KC = 32
T = 1024
HALO = 2048
EXT = HALO + T
DFF = 11008
FKC = 86
SCALE = 128 ** -0.5
EPS = 1e-6
NEG = -30000.0
C_QA, C_KA, C_VA, C_QB, C_KC, C_VC, C_KSL, C_VSL, C_KWN, C_VWN, C_GN, C_GM = (
    0, 1536, 3072, 4608, 6656, 7168, 7680, 8192, 8704, 9216, 9728, 9776)

ENGS = ("pe", "act", "dve", "pool", "sp")


import os
_STOP = int(os.environ.get("MK_STOP", "1000"))
_DBG = [x for x in os.environ.get("MK_DBG", "").split(",") if x]


class StopBuild(Exception):
    pass


class Sched:
    def __init__(self, nc, st):
        self.nc = nc
        self.esem = {e: st.enter_context(nc.semaphore("s_" + e)) for e in ENGS}
        self.dsem = {}
        self.st = st
        self.seq = {e: 0 for e in ENGS}
        self.nsig = {e: 0 for e in ENGS}
        self.known = {e: {} for e in ENGS}
        self.dcount = {}
        self._reset()

    def _reset(self):
        self.ops = {e: [] for e in ENGS}
        self.lastw = {}
        self.readers = {}
        self.signal = {e: set() for e in ENGS}
        self.dma_issuer = {}

    def _need(self, eng, seq, tok, waits):
        kind, src, val = tok
        if kind == 'E':
            if src == eng:
                if eng == 'pe' or seq - val > 2:
                    return
            k = ('E', src)
        else:
            k = ('D', src)
        if self.known[eng].get(k, -1) >= val:
            return
        if val > waits.get(k, -1):
            waits[k] = val

    def op(self, eng, fn, reads=(), writes=(), dma=None):
        if getattr(self, 'stopped', False):
            return
        seq = self.seq[eng]
        self.seq[eng] += 1
        waits = {}
        for r in reads:
            t = self.lastw.get(r)
            if t is not None:
                self._need(eng, seq, t, waits)
        for w in writes:
            t = self.lastw.get(w)
            if t is not None:
                self._need(eng, seq, t, waits)
            for t in self.readers.get(w, ()):
                self._need(eng, seq, t, waits)
        for k, v in waits.items():
            self.known[eng][k] = v
            if k[0] == 'E':
                self.signal[k[1]].add(v)
        if dma is not None:
            if dma not in self.dsem:
                self.dsem[dma] = self.st.enter_context(self.nc.semaphore("d_" + str(dma)))
            c = self.dcount.get(dma, 0) + 1
            self.dcount[dma] = c
            tok = ('D', dma, c)
            self.dma_issuer.setdefault(eng, {})[dma] = c
        else:
            tok = ('E', eng, seq)
        for r in reads:
            self.readers.setdefault(r, []).append(tok)
        for w in writes:
            self.lastw[w] = tok
            self.readers[w] = []
        self.ops[eng].append((seq, fn, waits, dma))

    def flush(self):
        nc = self.nc
        if getattr(self, 'stopped', False):
            self._reset()
            return
        last = {}
        for e in ENGS:
            cs = [o[0] for o in self.ops[e] if o[3] is None and o[1] is not None]
            if cs:
                last[e] = cs[-1]
                self.signal[e].add(cs[-1])
        for e in ENGS:
            waits = {}
            for e2, s in last.items():
                if e2 != e and self.known[e].get(('E', e2), -1) < s:
                    waits[('E', e2)] = s
                    self.known[e][('E', e2)] = s
            for c, v in self.dcount.items():
                if self.known[e].get(('D', c), -1) < v:
                    waits[('D', c)] = v
                    self.known[e][('D', c)] = v
            self.ops[e].append((None, None, waits, None))
        rank = {}
        for e in ENGS:
            srt = sorted(self.signal[e])
            rank[e] = {s: self.nsig[e] + i + 1 for i, s in enumerate(srt)}
            self.nsig[e] += len(srt)
        esem, dsem = self.esem, self.dsem

        def run(e, engobj):
            sig = self.signal[e]
            for (seq, fn, waits, dma) in self.ops[e]:
                for k, v in waits.items():
                    if k[0] == 'E':
                        engobj.wait_ge(esem[k[1]], rank[k[1]][v])
                    else:
                        engobj.wait_ge(dsem[k[1]], 16 * v)
                if fn is None:
                    continue
                ins = fn(engobj)
                if dma is not None:
                    ins.then_inc(dsem[dma], 16)
                elif seq in sig:
                    ins.then_inc(esem[e], 1)

        with nc.Block() as block:
            @block.tensor
            def _(eng):
                run('pe', eng)

            @block.scalar
            def _(eng):
                run('act', eng)

            @block.vector
            def _(eng):
                run('dve', eng)

            @block.gpsimd
            def _(eng):
                run('pool', eng)

            @block.sync
            def _(eng):
                run('sp', eng)
        self._reset()
        self.nflush = getattr(self, 'nflush', 0) + 1
        if self.nflush >= _STOP:
            self.stopped = True


def build():
    nc = bass.Bass("TRN2", target_bir_lowering=False)

    def din(name, shape, dt=F32):
        return nc.dram_tensor(name, list(shape), dt, kind="ExternalInput").ap()

    def dscr(name, shape, dt):
        return nc.dram_tensor(name, list(shape), dt, kind=("ExternalOutput" if name in _DBG else "Internal")).ap()

    xT_full = din("xT_full", [D, S_ALL])
    xT_ext = din("xT_ext", [D, EXT])
    pT = din("pT", [256, T])
    gains = din("gains", [128, 8 * 32])
    w_in = din("w_in", [D, 17968])
    peT_k = din("peT_k", [128, 32]); peT_v = din("peT_v", [128, 32])
    w_ck1 = din("w_ck1", [4096, 128]); w_ck2 = din("w_ck2", [128, 128])
    w_cv1 = din("w_cv1", [4096, 128]); w_cv2 = din("w_cv2", [128, 128])
    w_a = din("w_a", [512, D]); w_b = din("w_b", [2048, D]); w_out = din("w_out", [D, D])
    w_gu = din("w_gu", [D, 2 * DFF]); w_down = din("w_down", [DFF, D])
    w_pg = din("w_pg", [D, D]); w_ple = din("w_ple", [256, D])
    cs_full = din("cs_full", [128, 2, S_ALL])
    cs_ext = din("cs_ext", [128, 2, EXT])
    consts = din("consts", [128, 3, 128], BF16)
    xsel = din("xsel", [128, S_ALL], BF16)
    ovm = din("ovm", [128, 4, 128], BF16)
    gsel = din("gsel", [48, 48, 128], BF16)
    biasC = din("biasC", [8, 128, 4, 512], BF16)
    addF = din("addF", [8, 128, 128])
    diagb = din("diagb", [128, 8, 512], BF16)
    biasW = din("biasW", [8, 128, 5, 512], BF16)
    biasD = din("biasD", [128, 22, 512], BF16)
    outT = nc.dram_tensor("outT", [D, T], F32, kind="ExternalOutput").ap()

    kcT_d = dscr("kcT_d", [8, 128, S_ALL], BF16)
    kslT_d = dscr("kslT_d", [4, 128, S_ALL], BF16)
    vsl_d = dscr("vsl_d", [S_ALL, 512], BF16)
    kaT_d = dscr("kaT_d", [12, 128, EXT], BF16)
    va_d = dscr("va_d", [EXT, 1536], BF16)
    kwnT_d = dscr("kwnT_d", [4, 128, EXT], BF16)
    vwn_d = dscr("vwn_d", [EXT, 512], BF16)
    gmix_d = dscr("gmix_d", [64, 128, T], BF16)
    pre_d = dscr("pre_d", [32, 128, T], F32)
    x1_d = dscr("x1_d", [32, 128, T], F32)
    x2_d = dscr("x2_d", [32, 128, T], F32)
    act_d = dscr("act_d", [FKC, 128, T], BF16)
    qaT_d = dscr("qaT_d", [12, 128, T], BF16)
    qrT_d = dscr("qrT_d", [16, 128, T], BF16)
    qoT_d = dscr("qoT_d", [16, 128, T], BF16)
    h_d = dscr("h_d", [32, 128, T], BF16)
    ya_d = dscr("ya_d", [4, 128, T], BF16)
    yb_d = dscr("yb_d", [16, 128, T], BF16)

    with ExitStack() as top:
      try:
        S = Sched(nc, top)
        _build_body(nc, top, S, locals())
      except StopBuild:
        pass
    return nc


def _build_body(nc, top, S, L):
    globals().update({k: v for k, v in L.items() if k not in ('nc', 'top', 'S')})
    if True:
        _uid = [0]

        def sbt(st, name, shape, dt):
            _uid[0] += 1
            return st.enter_context(nc.sbuf_tensor("%s_%d" % (name, _uid[0]), list(shape), dt))
        ps = top.enter_context(nc.psum_tensor("ps", [128, 8, 512], F32))
        cst = sbt(top, "cst", [128, 3, 128], BF16)
        ident, swp, ones = cst[:, 0, :], cst[:, 1, :], cst[:, 2, :]
        gn = sbt(top, "gn", [128, 8 * 32], F32)
        kc_c = sbt(top, "kc_c", [128, 4, 512], BF16)
        vc_c = sbt(top, "vc_c", [128, 4, 4, 128], BF16)
        gsT = sbt(top, "gsT", [48, T], BF16)
        rs2p = sbt(top, "rs2p", [128, 2, 512], F32)

        S.op('sp', lambda e: e.dma_start(out=cst[:], in_=consts), writes=['cst'], dma='c')
        S.op('sp', lambda e: e.dma_start(out=gn[:], in_=gains), writes=['cst'], dma='c')
        epsb = sbt(top, "epsb", [128, 1], F32)
        S.op('pool', lambda e: e.memset(epsb[:], EPS), writes=['epsb'])
        S.op('pool', lambda e: e.memset(vc_c[:], 0.0), writes=['vc_c'])
        S.op('pool', lambda e: e.memset(kc_c[:], 0.0), writes=['kc_c'])
        G_MIXPRE, G_MIXPOST, G_FFNPRE, G_FFNPOST, G_PLEPRE, G_PLEPOST = range(6)

        def gcol(gi, c):
            return gn[:, gi * 32 + c: gi * 32 + c + 1]

        bank_rr = [0]

        def nb(n=4, base=0):
            b = base + bank_rr[0] % n
            bank_rr[0] += 1
            return b

        xT_full_v = xT_full.rearrange("(c p) t -> p c t", p=128)
        xT_ext_v = xT_ext.rearrange("(c p) t -> p c t", p=128)

        def norm_tile(st_, xs, hT, src_v, t0, gi, sq, rs):
            for q in range(4):
                S.op('sp', lambda e, q=q: e.dma_start(out=xs[:, 8 * q:8 * q + 8, :], in_=src_v[:, 8 * q:8 * q + 8, t0:t0 + 512]),
                     writes=[('xs', q)], dma='xs%d' % q)
            b = 7
            for c in range(KC):
                S.op('act', lambda e, c=c: e.activation(out=sq[:, c % 2, :], in_=xs[:, c, :], func=AF.Square),
                     reads=[('xs', c // 8)], writes=[('sq', c % 2)])
                S.op('pe', lambda e, c=c: e.matmul(ps[:, b, :], lhsT=ones, rhs=sq[:, c % 2, :], start=(c == 0), stop=(c == KC - 1)),
                     reads=[('sq', c % 2), 'cst'], writes=[('ps', b)])
            S.op('act', lambda e: e.activation(out=rs[:], in_=ps[:, b, :], func=AF.Sqrt, scale=1.0 / D, bias=epsb[:]),
                 reads=[('ps', b), 'epsb'], writes=['rs'])
            S.op('dve', lambda e: e.reciprocal(out=rs[:], in_=rs[:]), reads=['rs'], writes=['rs'])
            for c in range(KC):
                S.op('dve', lambda e, c=c: e.scalar_tensor_tensor(out=hT[:, c, :], in0=xs[:, c, :], scalar=gcol(gi, c), in1=rs[:], op0=ALU.mult, op1=ALU.mult),
                     reads=[('xs', c // 8), 'rs', 'cst'], writes=[('hT', id(hT), c)])

        wslot = [0]

        def load_w(wb, w_ap, r0, kc_n, c0, ncols):
            s = wslot[0] % 2
            wslot[0] += 1
            src = w_ap[r0:r0 + kc_n * 128, c0:c0 + ncols].rearrange("(k p) c -> p k c", p=128)
            half = max(1, kc_n // 2)
            for h0 in range(0, kc_n, half):
                h1 = min(kc_n, h0 + half)
                S.op('pool', lambda e, s=s, h0=h0, h1=h1: e.dma_start(out=wb[:, s, h0:h1, :ncols], in_=src[:, h0:h1, :]),
                     writes=[('wb', s)], dma='wb%d' % s)
            return s

        def mm_fm(wb, s, kc_n, csub, act_fn, n, b, extra_reads=()):
            for kc in range(kc_n):
                S.op('pe', lambda e, kc=kc: e.matmul(ps[:, b, :n], lhsT=wb[:, s, kc, csub * 128:(csub + 1) * 128], rhs=act_fn(kc),
                                                    start=(kc == 0), stop=(kc == kc_n - 1)),
                     reads=[('wb', s)] + list(extra_reads), writes=[('ps', b)])

        def rope_store(b, n, cs_t, t_off, dst_fn, tmpq, tmp1, key, extra_reads=()):
            i = key % 2
            S.op('dve', lambda e: e.tensor_copy(out=tmpq[:, i, :n], in_=ps[:, b, :n]), reads=[('ps', b)] + list(extra_reads), writes=[('tq', i)])
            b2 = nb()
            S.op('pe', lambda e: e.matmul(ps[:, b2, :n], lhsT=swp, rhs=tmpq[:, i, :n], start=True, stop=True),
                 reads=[('tq', i), 'cst'], writes=[('ps', b2)])
            S.op('dve', lambda e: e.tensor_tensor(out=tmp1[:, i, :n], in0=ps[:, b, :n], in1=cs_t[:, 0, t_off:t_off + n], op=ALU.mult),
                 reads=[('ps', b), 'cs'] + list(extra_reads), writes=[('t1', i)])
            S.op('dve', lambda e: e.tensor_tensor(out=tmp1[:, 2 + i, :n], in0=ps[:, b2, :n], in1=cs_t[:, 1, t_off:t_off + n], op=ALU.mult),
                 reads=[('ps', b2), 'cs'] + list(extra_reads), writes=[('t2', i)])
            dst, wkeys = dst_fn()
            S.op('dve', lambda e: e.tensor_tensor(out=dst, in0=tmp1[:, i, :n], in1=tmp1[:, 2 + i, :n], op=ALU.add),
                 reads=[('t1', i), ('t2', i)], writes=wkeys)

        for pss in range(2):
            with ExitStack() as st:
                wres = sbt(st, "wres", [128, KC, 1024], BF16)
                xsb = sbt(st, "xsb", [128, KC, 512], BF16)
                hT2 = [sbt(st, "hTa", [128, KC, 512], BF16), sbt(st, "hTb", [128, KC, 512], BF16)]
                sq = sbt(st, "sq", [128, 2, 512], BF16)
                rs = sbt(st, "rs", [128, 512], F32)
                ob = sbt(st, "ob", [128, 4, 512], BF16)
                tmpq = sbt(st, "tmpq", [128, 2, 512], BF16)
                tmp1 = sbt(st, "tmp1", [128, 4, 512], F32)
                cs_t = sbt(st, "cs_t", [128, 2, 2, 512], F32)
                c0 = C_KC if pss == 0 else C_KSL
                srcw = w_in[:, c0:c0 + 1024].rearrange("(k p) c -> p k c", p=128)
                for q in range(4):
                    S.op('pool', lambda e, q=q: e.dma_start(out=wres[:, 8 * q:8 * q + 8, :], in_=srcw[:, 8 * q:8 * q + 8, :]),
                         writes=['wres'], dma='wres')
                NT = S_ALL // 512

                def normA(tt):
                    t0 = tt * 512
                    for q in range(4):
                        S.op('pool', lambda e, q=q: e.dma_start(out=xsb[:, 8 * q:8 * q + 8, :], in_=xT_full_v[:, 8 * q:8 * q + 8, t0:t0 + 512]),
                             writes=[('xs', q)], dma='xs%d' % q)
                    for c in range(KC):
                        S.op('act', lambda e, c=c: e.activation(out=sq[:, c % 2, :], in_=xsb[:, c, :], func=AF.Square),
                             reads=[('xs', c // 8)], writes=[('sq', c % 2)])
                        S.op('pe', lambda e, c=c: e.matmul(ps[:, 7, :], lhsT=ones, rhs=sq[:, c % 2, :], start=(c == 0), stop=(c == KC - 1)),
                             reads=[('sq', c % 2), 'cst'], writes=[('ps', 7)])
                    S.op('act', lambda e: e.activation(out=rs[:], in_=ps[:, 7, :], func=AF.Sqrt, scale=1.0 / D, bias=epsb[:]),
                         reads=[('ps', 7), 'epsb'], writes=['rs'])
                    S.op('dve', lambda e: e.reciprocal(out=rs[:], in_=rs[:]), reads=['rs'], writes=['rs'])

                def normB(tt):
                    h = hT2[tt % 2]
                    for c in range(KC):
                        S.op('dve', lambda e, c=c: e.scalar_tensor_tensor(out=h[:, c, :], in0=xsb[:, c, :], scalar=gcol(G_MIXPRE, c), in1=rs[:], op0=ALU.mult, op1=ALU.mult),
                             reads=[('xs', c // 8), 'rs', 'cst'], writes=[('hT', tt % 2, c)])

                def unit_fm(tt, ct):
                    hT = hT2[tt % 2]
                    hreads = [('hT', tt % 2, c) for c in range(KC)]
                    t0 = tt * 512
                    b = nb()
                    for kc in range(KC):
                        S.op('pe', lambda e, kc=kc: e.matmul(ps[:, b, :], lhsT=wres[:, kc, ct * 128:(ct + 1) * 128], rhs=hT[:, kc, :],
                                                             start=(kc == 0), stop=(kc == KC - 1)),
                             reads=['wres'] + (hreads if kc == 0 else []), writes=[('ps', b)])
                    o = ct % 4
                    if pss == 0:
                        S.op('act', lambda e: e.copy(out=ob[:, o, :], in_=ps[:, b, :]), reads=[('ps', b)], writes=[('ob', o)])
                        S.op('sp', lambda e: e.dma_start(out=kcT_d[ct, :, t0:t0 + 512], in_=ob[:, o, :]), reads=[('ob', o)], dma='ob%d' % o)
                    else:
                        rope_store(b, 512, cs_t[:, tt % 2], 0, lambda: (ob[:, o, :], [('ob', o)]), tmpq, tmp1, ct, [('cs', tt % 2)])
                        S.op('sp', lambda e: e.dma_start(out=kslT_d[ct, :, t0:t0 + 512], in_=ob[:, o, :]), reads=[('ob', o)], dma='ob%d' % o)

                def unit_tm(tt, tk):
                    hT = hT2[tt % 2]
                    hreads = [('hT', tt % 2, c) for c in range(KC)]
                    t0 = tt * 512
                    b = nb()
                    for kc in range(KC):
                        S.op('pe', lambda e, kc=kc: e.matmul(ps[:, b, :], lhsT=hT[:, kc, tk * 128:(tk + 1) * 128], rhs=wres[:, kc, 512:1024],
                                                             start=(kc == 0), stop=(kc == KC - 1)),
                             reads=['wres'] + (hreads if kc == 0 else []), writes=[('ps', b)])
                    S.op('act', lambda e: e.copy(out=ob[:, tk, :], in_=ps[:, b, :]), reads=[('ps', b)], writes=[('ob', tk)])
                    S.op('sp', lambda e: e.dma_start(out=vsl_d[t0 + tk * 128:t0 + (tk + 1) * 128, :], in_=ob[:, tk, :]), reads=[('ob', tk)], dma='ob%d' % tk)

                normA(0)
                normB(0)
                for tt in range(NT):
                    if pss == 1:
                        S.op('sp', lambda e, tt=tt: e.dma_start(out=cs_t[:, tt % 2], in_=cs_full[:, :, tt * 512:(tt + 1) * 512]), writes=[('cs', tt % 2)], dma='cs%d' % (tt % 2))
                        units = [(unit_fm, ct) for ct in range(4)] + [(unit_tm, tk) for tk in range(4)]
                    else:
                        units = [(unit_fm, ct) for ct in range(8)]
                    for fn_, a_ in units[:4]:
                        fn_(tt, a_)
                    if tt + 1 < NT:
                        normA(tt + 1)
                        normB(tt + 1)
                    for fn_, a_ in units[4:]:
                        fn_(tt, a_)
                S.flush()

        with ExitStack() as st:
            kin = sbt(st, "kin", [128, 2, S_ALL], BF16)
            w1 = sbt(st, "w1", [128, 2, 32, 128], BF16)
            w2 = sbt(st, "w2", [128, 2, 128], BF16)
            pe_b = sbt(st, "pe_b", [128, 2, 32], BF16)
            pe2 = sbt(st, "pe2", [128, 2, 32, 2], BF16)
            cb = sbt(st, "cb", [128, 2], F32)
            xg = sbt(st, "xg", [128, 2, 512], F32)
            tg = sbt(st, "tg", [128, 2, 512], F32)
            ge = sbt(st, "ge", [128, 2, 512], BF16)
            for kv, (wa1, wa2, pea) in enumerate(((w_ck1, w_ck2, peT_k), (w_cv1, w_cv2, peT_v))):
                S.op('pool', lambda e, kv=kv, wa1=wa1: e.dma_start(out=w1[:, kv], in_=wa1.rearrange("(l d) h -> d l h", d=128)), writes=['w1'], dma='c')
                S.op('pool', lambda e, kv=kv, wa2=wa2: e.dma_start(out=w2[:, kv], in_=wa2), writes=['w1'], dma='c')
                S.op('pool', lambda e, kv=kv, pea=pea: e.dma_start(out=pe_b[:, kv], in_=pea), writes=['w1'], dma='c')
            for j2 in range(2):
                S.op('dve', lambda e, j2=j2: e.tensor_copy(out=pe2[:, :, :, j2], in_=pe_b[:]), reads=['w1'], writes=[('pe2', j2)])
            for kv in range(2):
                b = nb()
                for l in range(32):
                    S.op('pe', lambda e, l=l, kv=kv, b=b: e.matmul(ps[:, b, 0:2], lhsT=w1[:, kv, l, :], rhs=pe2[:, kv, l, :], start=(l == 0), stop=(l == 31)),
                         reads=['w1', ('pe2', 0), ('pe2', 1)], writes=[('ps', b)])
                S.op('dve', lambda e, kv=kv, b=b: e.tensor_copy(out=cb[:, kv:kv + 1], in_=ps[:, b, 0:1]), reads=[('ps', b)], writes=[('cb', kv)])
            NCMP = 511
            for g in range(4):
                for kv in range(2):
                    i = (g * 2 + kv) % 2
                    S.op('sp', lambda e, g=g, kv=kv, i=i: e.dma_start(out=kin[:, i, :], in_=kcT_d[kv * 4 + g]), writes=[('kin', i)], dma='kin%d' % i)
                    b = nb()
                    for l in range(32):
                        S.op('pe', lambda e, l=l, kv=kv, i=i, b=b: e.matmul(ps[:, b, :NCMP], lhsT=w1[:, kv, l, :], rhs=kin[:, i, l:l + 16 * (NCMP - 1) + 1:16],
                                                                      start=(l == 0), stop=(l == 31)),
                             reads=['w1', ('kin', i)], writes=[('ps', b)])
                    S.op('dve', lambda e, kv=kv, i=i, b=b: e.tensor_scalar(out=xg[:, i, :NCMP], in0=ps[:, b, :NCMP], scalar1=cb[:, kv:kv + 1], scalar2=None, op0=ALU.add),
                         reads=[('ps', b), ('cb', kv)], writes=[('xg', i)])
                    S.op('dve', lambda e, i=i: e.tensor_tensor(out=tg[:, i, :NCMP], in0=xg[:, i, :NCMP], in1=xg[:, i, :NCMP], op=ALU.mult),
                         reads=[('xg', i)], writes=[('tg', i)])
                    S.op('dve', lambda e, i=i: e.tensor_scalar(out=tg[:, i, :NCMP], in0=tg[:, i, :NCMP], scalar1=0.044715, scalar2=1.0, op0=ALU.mult, op1=ALU.add),
                         reads=[('tg', i)], writes=[('tg', i)])
                    S.op('dve', lambda e, i=i: e.tensor_tensor(out=tg[:, i, :NCMP], in0=tg[:, i, :NCMP], in1=xg[:, i, :NCMP], op=ALU.mult),
                         reads=[('tg', i), ('xg', i)], writes=[('tg', i)])
                    S.op('act', lambda e, i=i: e.activation(out=tg[:, i, :NCMP], in_=tg[:, i, :NCMP], func=AF.Sigmoid, scale=1.5957691216057308),
                         reads=[('tg', i)], writes=[('tg', i)])
                    S.op('dve', lambda e, i=i: e.tensor_tensor(out=ge[:, i, :NCMP], in0=tg[:, i, :NCMP], in1=xg[:, i, :NCMP], op=ALU.mult),
                         reads=[('tg', i), ('xg', i)], writes=[('ge', i)])
                    if kv == 0:
                        b2 = nb()
                        S.op('pe', lambda e, i=i, b2=b2: e.matmul(ps[:, b2, :NCMP], lhsT=w2[:, 0, :], rhs=ge[:, i, :NCMP], start=True, stop=True),
                             reads=['w1', ('ge', i)], writes=[('ps', b2)])
                        S.op('act', lambda e, g=g, b2=b2: e.copy(out=kc_c[:, g, :NCMP], in_=ps[:, b2, :NCMP]), reads=[('ps', b2)], writes=['kc_c'])
                    else:
                        for j in range(4):
                            m = 128 if j < 3 else 127
                            b2 = nb()
                            S.op('pe', lambda e, i=i, j=j, m=m, b2=b2: e.matmul(ps[:m, b2, :128], lhsT=ge[:, i, j * 128:j * 128 + m], rhs=w2[:, 1, :], start=True, stop=True),
                                 reads=['w1', ('ge', i)], writes=[('ps', b2)])
                            S.op('act', lambda e, g=g, j=j, m=m, b2=b2: e.copy(out=vc_c[:m, g, j, :], in_=ps[:m, b2, :128]), reads=[('ps', b2)], writes=['vc_c'])
            S.flush()

        for half in range(3):
            with ExitStack() as st:
                hT3 = [sbt(st, "hT3_%d" % i, [128, KC, 512], BF16) for i in range(2)]
                xs = sbt(st, "xs", [128, KC, 512], F32)
                sq = sbt(st, "sq", [128, 2, 512], BF16)
                rs = sbt(st, "rs", [128, 512], F32)
                wb = sbt(st, "wb", [128, 2, KC, 256], BF16)
                ob = sbt(st, "ob", [128, 4, 512], BF16)
                tmpq = sbt(st, "tmpq", [128, 2, 512], BF16)
                tmp1 = sbt(st, "tmp1", [128, 4, 512], F32)
                cs_t = sbt(st, "cs_t", [128, 2, 1024], F32)
                tb = half * 1024
                S.op('sp', lambda e, tb=tb: e.dma_start(out=cs_t[:], in_=cs_ext[:, :, tb:tb + 1024]), writes=['cs'], dma='cs')
                for i in range(2):
                    norm_tile(st, xs, hT3[i], xT_ext_v, tb + i * 512, G_MIXPRE, sq, rs)
                hr = [[('hT', id(hT3[i]), c) for c in range(KC)] for i in range(2)]
                okey = [0]

                def fm_cols(c0, ncols, tiles, sink):
                    for w0 in range(0, ncols, 256):
                        wn = min(256, ncols - w0)
                        s = load_w(wb, w_in, 0, KC, c0 + w0, wn)
                        for i in tiles:
                            for cs_ in range((wn + 127) // 128):
                                mrows = min(128, wn - cs_ * 128)
                                b = nb()
                                for kc in range(KC):
                                    S.op('pe', lambda e, kc=kc, s=s, cs_=cs_, i=i, b=b, mrows=mrows: e.matmul(
                                        ps[:mrows, b, :], lhsT=wb[:, s, kc, cs_ * 128:cs_ * 128 + mrows], rhs=hT3[i][:, kc, :],
                                        start=(kc == 0), stop=(kc == KC - 1)),
                                        reads=[('wb', s)] + (hr[i] if kc == 0 else []), writes=[('ps', b)])
                                sink((w0 + cs_ * 128) // 128, i, b, mrows)

                def tm_cols(c0, ncols, tiles, dst_d, dcol0):
                    for w0 in range(0, ncols, 256):
                        s = load_w(wb, w_in, 0, KC, c0 + w0, 256)
                        for i in tiles:
                            for tk in range(4):
                                b = nb()
                                for kc in range(KC):
                                    S.op('pe', lambda e, kc=kc, s=s, i=i, tk=tk, b=b: e.matmul(
                                        ps[:, b, :256], lhsT=hT3[i][:, kc, tk * 128:(tk + 1) * 128], rhs=wb[:, s, kc, :],
                                        start=(kc == 0), stop=(kc == KC - 1)),
                                        reads=[('wb', s)] + (hr[i] if kc == 0 else []), writes=[('ps', b)])
                                o = okey[0] % 4
                                okey[0] += 1
                                S.op('act', lambda e, b=b, o=o: e.copy(out=ob[:, o, :256], in_=ps[:, b, :256]), reads=[('ps', b)], writes=[('ob', o)])
                                r0 = tb + i * 512 + tk * 128
                                S.op('sp', lambda e, o=o, r0=r0, w0=w0: e.dma_start(out=dst_d[r0:r0 + 128, dcol0 + w0:dcol0 + w0 + 256], in_=ob[:, o, :256]),
                                     reads=[('ob', o)], dma='ob%d' % o)

                def sink_rope_dram(dst_d):
                    def sink(ct, i, b, mrows):
                        o = okey[0] % 4
                        okey[0] += 1
                        rope_store(b, 512, cs_t, i * 512, lambda o=o: (ob[:, o, :], [('ob', o)]), tmpq, tmp1, okey[0])
                        t0 = tb + i * 512
                        S.op('sp', lambda e, ct=ct, o=o, t0=t0: e.dma_start(out=dst_d[ct, :, t0:t0 + 512], in_=ob[:, o, :]),
                             reads=[('ob', o)], dma='ob%d' % o)
                    return sink

                fm_cols(C_KA, 1536, range(2), sink_rope_dram(kaT_d))
                fm_cols(C_KWN, 512, range(2), sink_rope_dram(kwnT_d))
                tm_cols(C_VA, 1536, range(2), va_d, 0)
                tm_cols(C_VWN, 512, range(2), vwn_d, 0)
                if half == 2:
                    own = (0, 1)

                    def sink_q(dst_d, extra=()):
                        def sink(ct, i, b, mrows):
                            t0 = i * 512
                            o = okey[0] % 4
                            okey[0] += 1
                            rope_store(b, 512, cs_t, i * 512, lambda o=o: (ob[:, o, :], [('ob', o)]), tmpq, tmp1, okey[0], extra)
                            S.op('sp', lambda e: e.dma_start(out=dst_d[ct, :, t0:t0 + 512], in_=ob[:, o, :]), reads=[('ob', o)], dma='ob%d' % o)
                        return sink
                    fm_cols(C_QA, 1536, own, sink_q(qaT_d))

                    def sink_qb(ct, i, b, mrows):
                        t0 = i * 512
                        o = okey[0] % 4
                        okey[0] += 1
                        S.op('act', lambda e: e.copy(out=ob[:, o, :], in_=ps[:, b, :]), reads=[('ps', b)], writes=[('ob', o)])
                        S.op('sp', lambda e: e.dma_start(out=qrT_d[ct, :, t0:t0 + 512], in_=ob[:, o, :]), reads=[('ob', o)], dma='ob%d' % o)
                        sink_q(qoT_d, [('ob', o)])(ct, i, b, mrows)
                    fm_cols(C_QB, 2048, own, sink_qb)

                    def sink_gn(ct, i, b, mrows):
                        t0 = i * 512
                        S.op('act', lambda e: e.activation(out=gsT[:, t0:t0 + 512], in_=ps[:48, b, :], func=AF.Sigmoid), reads=[('ps', b)], writes=['gsT'])
                    fm_cols(C_GN, 48, own, sink_gn)

                    def sink_gm(ct, i, b, mrows):
                        t0 = i * 512
                        o = okey[0] % 4
                        okey[0] += 1
                        S.op('act', lambda e: e.activation(out=ob[:, o, :], in_=ps[:, b, :], func=AF.Sigmoid), reads=[('ps', b)], writes=[('ob', o)])
                        S.op('sp', lambda e: e.dma_start(out=gmix_d[ct, :, t0:t0 + 512], in_=ob[:, o, :]), reads=[('ob', o)], dma='ob%d' % o)
                    fm_cols(C_GM, 8192, own, sink_gm)
                S.flush()

        acc_i = [0]

        def attn_chunks(chunks, nq, n_heads_cols, o_bank, d_bank):
            ncols = n_heads_cols
            nchunks = len(chunks)

            def emit_pv(ci, ch, pi):
                nk = ch['nk']
                for h in range(4):
                    v_ap, rd = ch['v_fn'](h)
                    S.op('pe', lambda e, v_ap=v_ap, h=h, nk=nk, pi=pi, ci=ci: e.matmul(
                        ps[:, o_bank, h * nq:(h + 1) * nq], lhsT=v_ap, rhs=PT[:nk, pi, h * nq:(h + 1) * nq],
                        start=(ci == 0 and h == 0), stop=(ci == nchunks - 1)), reads=rd + [('PT', pi)], writes=[('ps', o_bank)])
                S.op('pe', lambda e, nk=nk, pi=pi, ci=ci: e.matmul(ps[:, d_bank, :ncols], lhsT=cst[:nk, 2, :], rhs=PT[:nk, pi, :ncols],
                                                                start=(ci == 0), stop=(ci == nchunks - 1)),
                     reads=[('PT', pi), 'cst'], writes=[('ps', d_bank)])

            pend = None
            for ci, ch in enumerate(chunks):
                nk = ch['nk']
                b = nb()
                ops_ = ch['s_ops']
                for oi, (l_ap, r_ap, o_ap, rd) in enumerate(ops_):
                    S.op('pe', lambda e, l_ap=l_ap, r_ap=r_ap, o_ap=o_ap, b=b, st_=ch['starts'][oi], sp_=ch['stops'][oi]: e.matmul(
                        o_ap(b), lhsT=l_ap, rhs=r_ap, start=st_, stop=sp_), reads=rd, writes=[('ps', b)])
                pi = acc_i[0] % 3
                acc_i[0] += 1
                S.op('act', lambda e, b=b, nk=nk, pi=pi: e.activation(out=PT[:nk, pi, :ncols], in_=ps[:nk, b, :ncols], func=AF.Exp, scale=SCALE),
                     reads=[('ps', b)], writes=[('PT', pi)])
                if ch.get('keep') is not None:
                    ch['keep'](pi, nk)
                if not os.environ.get('MK_PIPEATTN'):
                    emit_pv(ci, ch, pi)
                    continue
                if pend is not None:
                    emit_pv(*pend)
                pend = (ci, ch, pi)
            if pend is not None:
                emit_pv(*pend)

        with ExitStack() as st:
            PT = sbt(st, "PT", [128, 3, 512], BF16)
            kaS = sbt(st, "kaS", [128, 4, EXT], BF16)
            vS = sbt(st, "vS", [128, 4, 512], BF16)
            bD = sbt(st, "bD", [128, 22, 512], BF16)
            numT = sbt(st, "numT", [128, 4, T], F32)
            denT = sbt(st, "denT", [128, 4, T], F32)
            qaT = sbt(st, "qaT", [128, 4, T], BF16)
            yaT = sbt(st, "yaT", [128, 4, T], BF16)
            S.op('sp', lambda e: e.dma_start(out=bD[:], in_=biasD), writes=['bD'], dma='c')
            vslot = [0]
            for grp, r in enumerate((1, 4, 16)):
                for h in range(4):
                    S.op('sp', lambda e, grp=grp, h=h: e.dma_start(out=kaS[:, h, :], in_=kaT_d[grp * 4 + h]), writes=['kaS'], dma='kaS')
                    S.op('sp', lambda e, grp=grp, h=h: e.dma_start(out=qaT[:, h, :], in_=qaT_d[grp * 4 + h]), writes=['qaT'], dma='qaT')
                nq = 128 if r < 16 else 64
                nblk = (T // r) // nq
                for rho in range(r):
                    for blk in range(nblk):
                        a0 = blk * nq
                        if grp == 0:
                            bidx = [blk * 2, blk * 2 + 1]
                        elif grp == 1:
                            bidx = [16 + blk * 2, 16 + blk * 2 + 1]
                        else:
                            bidx = [20, 21]
                        chunks = []
                        for ci, (ks, nk) in enumerate(((a0 - 128, 128), (a0, nq))):
                            e0 = HALO + r * ks + rho
                            vs = vslot[0] % 4
                            vslot[0] += 1
                            S.op('sp', lambda e, e0=e0, nk=nk, vs=vs, grp=grp, r=r: e.dma_start(
                                out=vS[:nk, vs, :], in_=va_d[e0:e0 + r * (nk - 1) + 1:r, grp * 512:(grp + 1) * 512]),
                                writes=[('vS', vs)], dma='vS%d' % vs)
                            q0 = rho + r * a0
                            s_ops = [(cst[:nk, 0, :nk], bD[:nk, bidx[ci], :4 * nq],
                                      (lambda b, nk=nk, nq=nq: ps[:nk, b, :4 * nq]), ['bD', 'cst'])]
                            for h in range(4):
                                s_ops.append((kaS[:, h, e0:e0 + r * (nk - 1) + 1:r],
                                              qaT[:, h, q0:q0 + r * (nq - 1) + 1:r],
                                              (lambda b, h=h, nk=nk, nq=nq: ps[:nk, b, h * nq:(h + 1) * nq]),
                                              ['kaS', 'qaT']))
                            chunks.append(dict(nk=nk, s_ops=s_ops, starts=[True] + [False] * 4, stops=[False] * 4 + [True],
                                               v_fn=(lambda h, vs=vs, nk=nk: (vS[:nk, vs, h * 128:(h + 1) * 128], [('vS', vs)]))))
                        ob_, db_ = (4, 5) if (acc_i[0] // 2) % 2 == 0 else (6, 7)
                        attn_chunks(chunks, nq, 4 * nq, ob_, db_)
                        q0 = rho + r * a0
                        dstn = numT[:, :, q0:q0 + r * (nq - 1) + 1:r]
                        dstd = denT[:, :, q0:q0 + r * (nq - 1) + 1:r]
                        srcn = ps[:, ob_, :4 * nq].rearrange("p (h q) -> p h q", h=4)
                        srcd = ps[:, db_, :4 * nq].rearrange("p (h q) -> p h q", h=4)
                        if grp == 0:
                            S.op('dve', lambda e, dstn=dstn, srcn=srcn: e.tensor_copy(out=dstn, in_=srcn), reads=[('ps', ob_)], writes=['numT'])
                            S.op('act', lambda e, dstd=dstd, srcd=srcd: e.copy(out=dstd, in_=srcd), reads=[('ps', db_)], writes=['denT'])
                        else:
                            S.op('dve', lambda e, dstn=dstn, srcn=srcn: e.tensor_tensor(out=dstn, in0=dstn, in1=srcn, op=ALU.add), reads=[('ps', ob_), 'numT'], writes=['numT'])
                            S.op('dve', lambda e, dstd=dstd, srcd=srcd: e.tensor_tensor(out=dstd, in0=dstd, in1=srcd, op=ALU.add), reads=[('ps', db_), 'denT'], writes=['denT'])
            S.op('dve', lambda e: e.reciprocal(out=denT[:], in_=denT[:]), reads=['denT'], writes=['denT'])
            S.op('dve', lambda e: e.tensor_tensor(out=yaT[:], in0=numT[:], in1=denT[:], op=ALU.mult), reads=['numT', 'denT'], writes=['yaT'])
            S.op('sp', lambda e: e.dma_start(out=ya_d.rearrange("h p t -> p h t"), in_=yaT[:]), reads=['yaT'], dma='c')
            S.flush()

        with ExitStack() as st:
            PT = sbt(st, "PT", [128, 3, 512], BF16)
            PK = sbt(st, "PK", [128, 4, 512], BF16)
            Ksel = sbt(st, "Ksel", [128, S_ALL], BF16)
            Vsel = sbt(st, "Vsel", [128, 64, 128], BF16)
            Kw = sbt(st, "Kw", [128, 1536], BF16)
            Vw = sbt(st, "Vw", [128, 12, 128], BF16)
            xs_sb = sbt(st, "xs_sb", [128, S_ALL], BF16)
            ov_sb = sbt(st, "ov_sb", [128, 4, 128], BF16)
            gs_sb = sbt(st, "gs_sb", [48, 48, 128], BF16)
            bC = sbt(st, "bC", [128, 4, 512], BF16)
            bW = sbt(st, "bW", [128, 5, 512], BF16)
            dg = sbt(st, "dg", [128, 8, 512], BF16)
            aF = sbt(st, "aF", [128, 128], F32)
            rden = sbt(st, "rden", [128, 512], F32)
            pn = sbt(st, "pn", [128, 2, 512], BF16)
            sc = sbt(st, "sc", [128, 128], F32)
            wk = sbt(st, "wk", [128, 128], F32)
            mx = sbt(st, "mx", [128, 16], F32)
            selb = sbt(st, "selb", [128, 128], BF16)
            selT = sbt(st, "selT", [128, 512], BF16)
            grep = sbt(st, "grep", [128, 512], F32)
            acc = sbt(st, "acc", [128, 512], F32)
            tmpo = sbt(st, "tmpo", [128, 512], F32)
            ybo = sbt(st, "ybo", [128, 512], BF16)
            qrT = sbt(st, "qrT", [128, 4, T], BF16)
            qoT = sbt(st, "qoT", [128, 4, T], BF16)
            S.op('sp', lambda e: e.dma_start(out=xs_sb[:], in_=xsel), writes=['k2'], dma='c')
            S.op('sp', lambda e: e.dma_start(out=ov_sb[:], in_=ovm), writes=['k2'], dma='c')
            S.op('sp', lambda e: e.dma_start(out=gs_sb[:], in_=gsel), writes=['k2'], dma='c')
            S.op('sp', lambda e: e.dma_start(out=dg[:], in_=diagb), writes=['k2'], dma='c')

            def finish_part(part, g, ob_, db_, first, QB0):
                S.op('dve', lambda e: e.tensor_scalar(out=rden[:], in0=ps[:, db_, :], scalar1=1e-30, scalar2=None, op0=ALU.max),
                     reads=[('ps', db_)], writes=['rden'])
                S.op('dve', lambda e: e.reciprocal(out=rden[:], in_=rden[:]), reads=['rden'], writes=['rden'])
                bg = nb()
                for h in range(4):
                    col = (g * 4 + h) * 3 + part
                    S.op('pe', lambda e, h=h, col=col: e.matmul(ps[:, bg, h * 128:(h + 1) * 128], lhsT=gs_sb[:, col, :], rhs=gsT[:, QB0:QB0 + 128], start=(h == 0), stop=True),
                         reads=['k2', 'gsT'], writes=[('ps', bg)])
                S.op('dve', lambda e: e.tensor_tensor(out=grep[:], in0=ps[:, bg, :], in1=rden[:], op=ALU.mult), reads=[('ps', bg), 'rden'], writes=['grep'])
                if first:
                    S.op('dve', lambda e: e.tensor_tensor(out=acc[:], in0=ps[:, ob_, :], in1=grep[:], op=ALU.mult), reads=[('ps', ob_), 'grep'], writes=['acc'])
                else:
                    S.op('dve', lambda e: e.tensor_tensor(out=tmpo[:], in0=ps[:, ob_, :], in1=grep[:], op=ALU.mult), reads=[('ps', ob_), 'grep'], writes=['tmpo'])
                    S.op('dve', lambda e: e.tensor_tensor(out=acc[:], in0=acc[:], in1=tmpo[:], op=ALU.add), reads=['acc', 'tmpo'], writes=['acc'])

            def nsa_block(g, qb):
                QB0 = qb * 128
                S.op('sp', lambda e, qb=qb: e.dma_start(out=bC[:], in_=biasC[qb]), writes=['bC'], dma='bC')
                S.op('sp', lambda e, qb=qb: e.dma_start(out=bW[:], in_=biasW[qb]), writes=['bW'], dma='bW')
                S.op('sp', lambda e, qb=qb: e.dma_start(out=aF[:], in_=addF[qb]), writes=['aF'], dma='aF')
                qr4 = qrT[:, :, QB0:QB0 + 128]
                qo4 = qoT[:, :, QB0:QB0 + 128]
                qrd = ['qrT']
                qod = ['qoT']
                full = lambda b: ps[:, b, :]
                chunks = []
                for j in range(4):
                    def keep(pi, nk, j=j):
                        S.op('pool', lambda e, pi=pi, j=j: e.tensor_copy(out=PK[:, j, :], in_=PT[:, pi, :]), reads=[('PT', pi)], writes=[('PK', j)])
                    chunks.append(dict(nk=128, starts=[True, False], stops=[False, True], keep=keep,
                                       s_ops=[(kc_c[:, g, j * 128:(j + 1) * 128], qr4, full, ['kc_c'] + qrd),
                                              (ident, bC[:, j, :], full, ['bC', 'cst'])],
                                       v_fn=(lambda h, j=j, g=g: (vc_c[:, g, j, :], ['vc_c']))))
                attn_chunks(chunks, 128, 512, 4, 5)
                finish_part(0, g, 4, 5, True, QB0)
                bi = nb()
                for j in range(4):
                    S.op('dve', lambda e, j=j: e.tensor_tensor(out=pn[:, j % 2, :], in0=PK[:, j, :], in1=rden[:], op=ALU.mult),
                         reads=[('PK', j), 'rden'], writes=[('pn', j % 2)])
                    for h in range(4):
                        S.op('pe', lambda e, j=j, h=h: e.matmul(ps[:, bi, :128], lhsT=pn[:, j % 2, h * 128:(h + 1) * 128], rhs=ov_sb[:, j, :],
                                                               start=(j == 0 and h == 0), stop=(j == 3 and h == 3)),
                             reads=[('pn', j % 2), 'k2'], writes=[('ps', bi)])
                S.op('dve', lambda e: e.tensor_tensor(out=sc[:], in0=ps[:, bi, :128], in1=aF[:], op=ALU.add), reads=[('ps', bi), 'aF'], writes=['sc'])
                S.op('dve', lambda e: e.max(out=mx[:, 0:8], in_=sc[:]), reads=['sc'], writes=['mx'])
                S.op('dve', lambda e: e.match_replace(out=wk[:], in_to_replace=mx[:, 0:8], in_values=sc[:], imm_value=-1e30), reads=['sc', 'mx'], writes=['wk'])
                S.op('dve', lambda e: e.max(out=mx[:, 8:16], in_=wk[:]), reads=['wk'], writes=['mx'])
                S.op('dve', lambda e: e.tensor_scalar(out=mx[:, 15:16], in0=mx[:, 15:16], scalar1=-1e29, scalar2=None, op0=ALU.max), reads=['mx'], writes=['mx'])
                S.op('dve', lambda e: e.tensor_scalar(out=selb[:], in0=sc[:], scalar1=mx[:, 15:16], scalar2=NEG, op0=ALU.is_lt, op1=ALU.mult),
                     reads=['sc', 'mx'], writes=['selb'])
                bt = nb()
                S.op('pe', lambda e: e.matmul(ps[:, bt, :128], lhsT=selb[:], rhs=ident, start=True, stop=True), reads=['selb', 'cst'], writes=[('ps', bt)])
                for h in range(4):
                    S.op('dve', (lambda e, h=h: e.tensor_copy(out=selT[:, h * 128:(h + 1) * 128], in_=ps[:, bt, :128])),
                         reads=[('ps', bt)], writes=[('selT', h)])
                selrd = [('selT', h) for h in range(4)]
                chunks = []
                nch = 57 + qb
                for j in range(nch):
                    s_ops = [(Ksel[:, j * 128:(j + 1) * 128], qo4, full, ['Ksel'] + qod),
                             (xs_sb[:, j * 128:(j + 1) * 128], selT[:], full, ['k2'] + selrd)]
                    if j % 8 == qb:
                        s_ops.append((ident, dg[:, j // 8, :], full, ['k2', 'cst']))
                    n = len(s_ops)
                    chunks.append(dict(nk=128, s_ops=s_ops, starts=[True] + [False] * (n - 1), stops=[False] * (n - 1) + [True],
                                       v_fn=(lambda h, j=j: (Vsel[:, j, :], ['Vsel']))))
                attn_chunks(chunks, 128, 512, 6, 7)
                finish_part(1, g, 6, 7, False, QB0)
                chunks = []
                for i in range(5):
                    kj = qb + i
                    chunks.append(dict(nk=128, starts=[True, False], stops=[False, True],
                                       s_ops=[(Kw[:, kj * 128:(kj + 1) * 128], qo4, full, ['Kw'] + qod),
                                              (ident, bW[:, i, :], full, ['bW', 'cst'])],
                                       v_fn=(lambda h, kj=kj: (Vw[:, kj, :], ['Vw']))))
                attn_chunks(chunks, 128, 512, 4, 5)
                finish_part(2, g, 4, 5, False, QB0)
                S.op('act', lambda e: e.copy(out=ybo[:], in_=acc[:]), reads=['acc'], writes=['ybo'])
                S.op('sp', lambda e: e.dma_start(out=yb_d[4 * g:4 * g + 4, :, QB0:QB0 + 128].rearrange("h p q -> p h q"), in_=ybo[:].rearrange("p (h q) -> p h q", h=4)), reads=['ybo'], dma='ybo')

            for g in range(4):
                S.op('sp', lambda e, g=g: e.dma_start(out=Ksel[:], in_=kslT_d[g]), writes=['Ksel'], dma='Ksel')
                for jj in range(8):
                    S.op('sp', lambda e, g=g, jj=jj: e.dma_start(out=Vsel[:, 8 * jj:8 * jj + 8, :], in_=vsl_d[1024 * jj:1024 * jj + 1024, g * 128:(g + 1) * 128].rearrange("(j k) d -> k j d", k=128)), writes=['Vsel'], dma='Vsel')
                S.op('sp', lambda e, g=g: e.dma_start(out=qrT[:], in_=qrT_d[4 * g:4 * g + 4].rearrange("h p t -> p h t")), writes=['qrT'], dma='qrT')
                S.op('sp', lambda e, g=g: e.dma_start(out=qoT[:], in_=qoT_d[4 * g:4 * g + 4].rearrange("h p t -> p h t")), writes=['qoT'], dma='qoT')
                S.op('sp', lambda e, g=g: e.dma_start(out=Kw[:], in_=kwnT_d[g, :, HALO - 512:EXT]), writes=['Kw'], dma='Kw')
                S.op('sp', lambda e, g=g: e.dma_start(out=Vw[:], in_=vwn_d[HALO - 512:EXT, g * 128:(g + 1) * 128].rearrange("(j k) d -> k j d", k=128)), writes=['Vw'], dma='Vw')
                for qb in range(8):
                    nsa_block(g, qb)
            S.flush()

        xo_v = xT_ext_v

        def epilogue(st, sums_banks, g_post, xin_fn, xout_d, g_next, hN, final_out=None):
            rsb = sbt(st, "rsb", [128, 2, 512], F32)
            hb = sbt(st, "hb", [128, 2, 512], BF16)
            pv = sbt(st, "pv", [128, 2, 512], F32)
            xv = sbt(st, "xv", [128, 2, 512], F32)
            sq2 = sbt(st, "sq2", [128, 2, 512], BF16)
            for tt in range(2):
                S.op('act', lambda e, tt=tt: e.activation(out=rsb[:, tt, :], in_=ps[:, sums_banks[tt], :], func=AF.Sqrt, scale=1.0 / D, bias=epsb[:]),
                     reads=[('ps', sums_banks[tt]), 'epsb'], writes=[('rsb', tt)])
                S.op('dve', lambda e, tt=tt: e.reciprocal(out=rsb[:, tt, :], in_=rsb[:, tt, :]), reads=[('rsb', tt)], writes=[('rsb', tt)])
            for tt in range(2):
                t0 = tt * 512
                for c in range(KC):
                    i = c % 2
                    S.op('sp', lambda e, c=c, i=i, t0=t0: e.dma_start(out=pv[:, i, :], in_=pre_d[c, :, t0:t0 + 512]), writes=[('pv', i)], dma='pv%d' % i)
                    S.op('sp', lambda e, c=c, i=i, t0=t0: e.dma_start(out=xv[:, i, :], in_=xin_fn(c, t0)), writes=[('xv', i)], dma='xv%d' % i)
                    S.op('dve', lambda e, c=c, i=i, tt=tt: e.scalar_tensor_tensor(out=pv[:, i, :], in0=pv[:, i, :], scalar=gcol(g_post, c), in1=rsb[:, tt, :], op0=ALU.mult, op1=ALU.mult),
                         reads=[('pv', i), ('rsb', tt), 'cst'], writes=[('pv', i)])
                    S.op('dve', lambda e, i=i: e.tensor_tensor(out=xv[:, i, :], in0=xv[:, i, :], in1=pv[:, i, :], op=ALU.add),
                         reads=[('pv', i), ('xv', i)], writes=[('xv', i)])
                    dst = final_out if final_out is not None else xout_d
                    S.op('sp', lambda e, c=c, i=i, t0=t0, dst=dst: e.dma_start(
                        out=(dst[c * 128:(c + 1) * 128, t0:t0 + 512] if final_out is not None else dst[c, :, t0:t0 + 512]), in_=xv[:, i, :]),
                        reads=[('xv', i)], dma='xo%d' % i)
                    if hN is not None:
                        S.op('act', lambda e, i=i: e.activation(out=sq2[:, i, :], in_=xv[:, i, :], func=AF.Square), reads=[('xv', i)], writes=[('sq2', i)])
                        S.op('pe', lambda e, c=c, i=i, tt=tt: e.matmul(ps[:, sums_banks[tt], :], lhsT=ones, rhs=sq2[:, i, :], start=(c == 0), stop=(c == KC - 1)),
                             reads=[('sq2', i), 'cst'] + ([('rsb', tt)] if c == 0 else []), writes=[('ps', sums_banks[tt])])
                        S.op('act', lambda e, c=c, i=i: e.activation(out=hb[:, i, :], in_=xv[:, i, :], func=AF.Copy, scale=gcol(g_next, c)),
                             reads=[('xv', i), 'cst'], writes=[('hb', i)])
                        S.op('sp', lambda e, c=c, i=i, t0=t0: e.dma_start(out=h_d[c, :, t0:t0 + 512], in_=hb[:, i, :]), reads=[('hb', i)], dma='hb%d' % i)
                if hN is not None:
                    S.op('act', lambda e, tt=tt: e.activation(out=rs2p[:, tt, :], in_=ps[:, sums_banks[tt], :], func=AF.Sqrt, scale=1.0 / D, bias=epsb[:]),
                         reads=[('ps', sums_banks[tt]), 'epsb'], writes=[('rs2', tt)])
                    S.op('dve', lambda e, tt=tt: e.reciprocal(out=rs2p[:, tt, :], in_=rs2p[:, tt, :]), reads=[('rs2', tt)], writes=[('rs2', tt)])

        def load_h(hT_):
            for q in range(4):
                S.op('sp', lambda e, q=q: e.dma_start(out=hT_[:, 8 * q:8 * q + 8, :], in_=h_d[8 * q:8 * q + 8].rearrange("c p t -> p c t")), writes=[('hld', q)], dma='hld')
            for c in range(KC):
                for tt in range(2):
                    S.op('dve' if (c + tt) % 2 else 'pool', lambda e, c=c, tt=tt: e.tensor_tensor(out=hT_[:, c, tt * 512:(tt + 1) * 512], in0=hT_[:, c, tt * 512:(tt + 1) * 512], in1=rs2p[:, tt, :], op=ALU.mult),
                         reads=[('hld', c // 8)], writes=[('hN', c, tt)])

        def pre_store(st_bufs, b, ct, tt, okey):
            po, sqp = st_bufs
            o = okey[0] % 2
            okey[0] += 1
            S.op('dve', lambda e: e.tensor_copy(out=po[:, o, :], in_=ps[:, b, :]), reads=[('ps', b)], writes=[('po', o)])
            S.op('act', lambda e: e.activation(out=sqp[:, o, :], in_=po[:, o, :], func=AF.Square), reads=[('po', o)], writes=[('sqp', o)])
            S.op('pe', lambda e: e.matmul(ps[:, 6 + tt, :], lhsT=ones, rhs=sqp[:, o, :], start=(ct == 0), stop=(ct == KC - 1)),
                 reads=[('sqp', o), 'cst'], writes=[('ps', 6 + tt)])
            S.op('sp', lambda e: e.dma_start(out=pre_d[ct, :, tt * 512:(tt + 1) * 512], in_=po[:, o, :]), reads=[('po', o)], dma='po%d' % o)

        if True:
            with ExitStack() as st:
                yaT = sbt(st, "yaT", [128, 4, T], BF16)
                ybT = sbt(st, "ybT", [128, 16, T], BF16)
                S.op('sp', lambda e: e.dma_start(out=yaT[:], in_=ya_d.rearrange("h p t -> p h t")), writes=['yaT'], dma='c')
                S.op('sp', lambda e: e.dma_start(out=ybT[:], in_=yb_d.rearrange("h p t -> p h t")), writes=['ybT'], dma='c')
                mT = sbt(st, "mT", [128, KC, T], BF16)
                wb = sbt(st, "wb", [128, 2, KC, 256], BF16)
                gm = sbt(st, "gm", [128, 2, 2, T], BF16)
                t1 = sbt(st, "t1", [128, 2, 512], F32)
                t2 = sbt(st, "t2", [128, 2, 512], F32)
                po = sbt(st, "po", [128, 2, 512], F32)
                sqp = sbt(st, "sqp", [128, 2, 512], BF16)
                k = [0]
                for w0 in range(0, D, 256):
                    sa = load_w(wb, w_a, 0, 4, w0, 256)
                    sb_ = load_w(wb, w_b, 0, 16, w0, 256)
                    for cs_ in range(2):
                        ct = w0 // 128 + cs_
                        gi = ct % 2
                        S.op('sp', lambda e, ct=ct, gi=gi: e.dma_start(out=gm[:, gi, 0, :], in_=gmix_d[ct]), writes=[('gm', gi)], dma='gm%d' % gi)
                        S.op('sp', lambda e, ct=ct, gi=gi: e.dma_start(out=gm[:, gi, 1, :], in_=gmix_d[32 + ct]), writes=[('gm', gi)], dma='gm%d' % gi)
                        for tt in range(2):
                            t0 = tt * 512
                            ba = nb(); bb = nb()
                            mm_fm(wb, sa, 4, cs_, lambda kc, t0=t0: yaT[:, kc, t0:t0 + 512], 512, ba, ['yaT'])
                            mm_fm(wb, sb_, 16, cs_, lambda kc, t0=t0: ybT[:, kc, t0:t0 + 512], 512, bb, ['ybT'])
                            o = k[0] % 2
                            k[0] += 1
                            S.op('dve', lambda e, ba=ba, o=o, gi=gi, t0=t0: e.tensor_tensor(out=t1[:, o, :], in0=ps[:, ba, :], in1=gm[:, gi, 0, t0:t0 + 512], op=ALU.mult),
                                 reads=[('ps', ba), ('gm', gi)], writes=[('t1', o)])
                            S.op('dve', lambda e, bb=bb, o=o, gi=gi, t0=t0: e.tensor_tensor(out=t2[:, o, :], in0=ps[:, bb, :], in1=gm[:, gi, 1, t0:t0 + 512], op=ALU.mult),
                                 reads=[('ps', bb), ('gm', gi)], writes=[('t2', o)])
                            S.op('dve', lambda e, o=o, ct=ct, t0=t0: e.tensor_tensor(out=mT[:, ct, t0:t0 + 512], in0=t1[:, o, :], in1=t2[:, o, :], op=ALU.add),
                                 reads=[('t1', o), ('t2', o)], writes=[('mT', ct)])
                mrd = [('mT', c) for c in range(KC)]
                ok = [0]
                for w0 in range(0, D, 256):
                    s = load_w(wb, w_out, 0, KC, w0, 256)
                    for cs_ in range(2):
                        ct = w0 // 128 + cs_
                        for tt in range(2):
                            t0 = tt * 512
                            b = nb()
                            mm_fm(wb, s, KC, cs_, lambda kc, t0=t0: mT[:, kc, t0:t0 + 512], 512, b, mrd)
                            pre_store((po, sqp), b, ct, tt, ok)
                S.flush()
            with ExitStack() as st:
                epilogue(st, (6, 7), G_MIXPOST, lambda c, t0: xT_ext_v[:, c, HALO + t0:HALO + t0 + 512], x1_d, G_FFNPRE, True)
                S.flush()

            with ExitStack() as st:
                hT2 = sbt(st, "hT2", [128, KC, T], BF16)
                load_h(hT2)
                wb = sbt(st, "wb", [128, 4, KC, 128], BF16)
                ao = sbt(st, "ao", [128, 2, T], BF16)
                sg = sbt(st, "sg", [128, 2, 512], F32)
                h2rd = [('hN', c, tt) for c in range(KC) for tt in range(2)]
                wk_ = [0]
                for ft in range(FKC):
                    slots = []
                    for half_, c0 in enumerate((ft * 128, DFF + ft * 128)):
                        s = wk_[0] % 4
                        wk_[0] += 1
                        src = w_gu[:, c0:c0 + 128].rearrange("(k p) c -> p k c", p=128)
                        S.op('pool', lambda e, s=s, src=src: e.dma_start(out=wb[:, s, :16, :], in_=src[:, :16, :]), writes=[('wb', s)], dma='wb%d' % s)
                        S.op('pool', lambda e, s=s, src=src: e.dma_start(out=wb[:, s, 16:, :], in_=src[:, 16:, :]), writes=[('wb', s)], dma='wb%d' % s)
                        slots.append(s)
                    ai = ft % 2
                    for tt in range(2):
                        t0 = tt * 512
                        bg = nb(); bu = nb()
                        for kc in range(KC):
                            S.op('pe', lambda e, kc=kc, bg=bg, s=slots[0], t0=t0: e.matmul(ps[:, bg, :], lhsT=wb[:, s, kc, :], rhs=hT2[:, kc, t0:t0 + 512], start=(kc == 0), stop=(kc == KC - 1)),
                                 reads=[('wb', slots[0])] + (h2rd if kc == 0 else []), writes=[('ps', bg)])
                        for kc in range(KC):
                            S.op('pe', lambda e, kc=kc, bu=bu, s=slots[1], t0=t0: e.matmul(ps[:, bu, :], lhsT=wb[:, s, kc, :], rhs=hT2[:, kc, t0:t0 + 512], start=(kc == 0), stop=(kc == KC - 1)),
                                 reads=[('wb', slots[1])], writes=[('ps', bu)])
                        S.op('act', lambda e, bg=bg, tt=tt: e.activation(out=sg[:, tt, :], in_=ps[:, bg, :], func=AF.Silu), reads=[('ps', bg)], writes=[('sg', tt)])
                        S.op('dve', lambda e, bu=bu, tt=tt, ai=ai, t0=t0: e.tensor_tensor(out=ao[:, ai, t0:t0 + 512], in0=sg[:, tt, :], in1=ps[:, bu, :], op=ALU.mult),
                             reads=[('sg', tt), ('ps', bu)], writes=[('ao', ai)])
                    S.op('sp', lambda e, ft=ft, ai=ai: e.dma_start(out=act_d[ft], in_=ao[:, ai, :]), reads=[('ao', ai)], dma='ao%d' % ai)
                S.flush()
        with ExitStack() as st:
            aT = sbt(st, "aT", [128, FKC, 512], BF16)
            wb = sbt(st, "wb", [128, 2, FKC, 128], BF16)
            po = sbt(st, "po", [128, 2, 512], F32)
            sqp = sbt(st, "sqp", [128, 2, 512], BF16)
            ok = [0]
            wk_ = [0]
            for tt in range(2):
                t0 = tt * 512
                S.op('sp', lambda e, t0=t0: e.dma_start(out=aT[:, :43, :], in_=act_d[:43, :, t0:t0 + 512].rearrange("f p t -> p f t")), writes=['aT'], dma='aT')
                S.op('sp', lambda e, t0=t0: e.dma_start(out=aT[:, 43:, :], in_=act_d[43:, :, t0:t0 + 512].rearrange("f p t -> p f t")), writes=['aT'], dma='aT')
                for ct in range(KC):
                    s = wk_[0] % 2
                    wk_[0] += 1
                    src = w_down[:, ct * 128:(ct + 1) * 128].rearrange("(k p) c -> p k c", p=128)
                    S.op('pool', lambda e, s=s, src=src: e.dma_start(out=wb[:, s, :43, :], in_=src[:, :43, :]), writes=[('wb', s)], dma='wb%d' % s)
                    S.op('pool', lambda e, s=s, src=src: e.dma_start(out=wb[:, s, 43:, :], in_=src[:, 43:, :]), writes=[('wb', s)], dma='wb%d' % s)
                    b = nb()
                    for kc in range(FKC):
                        S.op('pe', lambda e, kc=kc, s=s, b=b: e.matmul(ps[:, b, :], lhsT=wb[:, s, kc, :], rhs=aT[:, kc, :], start=(kc == 0), stop=(kc == FKC - 1)),
                             reads=[('wb', s), 'aT'], writes=[('ps', b)])
                    pre_store((po, sqp), b, ct, tt, ok)
            S.flush()
        if True:
            with ExitStack() as st:
                epilogue(st, (6, 7), G_FFNPOST, lambda c, t0: x1_d[c, :, t0:t0 + 512], x2_d, G_PLEPRE, True)
                S.flush()
            with ExitStack() as st:
                hT3 = sbt(st, "hT3", [128, KC, T], BF16)
                load_h(hT3)
                wb = sbt(st, "wb", [128, 2, KC, 256], BF16)
                wp = sbt(st, "wp", [128, 2, 2, 256], BF16)
                pb = sbt(st, "pb", [128, 2, T], BF16)
                sg = sbt(st, "sg", [128, 2, 512], F32)
                pl = sbt(st, "pl", [128, 2, 512], F32)
                po = sbt(st, "po", [128, 2, 512], F32)
                sqp = sbt(st, "sqp", [128, 2, 512], BF16)
                S.op('pool', lambda e: e.dma_start(out=pb[:], in_=pT.rearrange("(k p) t -> p k t", p=128)), writes=['pb'], dma='c')
                h3rd = [('hN', c, tt) for c in range(KC) for tt in range(2)]
                ok = [0]
                k = [0]
                for w0 in range(0, D, 256):
                    s = load_w(wb, w_pg, 0, KC, w0, 256)
                    S.op('pool', lambda e, s=s, w0=w0: e.dma_start(out=wp[:, s, :, :], in_=w_ple[:, w0:w0 + 256].rearrange("(k p) c -> p k c", p=128)),
                         writes=[('wp', s)], dma='wp%d' % s)
                    for cs_ in range(2):
                        ct = w0 // 128 + cs_
                        for tt in range(2):
                            t0 = tt * 512
                            bg = nb(); bp = nb()
                            mm_fm(wb, s, KC, cs_, lambda kc, t0=t0: hT3[:, kc, t0:t0 + 512], 512, bg, h3rd)
                            for kc in range(2):
                                S.op('pe', lambda e, kc=kc, s=s, cs_=cs_, bp=bp, t0=t0: e.matmul(ps[:, bp, :], lhsT=wp[:, s, kc, cs_ * 128:(cs_ + 1) * 128], rhs=pb[:, kc, t0:t0 + 512],
                                                                                         start=(kc == 0), stop=(kc == 1)),
                                     reads=[('wp', s), 'pb'], writes=[('ps', bp)])
                            o = k[0] % 2
                            k[0] += 1
                            S.op('act', lambda e, bg=bg, o=o: e.activation(out=sg[:, o, :], in_=ps[:, bg, :], func=AF.Sigmoid), reads=[('ps', bg)], writes=[('sg', o)])
                            S.op('dve', lambda e, bp=bp, o=o: e.tensor_tensor(out=pl[:, o, :], in0=sg[:, o, :], in1=ps[:, bp, :], op=ALU.mult),
                                 reads=[('sg', o), ('ps', bp)], writes=[('pl', o)])
                            o2 = ok[0] % 2
                            ok[0] += 1
                            S.op('act', lambda e, o=o, o2=o2: e.activation(out=sqp[:, o2, :], in_=pl[:, o, :], func=AF.Square), reads=[('pl', o)], writes=[('sqp', o2)])
                            S.op('pe', lambda e, o2=o2, ct=ct, tt=tt: e.matmul(ps[:, 6 + tt, :], lhsT=ones, rhs=sqp[:, o2, :], start=(ct == 0), stop=(ct == KC - 1)),
                                 reads=[('sqp', o2), 'cst'], writes=[('ps', 6 + tt)])
                            S.op('sp', lambda e, o=o, ct=ct, tt=tt: e.dma_start(out=pre_d[ct, :, tt * 512:(tt + 1) * 512], in_=pl[:, o, :]), reads=[('pl', o)], dma='po%d' % o)
                S.flush()
        with ExitStack() as st:
            epilogue(st, (6, 7), G_PLEPOST, lambda c, t0: x2_d[c, :, t0:t0 + 512], None, None, None, final_out=outT)
            S.flush()
    return nc


def _bias(valid, reps=4):
    b = np.where(valid, 0.0, NEG).astype(np.float32)
    return np.tile(b, (1, reps)).astype(ml_dtypes.bfloat16)


def _host_consts(core):
    bf = ml_dtypes.bfloat16
    start = core * T
    c = {}
    half = 64
    inv = (10000.0 ** (-np.arange(half, dtype=np.float32) / half)).astype(np.float32)

    def cs(pos):
        ang = pos.astype(np.float32)[None, :] * np.concatenate([inv, inv])[:, None]
        co = np.cos(ang).astype(np.float32)
        si = np.sin(ang).astype(np.float32)
        si[:half] *= -1.0
        return np.stack([co, si], axis=1).astype(np.float32)
    c["cs_full"] = cs(np.arange(S_ALL))
    c["cs_ext"] = cs(np.arange(start - HALO, start + T))
    ident = np.eye(128, dtype=np.float32)
    swp = np.zeros((128, 128), np.float32)
    for m in range(128):
        swp[(m + 64) % 128, m] = 1.0
    c["consts"] = np.stack([ident, swp, np.ones((128, 128), np.float32)], axis=1).astype(bf)
    keys = np.arange(S_ALL)
    c["xsel"] = (keys[None, :] // 64 == np.arange(128)[:, None]).astype(np.float32).astype(bf)
    n = np.arange(512)
    m = np.arange(128)
    ov = np.clip(np.minimum(n[:, None] * 16 + 32, m[None, :] * 64 + 64) - np.maximum(n[:, None] * 16, m[None, :] * 64), 0, None) / 32.0
    ov[511] = 0.0
    c["ovm"] = ov.reshape(4, 128, 128).transpose(1, 0, 2).astype(np.float32).astype(bf)
    gs = np.zeros((48, 48, 128), np.float32)
    for k in range(48):
        gs[k, k, :] = 1.0
    c["gsel"] = gs.astype(bf)
    bc = np.zeros((8, 128, 4, 512), bf)
    af = np.zeros((8, 128, 128), np.float32)
    bw = np.zeros((8, 128, 5, 512), bf)
    for qb in range(8):
        pos = start + qb * 128 + np.arange(128)
        for j in range(4):
            nn = j * 128 + np.arange(128)
            valid = (nn[:, None] * 16 + 31 <= pos[None, :]) & (nn[:, None] < 511)
            bc[qb, :, j, :] = _bias(valid)
        cur = pos // 64
        sblk = np.arange(128)
        forced = (sblk[None] == 0) | (sblk[None] == cur[:, None]) | (sblk[None] == cur[:, None] - 1)
        sval = sblk[None] * 64 <= pos[:, None]
        af[qb] = np.where(sval, 1000.0 * forced, -1e30).astype(np.float32)
        for i in range(5):
            kp = start + (qb - 4 + i) * 128 + np.arange(128)
            valid = (kp[:, None] <= pos[None, :]) & (pos[None, :] - kp[:, None] < 512) & (kp[:, None] >= 0)
            bw[qb, :, i, :] = _bias(valid)
    c["biasC"] = bc
    c["addF"] = af
    c["biasW"] = bw
    dg = np.zeros((128, 8, 512), bf)
    kk = np.arange(128)
    dg[:, core, :] = _bias(kk[:, None] <= kk[None, :])
    c["diagb"] = dg
    bd = np.zeros((128, 22, 512), bf)

    def dil(r, nq, a0, ks, nk):
        qpos = start + r * (a0 + np.arange(nq))
        kpos = start + r * (ks + np.arange(nk))
        valid = (kpos[:, None] >= 0) & (kpos[:, None] <= qpos[None, :]) & (qpos[None, :] - kpos[:, None] <= 128 * r)
        t = np.full((128, 4 * nq), NEG, np.float32)
        t[:nk] = np.tile(np.where(valid, 0.0, NEG), (1, 4))
        out = np.zeros((128, 512), np.float32)
        out[:, :4 * nq] = t
        return out.astype(bf)
    for blk in range(8):
        bd[:, blk * 2] = dil(1, 128, blk * 128, blk * 128 - 128, 128)
        bd[:, blk * 2 + 1] = dil(1, 128, blk * 128, blk * 128, 128)
    for blk in range(2):
        bd[:, 16 + blk * 2] = dil(4, 128, blk * 128, blk * 128 - 128, 128)
        bd[:, 16 + blk * 2 + 1] = dil(4, 128, blk * 128, blk * 128, 128)
    bd[:, 20] = dil(16, 64, 0, -128, 128)
    bd[:, 21] = dil(16, 64, 0, 0, 64)
    c["biasD"] = bd
    return c


_NC_CACHE = {}


def kernel(**inputs):
    f = lambda k: np.asarray(inputs[k], dtype=np.float32)
    x = f("x")[0]
    xT = np.ascontiguousarray(x.T)
    p = f("p")[0, 0]
    gl = lambda v: np.ascontiguousarray(v.reshape(32, 128).T)
    gains = np.concatenate([gl(f(k)[0]) for k in ("g_mix_pre", "g_mix_post", "g_ffn_pre", "g_ffn_post", "g_ple_pre", "g_ple_post")]
                           + [np.zeros((128, 64), np.float32)], axis=1)
    shared = {
        "xT_full": xT, "gains": np.ascontiguousarray(gains), "w_in": f("w_in")[0],
        "peT_k": np.ascontiguousarray(f("pe_ck")[0].T), "peT_v": np.ascontiguousarray(f("pe_cv")[0].T),
        "w_ck1": f("w_ck1")[0], "w_ck2": f("w_ck2")[0], "w_cv1": f("w_cv1")[0], "w_cv2": f("w_cv2")[0],
        "w_a": f("w_a")[0], "w_b": f("w_b")[0], "w_out": f("w_out")[0], "w_gu": f("w_gu")[0],
        "w_down": f("w_down")[0], "w_pg": f("w_ple_gate")[0], "w_ple": f("w_ple")[0],
    }
    in_maps = []
    for c in range(NCORES):
        start = c * T
        ext = np.zeros((D, EXT), np.float32)
        lo = max(0, start - HALO)
        ext[:, EXT - (start + T - lo):] = xT[:, lo:start + T]
        m = dict(shared)
        m["xT_ext"] = ext
        m["pT"] = np.ascontiguousarray(p[start:start + T].T)
        m.update(_host_consts(c))
        in_maps.append(m)
    if "nc" not in _NC_CACHE:
        _NC_CACHE["nc"] = build()
    _ncr = int(os.environ.get("MK_NCORES", NCORES))
    _c0 = int(os.environ.get("MK_CORE0", 0))
    if _ncr != NCORES:
        res = run_bass_kernel_spmd(_NC_CACHE["nc"], in_maps[_c0:_c0 + _ncr], core_ids=list(range(_ncr)))
        _NC_CACHE["res"] = res
        return None
    res = run_bass_kernel_spmd(_NC_CACHE["nc"], in_maps, core_ids=list(range(NCORES)))
    if _DBG:
        _NC_CACHE["res"] = res
    out = np.concatenate([np.asarray(r["outT"]).T for r in res.results], axis=0)
    return out.reshape(1, S_ALL, D).astype(np.float32)
```

```python
import numpy as np
import ml_dtypes
from contextlib import ExitStack
import concourse.bass as bass
import concourse.mybir as mybir
from concourse.bass_utils import run_bass_kernel_spmd

F32 = mybir.dt.float32
BF16 = mybir.dt.bfloat16
AF = mybir.ActivationFunctionType
ALU = mybir.AluOpType

NCORES = 8
S_ALL = 8192
D = 4096
KC = 32
T = 1024
HALO = 2048
EXT = HALO + T
DFF = 11008
FKC = 86
SCALE = 128 ** -0.5
EPS = 1e-6
NEG = -30000.0
C_QA, C_KA, C_VA, C_QB, C_KC, C_VC, C_KSL, C_VSL, C_KWN, C_VWN, C_GN, C_GM = (
    0, 1536, 3072, 4608, 6656, 7168, 7680, 8192, 8704, 9216, 9728, 9776)

ENGS = ("pe", "act", "dve", "pool", "sp")


import os
_STOP = int(os.environ.get("MK_STOP", "1000"))
_DBG = [x for x in os.environ.get("MK_DBG", "").split(",") if x]


class StopBuild(Exception):
    pass


class Sched:
    def __init__(self, nc, st):
        self.nc = nc
        self.esem = {e: st.enter_context(nc.semaphore("s_" + e)) for e in ENGS}
        self.dsem = {}
        self.st = st
        self.seq = {e: 0 for e in ENGS}
        self.nsig = {e: 0 for e in ENGS}
        self.known = {e: {} for e in ENGS}
        self.dcount = {}
        self._reset()

    def _reset(self):
        self.ops = {e: [] for e in ENGS}
        self.lastw = {}
        self.readers = {}
        self.signal = {e: set() for e in ENGS}
        self.dma_issuer = {}

    def _need(self, eng, seq, tok, waits):
        kind, src, val = tok
        if kind == 'E':
            if src == eng:
                if eng == 'pe' or seq - val > 2:
                    return
            k = ('E', src)
        else:
            k = ('D', src)
        if self.known[eng].get(k, -1) >= val:
            return
        if val > waits.get(k, -1):
            waits[k] = val

    def op(self, eng, fn, reads=(), writes=(), dma=None):
        if getattr(self, 'stopped', False):
            return
        seq = self.seq[eng]
        self.seq[eng] += 1
        waits = {}
        for r in reads:
            t = self.lastw.get(r)
            if t is not None:
                self._need(eng, seq, t, waits)
        for w in writes:
            t = self.lastw.get(w)
            if t is not None:
                self._need(eng, seq, t, waits)
            for t in self.readers.get(w, ()):
                self._need(eng, seq, t, waits)
        for k, v in waits.items():
            self.known[eng][k] = v
            if k[0] == 'E':
                self.signal[k[1]].add(v)
        if dma is not None:
            if dma not in self.dsem:
                self.dsem[dma] = self.st.enter_context(self.nc.semaphore("d_" + str(dma)))
            c = self.dcount.get(dma, 0) + 1
            self.dcount[dma] = c
            tok = ('D', dma, c)
            self.dma_issuer.setdefault(eng, {})[dma] = c
        else:
            tok = ('E', eng, seq)
        for r in reads:
            self.readers.setdefault(r, []).append(tok)
        for w in writes:
            self.lastw[w] = tok
            self.readers[w] = []
        self.ops[eng].append((seq, fn, waits, dma))

    def flush(self):
        nc = self.nc
        if getattr(self, 'stopped', False):
            self._reset()
            return
        last = {}
        for e in ENGS:
            cs = [o[0] for o in self.ops[e] if o[3] is None and o[1] is not None]
            if cs:
                last[e] = cs[-1]
                self.signal[e].add(cs[-1])
        for e in ENGS:
            waits = {}
            for e2, s in last.items():
                if e2 != e and self.known[e].get(('E', e2), -1) < s:
                    waits[('E', e2)] = s
                    self.known[e][('E', e2)] = s
            for c, v in self.dcount.items():
                if self.known[e].get(('D', c), -1) < v:
                    waits[('D', c)] = v
                    self.known[e][('D', c)] = v
            self.ops[e].append((None, None, waits, None))
        rank = {}
        for e in ENGS:
            srt = sorted(self.signal[e])
            rank[e] = {s: self.nsig[e] + i + 1 for i, s in enumerate(srt)}
            self.nsig[e] += len(srt)
        esem, dsem = self.esem, self.dsem

        def run(e, engobj):
            sig = self.signal[e]
            for (seq, fn, waits, dma) in self.ops[e]:
                for k, v in waits.items():
                    if k[0] == 'E':
                        engobj.wait_ge(esem[k[1]], rank[k[1]][v])
                    else:
                        engobj.wait_ge(dsem[k[1]], 16 * v)
                if fn is None:
                    continue
                ins = fn(engobj)
                if dma is not None:
                    ins.then_inc(dsem[dma], 16)
                elif seq in sig:
                    ins.then_inc(esem[e], 1)

        with nc.Block() as block:
            @block.tensor
            def _(eng):
                run('pe', eng)

            @block.scalar
            def _(eng):
                run('act', eng)

            @block.vector
            def _(eng):
                run('dve', eng)

            @block.gpsimd
            def _(eng):
                run('pool', eng)

            @block.sync
            def _(eng):
                run('sp', eng)
        self._reset()
        self.nflush = getattr(self, 'nflush', 0) + 1
        if self.nflush >= _STOP:
            self.stopped = True


def build():
    nc = bass.Bass("TRN2", target_bir_lowering=False)

    def din(name, shape, dt=F32):
        return nc.dram_tensor(name, list(shape), dt, kind="ExternalInput").ap()

    def dscr(name, shape, dt):
        return nc.dram_tensor(name, list(shape), dt, kind=("ExternalOutput" if name in _DBG else "Internal")).ap()

    xT_full = din("xT_full", [D, S_ALL])
    xT_ext = din("xT_ext", [D, EXT])
    pT = din("pT", [256, T])
    gains = din("gains", [128, 8 * 32])
    w_in = din("w_in", [D, 17968])
    peT_k = din("peT_k", [128, 32]); peT_v = din("peT_v", [128, 32])
    w_ck1 = din("w_ck1", [4096, 128]); w_ck2 = din("w_ck2", [128, 128])
    w_cv1 = din("w_cv1", [4096, 128]); w_cv2 = din("w_cv2", [128, 128])
    w_a = din("w_a", [512, D]); w_b = din("w_b", [2048, D]); w_out = din("w_out", [D, D])
    w_gu = din("w_gu", [D, 2 * DFF]); w_down = din("w_down", [DFF, D])
    w_pg = din("w_pg", [D, D]); w_ple = din("w_ple", [256, D])
    cs_full = din("cs_full", [128, 2, S_ALL])
    cs_ext = din("cs_ext", [128, 2, EXT])
    consts = din("consts", [128, 3, 128], BF16)
    xsel = din("xsel", [128, S_ALL], BF16)
    ovm = din("ovm", [128, 4, 128], BF16)
    gsel = din("gsel", [48, 48, 128], BF16)
    biasC = din("biasC", [8, 128, 4, 512], BF16)
    addF = din("addF", [8, 128, 128])
    diagb = din("diagb", [128, 8, 512], BF16)
    biasW = din("biasW", [8, 128, 5, 512], BF16)
    biasD = din("biasD", [128, 22, 512], BF16)
    outT = nc.dram_tensor("outT", [D, T], F32, kind="ExternalOutput").ap()

    kcT_d = dscr("kcT_d", [8, 128, S_ALL], BF16)
    kslT_d = dscr("kslT_d", [4, 128, S_ALL], BF16)
    vsl_d = dscr("vsl_d", [S_ALL, 512], BF16)
    kaT_d = dscr("kaT_d", [12, 128, EXT], BF16)
    va_d = dscr("va_d", [EXT, 1536], BF16)
    kwnT_d = dscr("kwnT_d", [4, 128, EXT], BF16)
    vwn_d = dscr("vwn_d", [EXT, 512], BF16)
    gmix_d = dscr("gmix_d", [64, 128, T], BF16)
    pre_d = dscr("pre_d", [32, 128, T], F32)
    x1_d = dscr("x1_d", [32, 128, T], F32)
    x2_d = dscr("x2_d", [32, 128, T], F32)
    act_d = dscr("act_d", [FKC, 128, T], BF16)
    qaT_d = dscr("qaT_d", [12, 128, T], BF16)
    qrT_d = dscr("qrT_d", [16, 128, T], BF16)
    qoT_d = dscr("qoT_d", [16, 128, T], BF16)
    h_d = dscr("h_d", [32, 128, T], BF16)
    ya_d = dscr("ya_d", [4, 128, T], BF16)
    yb_d = dscr("yb_d", [16, 128, T], BF16)

    with ExitStack() as top:
      try:
        S = Sched(nc, top)
        _build_body(nc, top, S, locals())
      except StopBuild:
        pass
    return nc


def _build_body(nc, top, S, L):
    globals().update({k: v for k, v in L.items() if k not in ('nc', 'top', 'S')})
    if True:
        _uid = [0]

        def sbt(st, name, shape, dt):
            _uid[0] += 1
            return st.enter_context(nc.sbuf_tensor("%s_%d" % (name, _uid[0]), list(shape), dt))
        ps = top.enter_context(nc.psum_tensor("ps", [128, 8, 512], F32))
        cst = sbt(top, "cst", [128, 3, 128], BF16)
        ident, swp, ones = cst[:, 0, :], cst[:, 1, :], cst[:, 2, :]
        gn = sbt(top, "gn", [128, 8 * 32], F32)
        kc_c = sbt(top, "kc_c", [128, 4, 512], BF16)
        vc_c = sbt(top, "vc_c", [128, 4, 4, 128], BF16)
        gsT = sbt(top, "gsT", [48, T], BF16)
        rs2p = sbt(top, "rs2p", [128, 2, 512], F32)

        S.op('sp', lambda e: e.dma_start(out=cst[:], in_=consts), writes=['cst'], dma='c')
        S.op('sp', lambda e: e.dma_start(out=gn[:], in_=gains), writes=['cst'], dma='c')
        epsb = sbt(top, "epsb", [128, 1], F32)
        S.op('pool', lambda e: e.memset(epsb[:], EPS), writes=['epsb'])
        S.op('pool', lambda e: e.memset(vc_c[:], 0.0), writes=['vc_c'])
        S.op('pool', lambda e: e.memset(kc_c[:], 0.0), writes=['kc_c'])
        G_MIXPRE, G_MIXPOST, G_FFNPRE, G_FFNPOST, G_PLEPRE, G_PLEPOST = range(6)

        def gcol(gi, c):
            return gn[:, gi * 32 + c: gi * 32 + c + 1]

        bank_rr = [0]

        def nb(n=4, base=0):
            b = base + bank_rr[0] % n
            bank_rr[0] += 1
            return b

        xT_full_v = xT_full.rearrange("(c p) t -> p c t", p=128)
        xT_ext_v = xT_ext.rearrange("(c p) t -> p c t", p=128)

        def norm_tile(st_, xs, hT, src_v, t0, gi, sq, rs):
            for q in range(4):
                S.op('sp', lambda e, q=q: e.dma_start(out=xs[:, 8 * q:8 * q + 8, :], in_=src_v[:, 8 * q:8 * q + 8, t0:t0 + 512]),
                     writes=[('xs', q)], dma='xs%d' % q)
            b = 7
            for c in range(KC):
                S.op('act', lambda e, c=c: e.activation(out=sq[:, c % 2, :], in_=xs[:, c, :], func=AF.Square),
                     reads=[('xs', c // 8)], writes=[('sq', c % 2)])
                S.op('pe', lambda e, c=c: e.matmul(ps[:, b, :], lhsT=ones, rhs=sq[:, c % 2, :], start=(c == 0), stop=(c == KC - 1)),
                     reads=[('sq', c % 2), 'cst'], writes=[('ps', b)])
            S.op('act', lambda e: e.activation(out=rs[:], in_=ps[:, b, :], func=AF.Sqrt, scale=1.0 / D, bias=epsb[:]),
                 reads=[('ps', b), 'epsb'], writes=['rs'])
            S.op('dve', lambda e: e.reciprocal(out=rs[:], in_=rs[:]), reads=['rs'], writes=['rs'])
            for c in range(KC):
                S.op('dve', lambda e, c=c: e.scalar_tensor_tensor(out=hT[:, c, :], in0=xs[:, c, :], scalar=gcol(gi, c), in1=rs[:], op0=ALU.mult, op1=ALU.mult),
                     reads=[('xs', c // 8), 'rs', 'cst'], writes=[('hT', id(hT), c)])

        wslot = [0]

        def load_w(wb, w_ap, r0, kc_n, c0, ncols):
            s = wslot[0] % 2
            wslot[0] += 1
            src = w_ap[r0:r0 + kc_n * 128, c0:c0 + ncols].rearrange("(k p) c -> p k c", p=128)
            half = max(1, kc_n // 2)
            for h0 in range(0, kc_n, half):
                h1 = min(kc_n, h0 + half)
                S.op('pool', lambda e, s=s, h0=h0, h1=h1: e.dma_start(out=wb[:, s, h0:h1, :ncols], in_=src[:, h0:h1, :]),
                     writes=[('wb', s)], dma='wb%d' % s)
            return s

        def mm_fm(wb, s, kc_n, csub, act_fn, n, b, extra_reads=()):
            for kc in range(kc_n):
                S.op('pe', lambda e, kc=kc: e.matmul(ps[:, b, :n], lhsT=wb[:, s, kc, csub * 128:(csub + 1) * 128], rhs=act_fn(kc),
                                                    start=(kc == 0), stop=(kc == kc_n - 1)),
                     reads=[('wb', s)] + list(extra_reads), writes=[('ps', b)])

        def rope_store(b, n, cs_t, t_off, dst_fn, tmpq, tmp1, key, extra_reads=()):
            i = key % 2
            S.op('dve', lambda e: e.tensor_copy(out=tmpq[:, i, :n], in_=ps[:, b, :n]), reads=[('ps', b)] + list(extra_reads), writes=[('tq', i)])
            b2 = nb()
            S.op('pe', lambda e: e.matmul(ps[:, b2, :n], lhsT=swp, rhs=tmpq[:, i, :n], start=True, stop=True),
                 reads=[('tq', i), 'cst'], writes=[('ps', b2)])
            S.op('dve', lambda e: e.tensor_tensor(out=tmp1[:, i, :n], in0=ps[:, b, :n], in1=cs_t[:, 0, t_off:t_off + n], op=ALU.mult),
                 reads=[('ps', b), 'cs'] + list(extra_reads), writes=[('t1', i)])
            S.op('dve', lambda e: e.tensor_tensor(out=tmp1[:, 2 + i, :n], in0=ps[:, b2, :n], in1=cs_t[:, 1, t_off:t_off + n], op=ALU.mult),
                 reads=[('ps', b2), 'cs'] + list(extra_reads), writes=[('t2', i)])
            dst, wkeys = dst_fn()
            S.op('dve', lambda e: e.tensor_tensor(out=dst, in0=tmp1[:, i, :n], in1=tmp1[:, 2 + i, :n], op=ALU.add),
                 reads=[('t1', i), ('t2', i)], writes=wkeys)

        for pss in range(2):
            with ExitStack() as st:
                wres = sbt(st, "wres", [128, KC, 1024], BF16)
                xsb = sbt(st, "xsb", [128, KC, 512], BF16)
                hT2 = [sbt(st, "hTa", [128, KC, 512], BF16), sbt(st, "hTb", [128, KC, 512], BF16)]
                sq = sbt(st, "sq", [128, 2, 512], BF16)
                rs = sbt(st, "rs", [128, 512], F32)
                ob = sbt(st, "ob", [128, 4, 512], BF16)
                tmpq = sbt(st, "tmpq", [128, 2, 512], BF16)
                tmp1 = sbt(st, "tmp1", [128, 4, 512], F32)
                cs_t = sbt(st, "cs_t", [128, 2, 2, 512], F32)
                c0 = C_KC if pss == 0 else C_KSL
                srcw = w_in[:, c0:c0 + 1024].rearrange("(k p) c -> p k c", p=128)
                for q in range(4):
                    S.op('pool', lambda e, q=q: e.dma_start(out=wres[:, 8 * q:8 * q + 8, :], in_=srcw[:, 8 * q:8 * q + 8, :]),
                         writes=['wres'], dma='wres')
                NT = S_ALL // 512

                def normA(tt):
                    t0 = tt * 512
                    for q in range(4):
                        S.op('pool', lambda e, q=q: e.dma_start(out=xsb[:, 8 * q:8 * q + 8, :], in_=xT_full_v[:, 8 * q:8 * q + 8, t0:t0 + 512]),
                             writes=[('xs', q)], dma='xs%d' % q)
                    for c in range(KC):
                        S.op('act', lambda e, c=c: e.activation(out=sq[:, c % 2, :], in_=xsb[:, c, :], func=AF.Square),
                             reads=[('xs', c // 8)], writes=[('sq', c % 2)])
                        S.op('pe', lambda e, c=c: e.matmul(ps[:, 7, :], lhsT=ones, rhs=sq[:, c % 2, :], start=(c == 0), stop=(c == KC - 1)),
                             reads=[('sq', c % 2), 'cst'], writes=[('ps', 7)])
                    S.op('act', lambda e: e.activation(out=rs[:], in_=ps[:, 7, :], func=AF.Sqrt, scale=1.0 / D, bias=epsb[:]),
                         reads=[('ps', 7), 'epsb'], writes=['rs'])
                    S.op('dve', lambda e: e.reciprocal(out=rs[:], in_=rs[:]), reads=['rs'], writes=['rs'])

                def normB(tt):
                    h = hT2[tt % 2]
                    for c in range(KC):
                        S.op('dve', lambda e, c=c: e.scalar_tensor_tensor(out=h[:, c, :], in0=xsb[:, c, :], scalar=gcol(G_MIXPRE, c), in1=rs[:], op0=ALU.mult, op1=ALU.mult),
                             reads=[('xs', c // 8), 'rs', 'cst'], writes=[('hT', tt % 2, c)])

                def unit_fm(tt, ct):
                    hT = hT2[tt % 2]
                    hreads = [('hT', tt % 2, c) for c in range(KC)]
                    t0 = tt * 512
                    b = nb()
                    for kc in range(KC):
                        S.op('pe', lambda e, kc=kc: e.matmul(ps[:, b, :], lhsT=wres[:, kc, ct * 128:(ct + 1) * 128], rhs=hT[:, kc, :],
                                                             start=(kc == 0), stop=(kc == KC - 1)),
                             reads=['wres'] + (hreads if kc == 0 else []), writes=[('ps', b)])
                    o = ct % 4
                    if pss == 0:
                        S.op('act', lambda e: e.copy(out=ob[:, o, :], in_=ps[:, b, :]), reads=[('ps', b)], writes=[('ob', o)])
                        S.op('sp', lambda e: e.dma_start(out=kcT_d[ct, :, t0:t0 + 512], in_=ob[:, o, :]), reads=[('ob', o)], dma='ob%d' % o)
                    else:
                        rope_store(b, 512, cs_t[:, tt % 2], 0, lambda: (ob[:, o, :], [('ob', o)]), tmpq, tmp1, ct, [('cs', tt % 2)])
                        S.op('sp', lambda e: e.dma_start(out=kslT_d[ct, :, t0:t0 + 512], in_=ob[:, o, :]), reads=[('ob', o)], dma='ob%d' % o)

                def unit_tm(tt, tk):
                    hT = hT2[tt % 2]
                    hreads = [('hT', tt % 2, c) for c in range(KC)]
                    t0 = tt * 512
                    b = nb()
                    for kc in range(KC):
                        S.op('pe', lambda e, kc=kc: e.matmul(ps[:, b, :], lhsT=hT[:, kc, tk * 128:(tk + 1) * 128], rhs=wres[:, kc, 512:1024],
                                                             start=(kc == 0), stop=(kc == KC - 1)),
                             reads=['wres'] + (hreads if kc == 0 else []), writes=[('ps', b)])
                    S.op('act', lambda e: e.copy(out=ob[:, tk, :], in_=ps[:, b, :]), reads=[('ps', b)], writes=[('ob', tk)])
                    S.op('sp', lambda e: e.dma_start(out=vsl_d[t0 + tk * 128:t0 + (tk + 1) * 128, :], in_=ob[:, tk, :]), reads=[('ob', tk)], dma='ob%d' % tk)

                normA(0)
                normB(0)
                for tt in range(NT):
                    if pss == 1:
                        S.op('sp', lambda e, tt=tt: e.dma_start(out=cs_t[:, tt % 2], in_=cs_full[:, :, tt * 512:(tt + 1) * 512]), writes=[('cs', tt % 2)], dma='cs%d' % (tt % 2))
                        units = [(unit_fm, ct) for ct in range(4)] + [(unit_tm, tk) for tk in range(4)]
                    else:
                        units = [(unit_fm, ct) for ct in range(8)]
                    for fn_, a_ in units[:4]:
                        fn_(tt, a_)
                    if tt + 1 < NT:
                        normA(tt + 1)
                        normB(tt + 1)
                    for fn_, a_ in units[4:]:
                        fn_(tt, a_)
                S.flush()

        with ExitStack() as st:
            kin = sbt(st, "kin", [128, 2, S_ALL], BF16)
            w1 = sbt(st, "w1", [128, 2, 32, 128], BF16)
            w2 = sbt(st, "w2", [128, 2, 128], BF16)
            pe_b = sbt(st, "pe_b", [128, 2, 32], BF16)
            pe2 = sbt(st, "pe2", [128, 2, 32, 2], BF16)
            cb = sbt(st, "cb", [128, 2], F32)
            xg = sbt(st, "xg", [128, 2, 512], F32)
            tg = sbt(st, "tg", [128, 2, 512], F32)
            ge = sbt(st, "ge", [128, 2, 512], BF16)
            for kv, (wa1, wa2, pea) in enumerate(((w_ck1, w_ck2, peT_k), (w_cv1, w_cv2, peT_v))):
                S.op('pool', lambda e, kv=kv, wa1=wa1: e.dma_start(out=w1[:, kv], in_=wa1.rearrange("(l d) h -> d l h", d=128)), writes=['w1'], dma='c')
                S.op('pool', lambda e, kv=kv, wa2=wa2: e.dma_start(out=w2[:, kv], in_=wa2), writes=['w1'], dma='c')
                S.op('pool', lambda e, kv=kv, pea=pea: e.dma_start(out=pe_b[:, kv], in_=pea), writes=['w1'], dma='c')
            for j2 in range(2):
                S.op('dve', lambda e, j2=j2: e.tensor_copy(out=pe2[:, :, :, j2], in_=pe_b[:]), reads=['w1'], writes=[('pe2', j2)])
            for kv in range(2):
                b = nb()
                for l in range(32):
                    S.op('pe', lambda e, l=l, kv=kv, b=b: e.matmul(ps[:, b, 0:2], lhsT=w1[:, kv, l, :], rhs=pe2[:, kv, l, :], start=(l == 0), stop=(l == 31)),
                         reads=['w1', ('pe2', 0), ('pe2', 1)], writes=[('ps', b)])
                S.op('dve', lambda e, kv=kv, b=b: e.tensor_copy(out=cb[:, kv:kv + 1], in_=ps[:, b, 0:1]), reads=[('ps', b)], writes=[('cb', kv)])
            NCMP = 511
            for g in range(4):
                for kv in range(2):
                    i = (g * 2 + kv) % 2
                    S.op('sp', lambda e, g=g, kv=kv, i=i: e.dma_start(out=kin[:, i, :], in_=kcT_d[kv * 4 + g]), writes=[('kin', i)], dma='kin%d' % i)
                    b = nb()
                    for l in range(32):
                        S.op('pe', lambda e, l=l, kv=kv, i=i, b=b: e.matmul(ps[:, b, :NCMP], lhsT=w1[:, kv, l, :], rhs=kin[:, i, l:l + 16 * (NCMP - 1) + 1:16],
                                                                      start=(l == 0), stop=(l == 31)),
                             reads=['w1', ('kin', i)], writes=[('ps', b)])
                    S.op('dve', lambda e, kv=kv, i=i, b=b: e.tensor_scalar(out=xg[:, i, :NCMP], in0=ps[:, b, :NCMP], scalar1=cb[:, kv:kv + 1], scalar2=None, op0=ALU.add),
                         reads=[('ps', b), ('cb', kv)], writes=[('xg', i)])
                    S.op('dve', lambda e, i=i: e.tensor_tensor(out=tg[:, i, :NCMP], in0=xg[:, i, :NCMP], in1=xg[:, i, :NCMP], op=ALU.mult),
                         reads=[('xg', i)], writes=[('tg', i)])
                    S.op('dve', lambda e, i=i: e.tensor_scalar(out=tg[:, i, :NCMP], in0=tg[:, i, :NCMP], scalar1=0.044715, scalar2=1.0, op0=ALU.mult, op1=ALU.add),
                         reads=[('tg', i)], writes=[('tg', i)])
                    S.op('dve', lambda e, i=i: e.tensor_tensor(out=tg[:, i, :NCMP], in0=tg[:, i, :NCMP], in1=xg[:, i, :NCMP], op=ALU.mult),
                         reads=[('tg', i), ('xg', i)], writes=[('tg', i)])
                    S.op('act', lambda e, i=i: e.activation(out=tg[:, i, :NCMP], in_=tg[:, i, :NCMP], func=AF.Sigmoid, scale=1.5957691216057308),
                         reads=[('tg', i)], writes=[('tg', i)])
                    S.op('dve', lambda e, i=i: e.tensor_tensor(out=ge[:, i, :NCMP], in0=tg[:, i, :NCMP], in1=xg[:, i, :NCMP], op=ALU.mult),
                         reads=[('tg', i), ('xg', i)], writes=[('ge', i)])
                    if kv == 0:
                        b2 = nb()
                        S.op('pe', lambda e, i=i, b2=b2: e.matmul(ps[:, b2, :NCMP], lhsT=w2[:, 0, :], rhs=ge[:, i, :NCMP], start=True, stop=True),
                             reads=['w1', ('ge', i)], writes=[('ps', b2)])
                        S.op('act', lambda e, g=g, b2=b2: e.copy(out=kc_c[:, g, :NCMP], in_=ps[:, b2, :NCMP]), reads=[('ps', b2)], writes=['kc_c'])
                    else:
                        for j in range(4):
                            m = 128 if j < 3 else 127
                            b2 = nb()
                            S.op('pe', lambda e, i=i, j=j, m=m, b2=b2: e.matmul(ps[:m, b2, :128], lhsT=ge[:, i, j * 128:j * 128 + m], rhs=w2[:, 1, :], start=True, stop=True),
                                 reads=['w1', ('ge', i)], writes=[('ps', b2)])
                            S.op('act', lambda e, g=g, j=j, m=m, b2=b2: e.copy(out=vc_c[:m, g, j, :], in_=ps[:m, b2, :128]), reads=[('ps', b2)], writes=['vc_c'])
            S.flush()

        for half in range(3):
            with ExitStack() as st:
                hT3 = [sbt(st, "hT3_%d" % i, [128, KC, 512], BF16) for i in range(2)]
                xs = sbt(st, "xs", [128, KC, 512], F32)
                sq = sbt(st, "sq", [128, 2, 512], BF16)
                rs = sbt(st, "rs", [128, 512], F32)
                wb = sbt(st, "wb", [128, 2, KC, 256], BF16)
                ob = sbt(st, "ob", [128, 4, 512], BF16)
                tmpq = sbt(st, "tmpq", [128, 2, 512], BF16)
                tmp1 = sbt(st, "tmp1", [128, 4, 512], F32)
                cs_t = sbt(st, "cs_t", [128, 2, 1024], F32)
                tb = half * 1024
                S.op('sp', lambda e, tb=tb: e.dma_start(out=cs_t[:], in_=cs_ext[:, :, tb:tb + 1024]), writes=['cs'], dma='cs')
                for i in range(2):
                    norm_tile(st, xs, hT3[i], xT_ext_v, tb + i * 512, G_MIXPRE, sq, rs)
                hr = [[('hT', id(hT3[i]), c) for c in range(KC)] for i in range(2)]
                okey = [0]

                def fm_cols(c0, ncols, tiles, sink):
                    for w0 in range(0, ncols, 256):
                        wn = min(256, ncols - w0)
                        s = load_w(wb, w_in, 0, KC, c0 + w0, wn)
                        for i in tiles:
                            for cs_ in range((wn + 127) // 128):
                                mrows = min(128, wn - cs_ * 128)
                                b = nb()
                                for kc in range(KC):
                                    S.op('pe', lambda e, kc=kc, s=s, cs_=cs_, i=i, b=b, mrows=mrows: e.matmul(
                                        ps[:mrows, b, :], lhsT=wb[:, s, kc, cs_ * 128:cs_ * 128 + mrows], rhs=hT3[i][:, kc, :],
                                        start=(kc == 0), stop=(kc == KC - 1)),
                                        reads=[('wb', s)] + (hr[i] if kc == 0 else []), writes=[('ps', b)])
                                sink((w0 + cs_ * 128) // 128, i, b, mrows)

                def tm_cols(c0, ncols, tiles, dst_d, dcol0):
                    for w0 in range(0, ncols, 256):
                        s = load_w(wb, w_in, 0, KC, c0 + w0, 256)
                        for i in tiles:
                            for tk in range(4):
                                b = nb()
                                for kc in range(KC):
                                    S.op('pe', lambda e, kc=kc, s=s, i=i, tk=tk, b=b: e.matmul(
                                        ps[:, b, :256], lhsT=hT3[i][:, kc, tk * 128:(tk + 1) * 128], rhs=wb[:, s, kc, :],
                                        start=(kc == 0), stop=(kc == KC - 1)),
                                        reads=[('wb', s)] + (hr[i] if kc == 0 else []), writes=[('ps', b)])
                                o = okey[0] % 4
                                okey[0] += 1
                                S.op('act', lambda e, b=b, o=o: e.copy(out=ob[:, o, :256], in_=ps[:, b, :256]), reads=[('ps', b)], writes=[('ob', o)])
                                r0 = tb + i * 512 + tk * 128
                                S.op('sp', lambda e, o=o, r0=r0, w0=w0: e.dma_start(out=dst_d[r0:r0 + 128, dcol0 + w0:dcol0 + w0 + 256], in_=ob[:, o, :256]),
                                     reads=[('ob', o)], dma='ob%d' % o)

                def sink_rope_dram(dst_d):
                    def sink(ct, i, b, mrows):
                        o = okey[0] % 4
                        okey[0] += 1
                        rope_store(b, 512, cs_t, i * 512, lambda o=o: (ob[:, o, :], [('ob', o)]), tmpq, tmp1, okey[0])
                        t0 = tb + i * 512
                        S.op('sp', lambda e, ct=ct, o=o, t0=t0: e.dma_start(out=dst_d[ct, :, t0:t0 + 512], in_=ob[:, o, :]),
                             reads=[('ob', o)], dma='ob%d' % o)
                    return sink

                fm_cols(C_KA, 1536, range(2), sink_rope_dram(kaT_d))
                fm_cols(C_KWN, 512, range(2), sink_rope_dram(kwnT_d))
                tm_cols(C_VA, 1536, range(2), va_d, 0)
                tm_cols(C_VWN, 512, range(2), vwn_d, 0)
                if half == 2:
                    own = (0, 1)

                    def sink_q(dst_d, extra=()):
                        def sink(ct, i, b, mrows):
                            t0 = i * 512
                            o = okey[0] % 4
                            okey[0] += 1
                            rope_store(b, 512, cs_t, i * 512, lambda o=o: (ob[:, o, :], [('ob', o)]), tmpq, tmp1, okey[0], extra)
                            S.op('sp', lambda e: e.dma_start(out=dst_d[ct, :, t0:t0 + 512], in_=ob[:, o, :]), reads=[('ob', o)], dma='ob%d' % o)
                        return sink
                    fm_cols(C_QA, 1536, own, sink_q(qaT_d))

                    def sink_qb(ct, i, b, mrows):
                        t0 = i * 512
                        o = okey[0] % 4
                        okey[0] += 1
                        S.op('act', lambda e: e.copy(out=ob[:, o, :], in_=ps[:, b, :]), reads=[('ps', b)], writes=[('ob', o)])
                        S.op('sp', lambda e: e.dma_start(out=qrT_d[ct, :, t0:t0 + 512], in_=ob[:, o, :]), reads=[('ob', o)], dma='ob%d' % o)
                        sink_q(qoT_d, [('ob', o)])(ct, i, b, mrows)
                    fm_cols(C_QB, 2048, own, sink_qb)

                    def sink_gn(ct, i, b, mrows):
                        t0 = i * 512
                        S.op('act', lambda e: e.activation(out=gsT[:, t0:t0 + 512], in_=ps[:48, b, :], func=AF.Sigmoid), reads=[('ps', b)], writes=['gsT'])
                    fm_cols(C_GN, 48, own, sink_gn)

                    def sink_gm(ct, i, b, mrows):
                        t0 = i * 512
                        o = okey[0] % 4
                        okey[0] += 1
                        S.op('act', lambda e: e.activation(out=ob[:, o, :], in_=ps[:, b, :], func=AF.Sigmoid), reads=[('ps', b)], writes=[('ob', o)])
                        S.op('sp', lambda e: e.dma_start(out=gmix_d[ct, :, t0:t0 + 512], in_=ob[:, o, :]), reads=[('ob', o)], dma='ob%d' % o)
                    fm_cols(C_GM, 8192, own, sink_gm)
                S.flush()

        acc_i = [0]

        def attn_chunks(chunks, nq, n_heads_cols, o_bank, d_bank):
            ncols = n_heads_cols
            nchunks = len(chunks)

            def emit_pv(ci, ch, pi):
                nk = ch['nk']
                for h in range(4):
                    v_ap, rd = ch['v_fn'](h)
                    S.op('pe', lambda e, v_ap=v_ap, h=h, nk=nk, pi=pi, ci=ci: e.matmul(
                        ps[:, o_bank, h * nq:(h + 1) * nq], lhsT=v_ap, rhs=PT[:nk, pi, h * nq:(h + 1) * nq],
                        start=(ci == 0 and h == 0), stop=(ci == nchunks - 1)), reads=rd + [('PT', pi)], writes=[('ps', o_bank)])
                S.op('pe', lambda e, nk=nk, pi=pi, ci=ci: e.matmul(ps[:, d_bank, :ncols], lhsT=cst[:nk, 2, :], rhs=PT[:nk, pi, :ncols],
                                                                start=(ci == 0), stop=(ci == nchunks - 1)),
                     reads=[('PT', pi), 'cst'], writes=[('ps', d_bank)])

            pend = None
            for ci, ch in enumerate(chunks):
                nk = ch['nk']
                b = nb()
                ops_ = ch['s_ops']
                for oi, (l_ap, r_ap, o_ap, rd) in enumerate(ops_):
                    S.op('pe', lambda e, l_ap=l_ap, r_ap=r_ap, o_ap=o_ap, b=b, st_=ch['starts'][oi], sp_=ch['stops'][oi]: e.matmul(
                        o_ap(b), lhsT=l_ap, rhs=r_ap, start=st_, stop=sp_), reads=rd, writes=[('ps', b)])
                pi = acc_i[0] % 3
                acc_i[0] += 1
                S.op('act', lambda e, b=b, nk=nk, pi=pi: e.activation(out=PT[:nk, pi, :ncols], in_=ps[:nk, b, :ncols], func=AF.Exp, scale=SCALE),
                     reads=[('ps', b)], writes=[('PT', pi)])
                if ch.get('keep') is not None:
                    ch['keep'](pi, nk)
                if os.environ.get('MK_OLDATTN'):
                    emit_pv(ci, ch, pi)
                    continue
                if pend is not None:
                    emit_pv(*pend)
                pend = (ci, ch, pi)
            if pend is not None:
                emit_pv(*pend)

        with ExitStack() as st:
            PT = sbt(st, "PT", [128, 3, 512], BF16)
            kaS = sbt(st, "kaS", [128, 4, EXT], BF16)
            vS = sbt(st, "vS", [128, 4, 512], BF16)
            bD = sbt(st, "bD", [128, 22, 512], BF16)
            numT = sbt(st, "numT", [128, 4, T], F32)
            denT = sbt(st, "denT", [128, 4, T], F32)
            qaT = sbt(st, "qaT", [128, 4, T], BF16)
            yaT = sbt(st, "yaT", [128, 4, T], BF16)
            S.op('sp', lambda e: e.dma_start(out=bD[:], in_=biasD), writes=['bD'], dma='c')
            vslot = [0]
            for grp, r in enumerate((1, 4, 16)):
                for h in range(4):
                    S.op('sp', lambda e, grp=grp, h=h: e.dma_start(out=kaS[:, h, :], in_=kaT_d[grp * 4 + h]), writes=['kaS'], dma='kaS')
                    S.op('sp', lambda e, grp=grp, h=h: e.dma_start(out=qaT[:, h, :], in_=qaT_d[grp * 4 + h]), writes=['qaT'], dma='qaT')
                nq = 128 if r < 16 else 64
                nblk = (T // r) // nq
                for rho in range(r):
                    for blk in range(nblk):
                        a0 = blk * nq
                        if grp == 0:
                            bidx = [blk * 2, blk * 2 + 1]
                        elif grp == 1:
                            bidx = [16 + blk * 2, 16 + blk * 2 + 1]
                        else:
                            bidx = [20, 21]
                        chunks = []
                        for ci, (ks, nk) in enumerate(((a0 - 128, 128), (a0, nq))):
                            e0 = HALO + r * ks + rho
                            vs = vslot[0] % 4
                            vslot[0] += 1
                            S.op('sp', lambda e, e0=e0, nk=nk, vs=vs, grp=grp, r=r: e.dma_start(
                                out=vS[:nk, vs, :], in_=va_d[e0:e0 + r * (nk - 1) + 1:r, grp * 512:(grp + 1) * 512]),
                                writes=[('vS', vs)], dma='vS%d' % vs)
                            q0 = rho + r * a0
                            s_ops = [(cst[:nk, 0, :nk], bD[:nk, bidx[ci], :4 * nq],
                                      (lambda b, nk=nk, nq=nq: ps[:nk, b, :4 * nq]), ['bD', 'cst'])]
                            for h in range(4):
                                s_ops.append((kaS[:, h, e0:e0 + r * (nk - 1) + 1:r],
                                              qaT[:, h, q0:q0 + r * (nq - 1) + 1:r],
                                              (lambda b, h=h, nk=nk, nq=nq: ps[:nk, b, h * nq:(h + 1) * nq]),
                                              ['kaS', 'qaT']))
                            chunks.append(dict(nk=nk, s_ops=s_ops, starts=[True] + [False] * 4, stops=[False] * 4 + [True],
                                               v_fn=(lambda h, vs=vs, nk=nk: (vS[:nk, vs, h * 128:(h + 1) * 128], [('vS', vs)]))))
                        ob_, db_ = (4, 5) if (acc_i[0] // 2) % 2 == 0 else (6, 7)
                        attn_chunks(chunks, nq, 4 * nq, ob_, db_)
                        q0 = rho + r * a0
                        dstn = numT[:, :, q0:q0 + r * (nq - 1) + 1:r]
                        dstd = denT[:, :, q0:q0 + r * (nq - 1) + 1:r]
                        srcn = ps[:, ob_, :4 * nq].rearrange("p (h q) -> p h q", h=4)
                        srcd = ps[:, db_, :4 * nq].rearrange("p (h q) -> p h q", h=4)
                        if grp == 0:
                            S.op('dve', lambda e, dstn=dstn, srcn=srcn: e.tensor_copy(out=dstn, in_=srcn), reads=[('ps', ob_)], writes=['numT'])
                            S.op('act', lambda e, dstd=dstd, srcd=srcd: e.copy(out=dstd, in_=srcd), reads=[('ps', db_)], writes=['denT'])
                        else:
                            S.op('dve', lambda e, dstn=dstn, srcn=srcn: e.tensor_tensor(out=dstn, in0=dstn, in1=srcn, op=ALU.add), reads=[('ps', ob_), 'numT'], writes=['numT'])
                            S.op('dve', lambda e, dstd=dstd, srcd=srcd: e.tensor_tensor(out=dstd, in0=dstd, in1=srcd, op=ALU.add), reads=[('ps', db_), 'denT'], writes=['denT'])
            S.op('dve', lambda e: e.reciprocal(out=denT[:], in_=denT[:]), reads=['denT'], writes=['denT'])
            S.op('dve', lambda e: e.tensor_tensor(out=yaT[:], in0=numT[:], in1=denT[:], op=ALU.mult), reads=['numT', 'denT'], writes=['yaT'])
            S.op('sp', lambda e: e.dma_start(out=ya_d.rearrange("h p t -> p h t"), in_=yaT[:]), reads=['yaT'], dma='c')
            S.flush()

        with ExitStack() as st:
            PT = sbt(st, "PT", [128, 3, 512], BF16)
            PK = sbt(st, "PK", [128, 4, 512], BF16)
            Ksel = sbt(st, "Ksel", [128, S_ALL], BF16)
            Vsel = sbt(st, "Vsel", [128, 64, 128], BF16)
            Kw = sbt(st, "Kw", [128, 1536], BF16)
            Vw = sbt(st, "Vw", [128, 12, 128], BF16)
            xs_sb = sbt(st, "xs_sb", [128, S_ALL], BF16)
            ov_sb = sbt(st, "ov_sb", [128, 4, 128], BF16)
            gs_sb = sbt(st, "gs_sb", [48, 48, 128], BF16)
            bC = sbt(st, "bC", [128, 4, 512], BF16)
            bW = sbt(st, "bW", [128, 5, 512], BF16)
            dg = sbt(st, "dg", [128, 8, 512], BF16)
            aF = sbt(st, "aF", [128, 128], F32)
            rden = sbt(st, "rden", [128, 512], F32)
            pn = sbt(st, "pn", [128, 2, 512], BF16)
            sc = sbt(st, "sc", [128, 128], F32)
            wk = sbt(st, "wk", [128, 128], F32)
            mx = sbt(st, "mx", [128, 16], F32)
            selb = sbt(st, "selb", [128, 128], BF16)
            selT = sbt(st, "selT", [128, 512], BF16)
            grep = sbt(st, "grep", [128, 512], F32)
            acc = sbt(st, "acc", [128, 512], F32)
            tmpo = sbt(st, "tmpo", [128, 512], F32)
            ybo = sbt(st, "ybo", [128, 512], BF16)
            qrT = sbt(st, "qrT", [128, 4, T], BF16)
            qoT = sbt(st, "qoT", [128, 4, T], BF16)
            S.op('sp', lambda e: e.dma_start(out=xs_sb[:], in_=xsel), writes=['k2'], dma='c')
            S.op('sp', lambda e: e.dma_start(out=ov_sb[:], in_=ovm), writes=['k2'], dma='c')
            S.op('sp', lambda e: e.dma_start(out=gs_sb[:], in_=gsel), writes=['k2'], dma='c')
            S.op('sp', lambda e: e.dma_start(out=dg[:], in_=diagb), writes=['k2'], dma='c')

            def finish_part(part, g, ob_, db_, first, QB0):
                S.op('dve', lambda e: e.tensor_scalar(out=rden[:], in0=ps[:, db_, :], scalar1=1e-30, scalar2=None, op0=ALU.max),
                     reads=[('ps', db_)], writes=['rden'])
                S.op('dve', lambda e: e.reciprocal(out=rden[:], in_=rden[:]), reads=['rden'], writes=['rden'])
                bg = nb()
                for h in range(4):
                    col = (g * 4 + h) * 3 + part
                    S.op('pe', lambda e, h=h, col=col: e.matmul(ps[:, bg, h * 128:(h + 1) * 128], lhsT=gs_sb[:, col, :], rhs=gsT[:, QB0:QB0 + 128], start=(h == 0), stop=True),
                         reads=['k2', 'gsT'], writes=[('ps', bg)])
                S.op('dve', lambda e: e.tensor_tensor(out=grep[:], in0=ps[:, bg, :], in1=rden[:], op=ALU.mult), reads=[('ps', bg), 'rden'], writes=['grep'])
                if first:
                    S.op('dve', lambda e: e.tensor_tensor(out=acc[:], in0=ps[:, ob_, :], in1=grep[:], op=ALU.mult), reads=[('ps', ob_), 'grep'], writes=['acc'])
                else:
                    S.op('dve', lambda e: e.tensor_tensor(out=tmpo[:], in0=ps[:, ob_, :], in1=grep[:], op=ALU.mult), reads=[('ps', ob_), 'grep'], writes=['tmpo'])
                    S.op('dve', lambda e: e.tensor_tensor(out=acc[:], in0=acc[:], in1=tmpo[:], op=ALU.add), reads=['acc', 'tmpo'], writes=['acc'])

            def nsa_block(g, qb):
                QB0 = qb * 128
                S.op('sp', lambda e, qb=qb: e.dma_start(out=bC[:], in_=biasC[qb]), writes=['bC'], dma='bC')
                S.op('sp', lambda e, qb=qb: e.dma_start(out=bW[:], in_=biasW[qb]), writes=['bW'], dma='bW')
                S.op('sp', lambda e, qb=qb: e.dma_start(out=aF[:], in_=addF[qb]), writes=['aF'], dma='aF')
                qr4 = qrT[:, :, QB0:QB0 + 128]
                qo4 = qoT[:, :, QB0:QB0 + 128]
                qrd = ['qrT']
                qod = ['qoT']
                full = lambda b: ps[:, b, :]
                chunks = []
                for j in range(4):
                    def keep(pi, nk, j=j):
                        S.op('pool', lambda e, pi=pi, j=j: e.tensor_copy(out=PK[:, j, :], in_=PT[:, pi, :]), reads=[('PT', pi)], writes=[('PK', j)])
                    chunks.append(dict(nk=128, starts=[True, False], stops=[False, True], keep=keep,
                                       s_ops=[(kc_c[:, g, j * 128:(j + 1) * 128], qr4, full, ['kc_c'] + qrd),
                                              (ident, bC[:, j, :], full, ['bC', 'cst'])],
                                       v_fn=(lambda h, j=j, g=g: (vc_c[:, g, j, :], ['vc_c']))))
                attn_chunks(chunks, 128, 512, 4, 5)
                finish_part(0, g, 4, 5, True, QB0)
                bi = nb()
                for j in range(4):
                    S.op('dve', lambda e, j=j: e.tensor_tensor(out=pn[:, j % 2, :], in0=PK[:, j, :], in1=rden[:], op=ALU.mult),
                         reads=[('PK', j), 'rden'], writes=[('pn', j % 2)])
                    for h in range(4):
                        S.op('pe', lambda e, j=j, h=h: e.matmul(ps[:, bi, :128], lhsT=pn[:, j % 2, h * 128:(h + 1) * 128], rhs=ov_sb[:, j, :],
                                                               start=(j == 0 and h == 0), stop=(j == 3 and h == 3)),
                             reads=[('pn', j % 2), 'k2'], writes=[('ps', bi)])
                S.op('dve', lambda e: e.tensor_tensor(out=sc[:], in0=ps[:, bi, :128], in1=aF[:], op=ALU.add), reads=[('ps', bi), 'aF'], writes=['sc'])
                S.op('dve', lambda e: e.max(out=mx[:, 0:8], in_=sc[:]), reads=['sc'], writes=['mx'])
                S.op('dve', lambda e: e.match_replace(out=wk[:], in_to_replace=mx[:, 0:8], in_values=sc[:], imm_value=-1e30), reads=['sc', 'mx'], writes=['wk'])
                S.op('dve', lambda e: e.max(out=mx[:, 8:16], in_=wk[:]), reads=['wk'], writes=['mx'])
                S.op('dve', lambda e: e.tensor_scalar(out=mx[:, 15:16], in0=mx[:, 15:16], scalar1=-1e29, scalar2=None, op0=ALU.max), reads=['mx'], writes=['mx'])
                S.op('dve', lambda e: e.tensor_scalar(out=selb[:], in0=sc[:], scalar1=mx[:, 15:16], scalar2=NEG, op0=ALU.is_lt, op1=ALU.mult),
                     reads=['sc', 'mx'], writes=['selb'])
                bt = nb()
                S.op('pe', lambda e: e.matmul(ps[:, bt, :128], lhsT=selb[:], rhs=ident, start=True, stop=True), reads=['selb', 'cst'], writes=[('ps', bt)])
                for h in range(4):
                    S.op('dve', (lambda e, h=h: e.tensor_copy(out=selT[:, h * 128:(h + 1) * 128], in_=ps[:, bt, :128])),
                         reads=[('ps', bt)], writes=[('selT', h)])
                selrd = [('selT', h) for h in range(4)]
                chunks = []
                nch = 57 + qb
                for j in range(nch):
                    s_ops = [(Ksel[:, j * 128:(j + 1) * 128], qo4, full, ['Ksel'] + qod),
                             (xs_sb[:, j * 128:(j + 1) * 128], selT[:], full, ['k2'] + selrd)]
                    if j % 8 == qb:
                        s_ops.append((ident, dg[:, j // 8, :], full, ['k2', 'cst']))
                    n = len(s_ops)
                    chunks.append(dict(nk=128, s_ops=s_ops, starts=[True] + [False] * (n - 1), stops=[False] * (n - 1) + [True],
                                       v_fn=(lambda h, j=j: (Vsel[:, j, :], ['Vsel']))))
                attn_chunks(chunks, 128, 512, 6, 7)
                finish_part(1, g, 6, 7, False, QB0)
                chunks = []
                for i in range(5):
                    kj = qb + i
                    chunks.append(dict(nk=128, starts=[True, False], stops=[False, True],
                                       s_ops=[(Kw[:, kj * 128:(kj + 1) * 128], qo4, full, ['Kw'] + qod),
                                              (ident, bW[:, i, :], full, ['bW', 'cst'])],
                                       v_fn=(lambda h, kj=kj: (Vw[:, kj, :], ['Vw']))))
                attn_chunks(chunks, 128, 512, 4, 5)
                finish_part(2, g, 4, 5, False, QB0)
                S.op('act', lambda e: e.copy(out=ybo[:], in_=acc[:]), reads=['acc'], writes=['ybo'])
                S.op('sp', lambda e: e.dma_start(out=yb_d[4 * g:4 * g + 4, :, QB0:QB0 + 128].rearrange("h p q -> p h q"), in_=ybo[:].rearrange("p (h q) -> p h q", h=4)), reads=['ybo'], dma='ybo')

            for g in range(4):
                S.op('sp', lambda e, g=g: e.dma_start(out=Ksel[:], in_=kslT_d[g]), writes=['Ksel'], dma='Ksel')
                for jj in range(8):
                    S.op('sp', lambda e, g=g, jj=jj: e.dma_start(out=Vsel[:, 8 * jj:8 * jj + 8, :], in_=vsl_d[1024 * jj:1024 * jj + 1024, g * 128:(g + 1) * 128].rearrange("(j k) d -> k j d", k=128)), writes=['Vsel'], dma='Vsel')
                S.op('sp', lambda e, g=g: e.dma_start(out=qrT[:], in_=qrT_d[4 * g:4 * g + 4].rearrange("h p t -> p h t")), writes=['qrT'], dma='qrT')
                S.op('sp', lambda e, g=g: e.dma_start(out=qoT[:], in_=qoT_d[4 * g:4 * g + 4].rearrange("h p t -> p h t")), writes=['qoT'], dma='qoT')
                S.op('sp', lambda e, g=g: e.dma_start(out=Kw[:], in_=kwnT_d[g, :, HALO - 512:EXT]), writes=['Kw'], dma='Kw')
                S.op('sp', lambda e, g=g: e.dma_start(out=Vw[:], in_=vwn_d[HALO - 512:EXT, g * 128:(g + 1) * 128].rearrange("(j k) d -> k j d", k=128)), writes=['Vw'], dma='Vw')
                for qb in range(8):
                    nsa_block(g, qb)
            S.flush()

        xo_v = xT_ext_v

        def epilogue(st, sums_banks, g_post, xin_fn, xout_d, g_next, hN, final_out=None):
            rsb = sbt(st, "rsb", [128, 2, 512], F32)
            hb = sbt(st, "hb", [128, 2, 512], BF16)
            pv = sbt(st, "pv", [128, 2, 512], F32)
            xv = sbt(st, "xv", [128, 2, 512], F32)
            sq2 = sbt(st, "sq2", [128, 2, 512], BF16)
            for tt in range(2):
                S.op('act', lambda e, tt=tt: e.activation(out=rsb[:, tt, :], in_=ps[:, sums_banks[tt], :], func=AF.Sqrt, scale=1.0 / D, bias=epsb[:]),
                     reads=[('ps', sums_banks[tt]), 'epsb'], writes=[('rsb', tt)])
                S.op('dve', lambda e, tt=tt: e.reciprocal(out=rsb[:, tt, :], in_=rsb[:, tt, :]), reads=[('rsb', tt)], writes=[('rsb', tt)])
            for tt in range(2):
                t0 = tt * 512
                for c in range(KC):
                    i = c % 2
                    S.op('sp', lambda e, c=c, i=i, t0=t0: e.dma_start(out=pv[:, i, :], in_=pre_d[c, :, t0:t0 + 512]), writes=[('pv', i)], dma='pv%d' % i)
                    S.op('sp', lambda e, c=c, i=i, t0=t0: e.dma_start(out=xv[:, i, :], in_=xin_fn(c, t0)), writes=[('xv', i)], dma='xv%d' % i)
                    S.op('dve', lambda e, c=c, i=i, tt=tt: e.scalar_tensor_tensor(out=pv[:, i, :], in0=pv[:, i, :], scalar=gcol(g_post, c), in1=rsb[:, tt, :], op0=ALU.mult, op1=ALU.mult),
                         reads=[('pv', i), ('rsb', tt), 'cst'], writes=[('pv', i)])
                    S.op('dve', lambda e, i=i: e.tensor_tensor(out=xv[:, i, :], in0=xv[:, i, :], in1=pv[:, i, :], op=ALU.add),
                         reads=[('pv', i), ('xv', i)], writes=[('xv', i)])
                    dst = final_out if final_out is not None else xout_d
                    S.op('sp', lambda e, c=c, i=i, t0=t0, dst=dst: e.dma_start(
                        out=(dst[c * 128:(c + 1) * 128, t0:t0 + 512] if final_out is not None else dst[c, :, t0:t0 + 512]), in_=xv[:, i, :]),
                        reads=[('xv', i)], dma='xo%d' % i)
                    if hN is not None:
                        S.op('act', lambda e, i=i: e.activation(out=sq2[:, i, :], in_=xv[:, i, :], func=AF.Square), reads=[('xv', i)], writes=[('sq2', i)])
                        S.op('pe', lambda e, c=c, i=i, tt=tt: e.matmul(ps[:, sums_banks[tt], :], lhsT=ones, rhs=sq2[:, i, :], start=(c == 0), stop=(c == KC - 1)),
                             reads=[('sq2', i), 'cst'] + ([('rsb', tt)] if c == 0 else []), writes=[('ps', sums_banks[tt])])
                        S.op('act', lambda e, c=c, i=i: e.activation(out=hb[:, i, :], in_=xv[:, i, :], func=AF.Copy, scale=gcol(g_next, c)),
                             reads=[('xv', i), 'cst'], writes=[('hb', i)])
                        S.op('sp', lambda e, c=c, i=i, t0=t0: e.dma_start(out=h_d[c, :, t0:t0 + 512], in_=hb[:, i, :]), reads=[('hb', i)], dma='hb%d' % i)
                if hN is not None:
                    S.op('act', lambda e, tt=tt: e.activation(out=rs2p[:, tt, :], in_=ps[:, sums_banks[tt], :], func=AF.Sqrt, scale=1.0 / D, bias=epsb[:]),
                         reads=[('ps', sums_banks[tt]), 'epsb'], writes=[('rs2', tt)])
                    S.op('dve', lambda e, tt=tt: e.reciprocal(out=rs2p[:, tt, :], in_=rs2p[:, tt, :]), reads=[('rs2', tt)], writes=[('rs2', tt)])

        def load_h(hT_):
            for q in range(4):
                S.op('sp', lambda e, q=q: e.dma_start(out=hT_[:, 8 * q:8 * q + 8, :], in_=h_d[8 * q:8 * q + 8].rearrange("c p t -> p c t")), writes=[('hld', q)], dma='hld')
            for c in range(KC):
                for tt in range(2):
                    S.op('dve' if (c + tt) % 2 else 'pool', lambda e, c=c, tt=tt: e.tensor_tensor(out=hT_[:, c, tt * 512:(tt + 1) * 512], in0=hT_[:, c, tt * 512:(tt + 1) * 512], in1=rs2p[:, tt, :], op=ALU.mult),
                         reads=[('hld', c // 8)], writes=[('hN', c, tt)])

        def pre_store(st_bufs, b, ct, tt, okey):
            po, sqp = st_bufs
            o = okey[0] % 2
            okey[0] += 1
            S.op('dve', lambda e: e.tensor_copy(out=po[:, o, :], in_=ps[:, b, :]), reads=[('ps', b)], writes=[('po', o)])
            S.op('act', lambda e: e.activation(out=sqp[:, o, :], in_=po[:, o, :], func=AF.Square), reads=[('po', o)], writes=[('sqp', o)])
            S.op('pe', lambda e: e.matmul(ps[:, 6 + tt, :], lhsT=ones, rhs=sqp[:, o, :], start=(ct == 0), stop=(ct == KC - 1)),
                 reads=[('sqp', o), 'cst'], writes=[('ps', 6 + tt)])
            S.op('sp', lambda e: e.dma_start(out=pre_d[ct, :, tt * 512:(tt + 1) * 512], in_=po[:, o, :]), reads=[('po', o)], dma='po%d' % o)

        if True:
            with ExitStack() as st:
                yaT = sbt(st, "yaT", [128, 4, T], BF16)
                ybT = sbt(st, "ybT", [128, 16, T], BF16)
                S.op('sp', lambda e: e.dma_start(out=yaT[:], in_=ya_d.rearrange("h p t -> p h t")), writes=['yaT'], dma='c')
                S.op('sp', lambda e: e.dma_start(out=ybT[:], in_=yb_d.rearrange("h p t -> p h t")), writes=['ybT'], dma='c')
                mT = sbt(st, "mT", [128, KC, T], BF16)
                wb = sbt(st, "wb", [128, 2, KC, 256], BF16)
                gm = sbt(st, "gm", [128, 2, 2, T], BF16)
                t1 = sbt(st, "t1", [128, 2, 512], F32)
                t2 = sbt(st, "t2", [128, 2, 512], F32)
                po = sbt(st, "po", [128, 2, 512], F32)
                sqp = sbt(st, "sqp", [128, 2, 512], BF16)
                k = [0]
                for w0 in range(0, D, 256):
                    sa = load_w(wb, w_a, 0, 4, w0, 256)
                    sb_ = load_w(wb, w_b, 0, 16, w0, 256)
                    for cs_ in range(2):
                        ct = w0 // 128 + cs_
                        gi = ct % 2
                        S.op('sp', lambda e, ct=ct, gi=gi: e.dma_start(out=gm[:, gi, 0, :], in_=gmix_d[ct]), writes=[('gm', gi)], dma='gm%d' % gi)
                        S.op('sp', lambda e, ct=ct, gi=gi: e.dma_start(out=gm[:, gi, 1, :], in_=gmix_d[32 + ct]), writes=[('gm', gi)], dma='gm%d' % gi)
                        for tt in range(2):
                            t0 = tt * 512
                            ba = nb(); bb = nb()
                            mm_fm(wb, sa, 4, cs_, lambda kc, t0=t0: yaT[:, kc, t0:t0 + 512], 512, ba, ['yaT'])
                            mm_fm(wb, sb_, 16, cs_, lambda kc, t0=t0: ybT[:, kc, t0:t0 + 512], 512, bb, ['ybT'])
                            o = k[0] % 2
                            k[0] += 1
                            S.op('dve', lambda e, ba=ba, o=o, gi=gi, t0=t0: e.tensor_tensor(out=t1[:, o, :], in0=ps[:, ba, :], in1=gm[:, gi, 0, t0:t0 + 512], op=ALU.mult),
                                 reads=[('ps', ba), ('gm', gi)], writes=[('t1', o)])
                            S.op('dve', lambda e, bb=bb, o=o, gi=gi, t0=t0: e.tensor_tensor(out=t2[:, o, :], in0=ps[:, bb, :], in1=gm[:, gi, 1, t0:t0 + 512], op=ALU.mult),
                                 reads=[('ps', bb), ('gm', gi)], writes=[('t2', o)])
                            S.op('dve', lambda e, o=o, ct=ct, t0=t0: e.tensor_tensor(out=mT[:, ct, t0:t0 + 512], in0=t1[:, o, :], in1=t2[:, o, :], op=ALU.add),
                                 reads=[('t1', o), ('t2', o)], writes=[('mT', ct)])
                mrd = [('mT', c) for c in range(KC)]
                ok = [0]
                for w0 in range(0, D, 256):
                    s = load_w(wb, w_out, 0, KC, w0, 256)
                    for cs_ in range(2):
                        ct = w0 // 128 + cs_
                        for tt in range(2):
                            t0 = tt * 512
                            b = nb()
                            mm_fm(wb, s, KC, cs_, lambda kc, t0=t0: mT[:, kc, t0:t0 + 512], 512, b, mrd)
                            pre_store((po, sqp), b, ct, tt, ok)
                S.flush()
            with ExitStack() as st:
                epilogue(st, (6, 7), G_MIXPOST, lambda c, t0: xT_ext_v[:, c, HALO + t0:HALO + t0 + 512], x1_d, G_FFNPRE, True)
                S.flush()

            with ExitStack() as st:
                hT2 = sbt(st, "hT2", [128, KC, T], BF16)
                load_h(hT2)
                wb = sbt(st, "wb", [128, 4, KC, 128], BF16)
                ao = sbt(st, "ao", [128, 2, T], BF16)
                sg = sbt(st, "sg", [128, 2, 512], F32)
                h2rd = [('hN', c, tt) for c in range(KC) for tt in range(2)]
                wk_ = [0]
                for ft in range(FKC):
                    slots = []
                    for half_, c0 in enumerate((ft * 128, DFF + ft * 128)):
                        s = wk_[0] % 4
                        wk_[0] += 1
                        src = w_gu[:, c0:c0 + 128].rearrange("(k p) c -> p k c", p=128)
                        S.op('pool', lambda e, s=s, src=src: e.dma_start(out=wb[:, s, :16, :], in_=src[:, :16, :]), writes=[('wb', s)], dma='wb%d' % s)
                        S.op('pool', lambda e, s=s, src=src: e.dma_start(out=wb[:, s, 16:, :], in_=src[:, 16:, :]), writes=[('wb', s)], dma='wb%d' % s)
                        slots.append(s)
                    ai = ft % 2
                    for tt in range(2):
                        t0 = tt * 512
                        bg = nb(); bu = nb()
                        for kc in range(KC):
                            S.op('pe', lambda e, kc=kc, bg=bg, s=slots[0], t0=t0: e.matmul(ps[:, bg, :], lhsT=wb[:, s, kc, :], rhs=hT2[:, kc, t0:t0 + 512], start=(kc == 0), stop=(kc == KC - 1)),
                                 reads=[('wb', slots[0])] + (h2rd if kc == 0 else []), writes=[('ps', bg)])
                        for kc in range(KC):
                            S.op('pe', lambda e, kc=kc, bu=bu, s=slots[1], t0=t0: e.matmul(ps[:, bu, :], lhsT=wb[:, s, kc, :], rhs=hT2[:, kc, t0:t0 + 512], start=(kc == 0), stop=(kc == KC - 1)),
                                 reads=[('wb', slots[1])], writes=[('ps', bu)])
                        S.op('act', lambda e, bg=bg, tt=tt: e.activation(out=sg[:, tt, :], in_=ps[:, bg, :], func=AF.Silu), reads=[('ps', bg)], writes=[('sg', tt)])
                        S.op('dve', lambda e, bu=bu, tt=tt, ai=ai, t0=t0: e.tensor_tensor(out=ao[:, ai, t0:t0 + 512], in0=sg[:, tt, :], in1=ps[:, bu, :], op=ALU.mult),
                             reads=[('sg', tt), ('ps', bu)], writes=[('ao', ai)])
                    S.op('sp', lambda e, ft=ft, ai=ai: e.dma_start(out=act_d[ft], in_=ao[:, ai, :]), reads=[('ao', ai)], dma='ao%d' % ai)
                S.flush()
        with ExitStack() as st:
            aT = sbt(st, "aT", [128, FKC, 512], BF16)
            wb = sbt(st, "wb", [128, 2, FKC, 128], BF16)
            po = sbt(st, "po", [128, 2, 512], F32)
            sqp = sbt(st, "sqp", [128, 2, 512], BF16)
            ok = [0]
            wk_ = [0]
            for tt in range(2):
                t0 = tt * 512
                S.op('sp', lambda e, t0=t0: e.dma_start(out=aT[:, :43, :], in_=act_d[:43, :, t0:t0 + 512].rearrange("f p t -> p f t")), writes=['aT'], dma='aT')
                S.op('sp', lambda e, t0=t0: e.dma_start(out=aT[:, 43:, :], in_=act_d[43:, :, t0:t0 + 512].rearrange("f p t -> p f t")), writes=['aT'], dma='aT')
                for ct in range(KC):
                    s = wk_[0] % 2
                    wk_[0] += 1
                    src = w_down[:, ct * 128:(ct + 1) * 128].rearrange("(k p) c -> p k c", p=128)
                    S.op('pool', lambda e, s=s, src=src: e.dma_start(out=wb[:, s, :43, :], in_=src[:, :43, :]), writes=[('wb', s)], dma='wb%d' % s)
                    S.op('pool', lambda e, s=s, src=src: e.dma_start(out=wb[:, s, 43:, :], in_=src[:, 43:, :]), writes=[('wb', s)], dma='wb%d' % s)
                    b = nb()
                    for kc in range(FKC):
                        S.op('pe', lambda e, kc=kc, s=s, b=b: e.matmul(ps[:, b, :], lhsT=wb[:, s, kc, :], rhs=aT[:, kc, :], start=(kc == 0), stop=(kc == FKC - 1)),
                             reads=[('wb', s), 'aT'], writes=[('ps', b)])
                    pre_store((po, sqp), b, ct, tt, ok)
            S.flush()
        if True:
            with ExitStack() as st:
                epilogue(st, (6, 7), G_FFNPOST, lambda c, t0: x1_d[c, :, t0:t0 + 512], x2_d, G_PLEPRE, True)
                S.flush()
            with ExitStack() as st:
                hT3 = sbt(st, "hT3", [128, KC, T], BF16)
                load_h(hT3)
                wb = sbt(st, "wb", [128, 2, KC, 256], BF16)
                wp = sbt(st, "wp", [128, 2, 2, 256], BF16)
                pb = sbt(st, "pb", [128, 2, T], BF16)
                sg = sbt(st, "sg", [128, 2, 512], F32)
                pl = sbt(st, "pl", [128, 2, 512], F32)
                po = sbt(st, "po", [128, 2, 512], F32)
                sqp = sbt(st, "sqp", [128, 2, 512], BF16)
                S.op('pool', lambda e: e.dma_start(out=pb[:], in_=pT.rearrange("(k p) t -> p k t", p=128)), writes=['pb'], dma='c')
                h3rd = [('hN', c, tt) for c in range(KC) for tt in range(2)]
                ok = [0]
                k = [0]
                for w0 in range(0, D, 256):
                    s = load_w(wb, w_pg, 0, KC, w0, 256)
                    S.op('pool', lambda e, s=s, w0=w0: e.dma_start(out=wp[:, s, :, :], in_=w_ple[:, w0:w0 + 256].rearrange("(k p) c -> p k c", p=128)),
                         writes=[('wp', s)], dma='wp%d' % s)
                    for cs_ in range(2):
                        ct = w0 // 128 + cs_
                        for tt in range(2):
                            t0 = tt * 512
                            bg = nb(); bp = nb()
                            mm_fm(wb, s, KC, cs_, lambda kc, t0=t0: hT3[:, kc, t0:t0 + 512], 512, bg, h3rd)
                            for kc in range(2):
                                S.op('pe', lambda e, kc=kc, s=s, cs_=cs_, bp=bp, t0=t0: e.matmul(ps[:, bp, :], lhsT=wp[:, s, kc, cs_ * 128:(cs_ + 1) * 128], rhs=pb[:, kc, t0:t0 + 512],
                                                                                         start=(kc == 0), stop=(kc == 1)),
                                     reads=[('wp', s), 'pb'], writes=[('ps', bp)])
                            o = k[0] % 2
                            k[0] += 1
                            S.op('act', lambda e, bg=bg, o=o: e.activation(out=sg[:, o, :], in_=ps[:, bg, :], func=AF.Sigmoid), reads=[('ps', bg)], writes=[('sg', o)])
                            S.op('dve', lambda e, bp=bp, o=o: e.tensor_tensor(out=pl[:, o, :], in0=sg[:, o, :], in1=ps[:, bp, :], op=ALU.mult),
                                 reads=[('sg', o), ('ps', bp)], writes=[('pl', o)])
                            o2 = ok[0] % 2
                            ok[0] += 1
                            S.op('act', lambda e, o=o, o2=o2: e.activation(out=sqp[:, o2, :], in_=pl[:, o, :], func=AF.Square), reads=[('pl', o)], writes=[('sqp', o2)])
                            S.op('pe', lambda e, o2=o2, ct=ct, tt=tt: e.matmul(ps[:, 6 + tt, :], lhsT=ones, rhs=sqp[:, o2, :], start=(ct == 0), stop=(ct == KC - 1)),
                                 reads=[('sqp', o2), 'cst'], writes=[('ps', 6 + tt)])
                            S.op('sp', lambda e, o=o, ct=ct, tt=tt: e.dma_start(out=pre_d[ct, :, tt * 512:(tt + 1) * 512], in_=pl[:, o, :]), reads=[('pl', o)], dma='po%d' % o)
                S.flush()
        with ExitStack() as st:
            epilogue(st, (6, 7), G_PLEPOST, lambda c, t0: x2_d[c, :, t0:t0 + 512], None, None, None, final_out=outT)
            S.flush()
    return nc


def _bias(valid, reps=4):
    b = np.where(valid, 0.0, NEG).astype(np.float32)
    return np.tile(b, (1, reps)).astype(ml_dtypes.bfloat16)


def _host_consts(core):
    bf = ml_dtypes.bfloat16
    start = core * T
    c = {}
    half = 64
    inv = (10000.0 ** (-np.arange(half, dtype=np.float32) / half)).astype(np.float32)

    def cs(pos):
        ang = pos.astype(np.float32)[None, :] * np.concatenate([inv, inv])[:, None]
        co = np.cos(ang).astype(np.float32)
        si = np.sin(ang).astype(np.float32)
        si[:half] *= -1.0
        return np.stack([co, si], axis=1).astype(np.float32)
    c["cs_full"] = cs(np.arange(S_ALL))
    c["cs_ext"] = cs(np.arange(start - HALO, start + T))
    ident = np.eye(128, dtype=np.float32)
    swp = np.zeros((128, 128), np.float32)
    for m in range(128):
        swp[(m + 64) % 128, m] = 1.0
    c["consts"] = np.stack([ident, swp, np.ones((128, 128), np.float32)], axis=1).astype(bf)
    keys = np.arange(S_ALL)
    c["xsel"] = (keys[None, :] // 64 == np.arange(128)[:, None]).astype(np.float32).astype(bf)
    n = np.arange(512)
    m = np.arange(128)
    ov = np.clip(np.minimum(n[:, None] * 16 + 32, m[None, :] * 64 + 64) - np.maximum(n[:, None] * 16, m[None, :] * 64), 0, None) / 32.0
    ov[511] = 0.0
    c["ovm"] = ov.reshape(4, 128, 128).transpose(1, 0, 2).astype(np.float32).astype(bf)
    gs = np.zeros((48, 48, 128), np.float32)
    for k in range(48):
        gs[k, k, :] = 1.0
    c["gsel"] = gs.astype(bf)
    bc = np.zeros((8, 128, 4, 512), bf)
    af = np.zeros((8, 128, 128), np.float32)
    bw = np.zeros((8, 128, 5, 512), bf)
    for qb in range(8):
        pos = start + qb * 128 + np.arange(128)
        for j in range(4):
            nn = j * 128 + np.arange(128)
            valid = (nn[:, None] * 16 + 31 <= pos[None, :]) & (nn[:, None] < 511)
            bc[qb, :, j, :] = _bias(valid)
        cur = pos // 64
        sblk = np.arange(128)
        forced = (sblk[None] == 0) | (sblk[None] == cur[:, None]) | (sblk[None] == cur[:, None] - 1)
        sval = sblk[None] * 64 <= pos[:, None]
        af[qb] = np.where(sval, 1000.0 * forced, -1e30).astype(np.float32)
        for i in range(5):
            kp = start + (qb - 4 + i) * 128 + np.arange(128)
            valid = (kp[:, None] <= pos[None, :]) & (pos[None, :] - kp[:, None] < 512) & (kp[:, None] >= 0)
            bw[qb, :, i, :] = _bias(valid)
    c["biasC"] = bc
    c["addF"] = af
    c["biasW"] = bw
    dg = np.zeros((128, 8, 512), bf)
    kk = np.arange(128)
    dg[:, core, :] = _bias(kk[:, None] <= kk[None, :])
    c["diagb"] = dg
    bd = np.zeros((128, 22, 512), bf)

    def dil(r, nq, a0, ks, nk):
        qpos = start + r * (a0 + np.arange(nq))
        kpos = start + r * (ks + np.arange(nk))
        valid = (kpos[:, None] >= 0) & (kpos[:, None] <= qpos[None, :]) & (qpos[None, :] - kpos[:, None] <= 128 * r)
        t = np.full((128, 4 * nq), NEG, np.float32)
        t[:nk] = np.tile(np.where(valid, 0.0, NEG), (1, 4))
        out = np.zeros((128, 512), np.float32)
        out[:, :4 * nq] = t
        return out.astype(bf)
    for blk in range(8):
        bd[:, blk * 2] = dil(1, 128, blk * 128, blk * 128 - 128, 128)
        bd[:, blk * 2 + 1] = dil(1, 128, blk * 128, blk * 128, 128)
    for blk in range(2):
        bd[:, 16 + blk * 2] = dil(4, 128, blk * 128, blk * 128 - 128, 128)
        bd[:, 16 + blk * 2 + 1] = dil(4, 128, blk * 128, blk * 128, 128)
    bd[:, 20] = dil(16, 64, 0, -128, 128)
    bd[:, 21] = dil(16, 64, 0, 0, 64)
    c["biasD"] = bd
    return c


_NC_CACHE = {}


def kernel(**inputs):
    f = lambda k: np.asarray(inputs[k], dtype=np.float32)
    x = f("x")[0]
    xT = np.ascontiguousarray(x.T)
    p = f("p")[0, 0]
    gl = lambda v: np.ascontiguousarray(v.reshape(32, 128).T)
    gains = np.concatenate([gl(f(k)[0]) for k in ("g_mix_pre", "g_mix_post", "g_ffn_pre", "g_ffn_post", "g_ple_pre", "g_ple_post")]
                           + [np.zeros((128, 64), np.float32)], axis=1)
    shared = {
        "xT_full": xT, "gains": np.ascontiguousarray(gains), "w_in": f("w_in")[0],
        "peT_k": np.ascontiguousarray(f("pe_ck")[0].T), "peT_v": np.ascontiguousarray(f("pe_cv")[0].T),
        "w_ck1": f("w_ck1")[0], "w_ck2": f("w_ck2")[0], "w_cv1": f("w_cv1")[0], "w_cv2": f("w_cv2")[0],
        "w_a": f("w_a")[0], "w_b": f("w_b")[0], "w_out": f("w_out")[0], "w_gu": f("w_gu")[0],
        "w_down": f("w_down")[0], "w_pg": f("w_ple_gate")[0], "w_ple": f("w_ple")[0],
    }
    in_maps = []
    for c in range(NCORES):
        start = c * T
        ext = np.zeros((D, EXT), np.float32)
        lo = max(0, start - HALO)
        ext[:, EXT - (start + T - lo):] = xT[:, lo:start + T]
        m = dict(shared)
        m["xT_ext"] = ext
        m["pT"] = np.ascontiguousarray(p[start:start + T].T)
        m.update(_host_consts(c))
        in_maps.append(m)
    if "nc" not in _NC_CACHE:
        _NC_CACHE["nc"] = build()
    _ncr = int(os.environ.get("MK_NCORES", NCORES))
    _c0 = int(os.environ.get("MK_CORE0", 0))
    if _ncr != NCORES:
        res = run_bass_kernel_spmd(_NC_CACHE["nc"], in_maps[_c0:_c0 + _ncr], core_ids=list(range(_ncr)))
        _NC_CACHE["res"] = res
        return None
    res = run_bass_kernel_spmd(_NC_CACHE["nc"], in_maps, core_ids=list(range(NCORES)))
    if _DBG:
        _NC_CACHE["res"] = res
    out = np.concatenate([np.asarray(r["outT"]).T for r in res.results], axis=0)
    return out.reshape(1, S_ALL, D).astype(np.float32)
```

```python
import numpy as np
import ml_dtypes
from contextlib import ExitStack
import concourse.bass as bass
import concourse.mybir as mybir
from concourse.bass_utils import run_bass_kernel_spmd

F32 = mybir.dt.float32
BF16 = mybir.dt.bfloat16
AF = mybir.ActivationFunctionType
ALU = mybir.AluOpType

NCORES = 8
S_ALL = 8192
D = 4096
KC = 32
T = 1024
HALO = 2048
EXT = HALO + T
DFF = 11008
FKC = 86
SCALE = 128 ** -0.5
EPS = 1e-6
NEG = -30000.0
C_QA, C_KA, C_VA, C_QB, C_KC, C_VC, C_KSL, C_VSL, C_KWN, C_VWN, C_GN, C_GM = (
    0, 1536, 3072, 4608, 6656, 7168, 7680, 8192, 8704, 9216, 9728, 9776)

ENGS = ("pe", "act", "dve", "pool", "sp")


import os
_STOP = int(os.environ.get("MK_STOP", "1000"))
_DBG = [x for x in os.environ.get("MK_DBG", "").split(",") if x]


class StopBuild(Exception):
    pass


class Sched:
    def __init__(self, nc, st):
        self.nc = nc
        self.esem = {e: st.enter_context(nc.semaphore("s_" + e)) for e in ENGS}
        self.dsem = {}
        self.st = st
        self.seq = {e: 0 for e in ENGS}
        self.nsig = {e: 0 for e in ENGS}
        self.known = {e: {} for e in ENGS}
        self.dcount = {}
        self._reset()

    def _reset(self):
        self.ops = {e: [] for e in ENGS}
        self.lastw = {}
        self.readers = {}
        self.signal = {e: set() for e in ENGS}
        self.dma_issuer = {}

    def _need(self, eng, seq, tok, waits):
        kind, src, val = tok
        if kind == 'E':
            if src == eng:
                if eng == 'pe' or seq - val > 2:
                    return
            k = ('E', src)
        else:
            k = ('D', src)
        if self.known[eng].get(k, -1) >= val:
            return
        if val > waits.get(k, -1):
            waits[k] = val

    def op(self, eng, fn, reads=(), writes=(), dma=None):
        if getattr(self, 'stopped', False):
            return
        seq = self.seq[eng]
        self.seq[eng] += 1
        waits = {}
        for r in reads:
            t = self.lastw.get(r)
            if t is not None:
                self._need(eng, seq, t, waits)
        for w in writes:
            t = self.lastw.get(w)
            if t is not None:
                self._need(eng, seq, t, waits)
            for t in self.readers.get(w, ()):
                self._need(eng, seq, t, waits)
        for k, v in waits.items():
            self.known[eng][k] = v
            if k[0] == 'E':
                self.signal[k[1]].add(v)
        if dma is not None:
            if dma not in self.dsem:
                self.dsem[dma] = self.st.enter_context(self.nc.semaphore("d_" + str(dma)))
            c = self.dcount.get(dma, 0) + 1
            self.dcount[dma] = c
            tok = ('D', dma, c)
            self.dma_issuer.setdefault(eng, {})[dma] = c
        else:
            tok = ('E', eng, seq)
        for r in reads:
            self.readers.setdefault(r, []).append(tok)
        for w in writes:
            self.lastw[w] = tok
            self.readers[w] = []
        self.ops[eng].append((seq, fn, waits, dma))

    def flush(self):
        nc = self.nc
        if getattr(self, 'stopped', False):
            self._reset()
            return
        last = {}
        for e in ENGS:
            cs = [o[0] for o in self.ops[e] if o[3] is None and o[1] is not None]
            if cs:
                last[e] = cs[-1]
                self.signal[e].add(cs[-1])
        for e in ENGS:
            waits = {}
            for e2, s in last.items():
                if e2 != e and self.known[e].get(('E', e2), -1) < s:
                    waits[('E', e2)] = s
                    self.known[e][('E', e2)] = s
            for c, v in self.dcount.items():
                if self.known[e].get(('D', c), -1) < v:
                    waits[('D', c)] = v
                    self.known[e][('D', c)] = v
            self.ops[e].append((None, None, waits, None))
        rank = {}
        for e in ENGS:
            srt = sorted(self.signal[e])
            rank[e] = {s: self.nsig[e] + i + 1 for i, s in enumerate(srt)}
            self.nsig[e] += len(srt)
        esem, dsem = self.esem, self.dsem

        def run(e, engobj):
            sig = self.signal[e]
            for (seq, fn, waits, dma) in self.ops[e]:
                for k, v in waits.items():
                    if k[0] == 'E':
                        engobj.wait_ge(esem[k[1]], rank[k[1]][v])
                    else:
                        engobj.wait_ge(dsem[k[1]], 16 * v)
                if fn is None:
                    continue
                ins = fn(engobj)
                if dma is not None:
                    ins.then_inc(dsem[dma], 16)
                elif seq in sig:
                    ins.then_inc(esem[e], 1)

        with nc.Block() as block:
            @block.tensor
            def _(eng):
                run('pe', eng)

            @block.scalar
            def _(eng):
                run('act', eng)

            @block.vector
            def _(eng):
                run('dve', eng)

            @block.gpsimd
            def _(eng):
                run('pool', eng)

            @block.sync
            def _(eng):
                run('sp', eng)
        self._reset()
        self.nflush = getattr(self, 'nflush', 0) + 1
        if self.nflush >= _STOP:
            self.stopped = True


def build():
    nc = bass.Bass("TRN2", target_bir_lowering=False)

    def din(name, shape, dt=F32):
        return nc.dram_tensor(name, list(shape), dt, kind="ExternalInput").ap()

    def dscr(name, shape, dt):
        return nc.dram_tensor(name, list(shape), dt, kind=("ExternalOutput" if name in _DBG else "Internal")).ap()

    xT_full = din("xT_full", [D, S_ALL])
    xT_ext = din("xT_ext", [D, EXT])
    pT = din("pT", [256, T])
    gains = din("gains", [128, 8 * 32])
    w_in = din("w_in", [D, 17968])
    peT_k = din("peT_k", [128, 32]); peT_v = din("peT_v", [128, 32])
    w_ck1 = din("w_ck1", [4096, 128]); w_ck2 = din("w_ck2", [128, 128])
    w_cv1 = din("w_cv1", [4096, 128]); w_cv2 = din("w_cv2", [128, 128])
    w_a = din("w_a", [512, D]); w_b = din("w_b", [2048, D]); w_out = din("w_out", [D, D])
    w_gu = din("w_gu", [D, 2 * DFF]); w_down = din("w_down", [DFF, D])
    w_pg = din("w_pg", [D, D]); w_ple = din("w_ple", [256, D])
    cs_full = din("cs_full", [128, 2, S_ALL])
    cs_ext = din("cs_ext", [128, 2, EXT])
    consts = din("consts", [128, 3, 128], BF16)
    xsel = din("xsel", [128, S_ALL], BF16)
    ovm = din("ovm", [128, 4, 128], BF16)
    gsel = din("gsel", [48, 48, 128], BF16)
    biasC = din("biasC", [8, 128, 4, 512], BF16)
    addF = din("addF", [8, 128, 128])
    diagb = din("diagb", [128, 8, 512], BF16)
    biasW = din("biasW", [8, 128, 5, 512], BF16)
    biasD = din("biasD", [128, 22, 512], BF16)
    outT = nc.dram_tensor("outT", [D, T], F32, kind="ExternalOutput").ap()

    kcT_d = dscr("kcT_d", [8, 128, S_ALL], BF16)
    kslT_d = dscr("kslT_d", [4, 128, S_ALL], BF16)
    vsl_d = dscr("vsl_d", [S_ALL, 512], BF16)
    kaT_d = dscr("kaT_d", [12, 128, EXT], BF16)
    va_d = dscr("va_d", [EXT, 1536], BF16)
    kwnT_d = dscr("kwnT_d", [4, 128, EXT], BF16)
    vwn_d = dscr("vwn_d", [EXT, 512], BF16)
    gmix_d = dscr("gmix_d", [64, 128, T], BF16)
    pre_d = dscr("pre_d", [32, 128, T], F32)
    x1_d = dscr("x1_d", [32, 128, T], F32)
    x2_d = dscr("x2_d", [32, 128, T], F32)
    act_d = dscr("act_d", [FKC, 128, T], BF16)
    qaT_d = dscr("qaT_d", [12, 128, T], BF16)
    qrT_d = dscr("qrT_d", [16, 128, T], BF16)
    qoT_d = dscr("qoT_d", [16, 128, T], BF16)
    h_d = dscr("h_d", [32, 128, T], BF16)
    ya_d = dscr("ya_d", [4, 128, T], BF16)
    yb_d = dscr("yb_d", [16, 128, T], BF16)

    with ExitStack() as top:
      try:
        S = Sched(nc, top)
        _build_body(nc, top, S, locals())
      except StopBuild:
        pass
    return nc


def _build_body(nc, top, S, L):
    globals().update({k: v for k, v in L.items() if k not in ('nc', 'top', 'S')})
    if True:
        _uid = [0]

        def sbt(st, name, shape, dt):
            _uid[0] += 1
            return st.enter_context(nc.sbuf_tensor("%s_%d" % (name, _uid[0]), list(shape), dt))
        ps = top.enter_context(nc.psum_tensor("ps", [128, 8, 512], F32))
        cst = sbt(top, "cst", [128, 3, 128], BF16)
        ident, swp, ones = cst[:, 0, :], cst[:, 1, :], cst[:, 2, :]
        gn = sbt(top, "gn", [128, 8 * 32], F32)
        kc_c = sbt(top, "kc_c", [128, 4, 512], BF16)
        vc_c = sbt(top, "vc_c", [128, 4, 4, 128], BF16)
        gsT = sbt(top, "gsT", [48, T], BF16)
        rs2p = sbt(top, "rs2p", [128, 2, 512], F32)

        S.op('sp', lambda e: e.dma_start(out=cst[:], in_=consts), writes=['cst'], dma='c')
        S.op('sp', lambda e: e.dma_start(out=gn[:], in_=gains), writes=['cst'], dma='c')
        epsb = sbt(top, "epsb", [128, 1], F32)
        S.op('pool', lambda e: e.memset(epsb[:], EPS), writes=['epsb'])
        S.op('pool', lambda e: e.memset(vc_c[:], 0.0), writes=['vc_c'])
        S.op('pool', lambda e: e.memset(kc_c[:], 0.0), writes=['kc_c'])
        G_MIXPRE, G_MIXPOST, G_FFNPRE, G_FFNPOST, G_PLEPRE, G_PLEPOST = range(6)

        def gcol(gi, c):
            return gn[:, gi * 32 + c: gi * 32 + c + 1]

        bank_rr = [0]

        def nb(n=4, base=0):
            b = base + bank_rr[0] % n
            bank_rr[0] += 1
            return b

        xT_full_v = xT_full.rearrange("(c p) t -> p c t", p=128)
        xT_ext_v = xT_ext.rearrange("(c p) t -> p c t", p=128)

        def norm_tile(st_, xs, hT, src_v, t0, gi, sq, rs):
            for q in range(4):
                S.op('sp', lambda e, q=q: e.dma_start(out=xs[:, 8 * q:8 * q + 8, :], in_=src_v[:, 8 * q:8 * q + 8, t0:t0 + 512]),
                     writes=[('xs', q)], dma='xs%d' % q)
            b = 7
            for c in range(KC):
                S.op('act', lambda e, c=c: e.activation(out=sq[:, c % 2, :], in_=xs[:, c, :], func=AF.Square),
                     reads=[('xs', c // 8)], writes=[('sq', c % 2)])
                S.op('pe', lambda e, c=c: e.matmul(ps[:, b, :], lhsT=ones, rhs=sq[:, c % 2, :], start=(c == 0), stop=(c == KC - 1)),
                     reads=[('sq', c % 2), 'cst'], writes=[('ps', b)])
            S.op('act', lambda e: e.activation(out=rs[:], in_=ps[:, b, :], func=AF.Sqrt, scale=1.0 / D, bias=epsb[:]),
                 reads=[('ps', b), 'epsb'], writes=['rs'])
            S.op('dve', lambda e: e.reciprocal(out=rs[:], in_=rs[:]), reads=['rs'], writes=['rs'])
            for c in range(KC):
                S.op('dve', lambda e, c=c: e.scalar_tensor_tensor(out=hT[:, c, :], in0=xs[:, c, :], scalar=gcol(gi, c), in1=rs[:], op0=ALU.mult, op1=ALU.mult),
                     reads=[('xs', c // 8), 'rs', 'cst'], writes=[('hT', id(hT), c)])

        wslot = [0]

        def load_w(wb, w_ap, r0, kc_n, c0, ncols):
            s = wslot[0] % 2
            wslot[0] += 1
            src = w_ap[r0:r0 + kc_n * 128, c0:c0 + ncols].rearrange("(k p) c -> p k c", p=128)
            half = max(1, kc_n // 2)
            for h0 in range(0, kc_n, half):
                h1 = min(kc_n, h0 + half)
                S.op('pool', lambda e, s=s, h0=h0, h1=h1: e.dma_start(out=wb[:, s, h0:h1, :ncols], in_=src[:, h0:h1, :]),
                     writes=[('wb', s)], dma='wb%d' % s)
            return s

        def mm_fm(wb, s, kc_n, csub, act_fn, n, b, extra_reads=()):
            for kc in range(kc_n):
                S.op('pe', lambda e, kc=kc: e.matmul(ps[:, b, :n], lhsT=wb[:, s, kc, csub * 128:(csub + 1) * 128], rhs=act_fn(kc),
                                                    start=(kc == 0), stop=(kc == kc_n - 1)),
                     reads=[('wb', s)] + list(extra_reads), writes=[('ps', b)])

        def rope_store(b, n, cs_t, t_off, dst_fn, tmpq, tmp1, key, extra_reads=()):
            i = key % 2
            S.op('dve', lambda e: e.tensor_copy(out=tmpq[:, i, :n], in_=ps[:, b, :n]), reads=[('ps', b)] + list(extra_reads), writes=[('tq', i)])
            b2 = nb()
            S.op('pe', lambda e: e.matmul(ps[:, b2, :n], lhsT=swp, rhs=tmpq[:, i, :n], start=True, stop=True),
                 reads=[('tq', i), 'cst'], writes=[('ps', b2)])
            S.op('dve', lambda e: e.tensor_tensor(out=tmp1[:, i, :n], in0=ps[:, b, :n], in1=cs_t[:, 0, t_off:t_off + n], op=ALU.mult),
                 reads=[('ps', b), 'cs'] + list(extra_reads), writes=[('t1', i)])
            S.op('dve', lambda e: e.tensor_tensor(out=tmp1[:, 2 + i, :n], in0=ps[:, b2, :n], in1=cs_t[:, 1, t_off:t_off + n], op=ALU.mult),
                 reads=[('ps', b2), 'cs'] + list(extra_reads), writes=[('t2', i)])
            dst, wkeys = dst_fn()
            S.op('dve', lambda e: e.tensor_tensor(out=dst, in0=tmp1[:, i, :n], in1=tmp1[:, 2 + i, :n], op=ALU.add),
                 reads=[('t1', i), ('t2', i)], writes=wkeys)

        for pss in range(2):
            with ExitStack() as st:
                wres = sbt(st, "wres", [128, KC, 1024], BF16)
                xsb = sbt(st, "xsb", [128, KC, 512], BF16)
                hT2 = [sbt(st, "hTa", [128, KC, 512], BF16), sbt(st, "hTb", [128, KC, 512], BF16)]
                sq = sbt(st, "sq", [128, 2, 512], BF16)
                rs = sbt(st, "rs", [128, 512], F32)
                ob = sbt(st, "ob", [128, 4, 512], BF16)
                tmpq = sbt(st, "tmpq", [128, 2, 512], BF16)
                tmp1 = sbt(st, "tmp1", [128, 4, 512], F32)
                cs_t = sbt(st, "cs_t", [128, 2, 2, 512], F32)
                c0 = C_KC if pss == 0 else C_KSL
                srcw = w_in[:, c0:c0 + 1024].rearrange("(k p) c -> p k c", p=128)
                for q in range(4):
                    S.op('pool', lambda e, q=q: e.dma_start(out=wres[:, 8 * q:8 * q + 8, :], in_=srcw[:, 8 * q:8 * q + 8, :]),
                         writes=['wres'], dma='wres')
                NT = S_ALL // 512

                def normA(tt):
                    t0 = tt * 512
                    for q in range(4):
                        S.op('pool', lambda e, q=q: e.dma_start(out=xsb[:, 8 * q:8 * q + 8, :], in_=xT_full_v[:, 8 * q:8 * q + 8, t0:t0 + 512]),
                             writes=[('xs', q)], dma='xs%d' % q)
                    for c in range(KC):
                        S.op('act', lambda e, c=c: e.activation(out=sq[:, c % 2, :], in_=xsb[:, c, :], func=AF.Square),
                             reads=[('xs', c // 8)], writes=[('sq', c % 2)])
                        S.op('pe', lambda e, c=c: e.matmul(ps[:, 7, :], lhsT=ones, rhs=sq[:, c % 2, :], start=(c == 0), stop=(c == KC - 1)),
                             reads=[('sq', c % 2), 'cst'], writes=[('ps', 7)])
                    S.op('act', lambda e: e.activation(out=rs[:], in_=ps[:, 7, :], func=AF.Sqrt, scale=1.0 / D, bias=epsb[:]),
                         reads=[('ps', 7), 'epsb'], writes=['rs'])
                    S.op('dve', lambda e: e.reciprocal(out=rs[:], in_=rs[:]), reads=['rs'], writes=['rs'])

                def normB(tt):
                    h = hT2[tt % 2]
                    for c in range(KC):
                        S.op('dve', lambda e, c=c: e.scalar_tensor_tensor(out=h[:, c, :], in0=xsb[:, c, :], scalar=gcol(G_MIXPRE, c), in1=rs[:], op0=ALU.mult, op1=ALU.mult),
                             reads=[('xs', c // 8), 'rs', 'cst'], writes=[('hT', tt % 2, c)])

                def unit_fm(tt, ct):
                    hT = hT2[tt % 2]
                    hreads = [('hT', tt % 2, c) for c in range(KC)]
                    t0 = tt * 512
                    b = nb()
                    for kc in range(KC):
                        S.op('pe', lambda e, kc=kc: e.matmul(ps[:, b, :], lhsT=wres[:, kc, ct * 128:(ct + 1) * 128], rhs=hT[:, kc, :],
                                                             start=(kc == 0), stop=(kc == KC - 1)),
                             reads=['wres'] + (hreads if kc == 0 else []), writes=[('ps', b)])
                    o = ct % 4
                    if pss == 0:
                        S.op('act', lambda e: e.copy(out=ob[:, o, :], in_=ps[:, b, :]), reads=[('ps', b)], writes=[('ob', o)])
                        S.op('sp', lambda e: e.dma_start(out=kcT_d[ct, :, t0:t0 + 512], in_=ob[:, o, :]), reads=[('ob', o)], dma='ob%d' % o)
                    else:
                        rope_store(b, 512, cs_t[:, tt % 2], 0, lambda: (ob[:, o, :], [('ob', o)]), tmpq, tmp1, ct, [('cs', tt % 2)])
                        S.op('sp', lambda e: e.dma_start(out=kslT_d[ct, :, t0:t0 + 512], in_=ob[:, o, :]), reads=[('ob', o)], dma='ob%d' % o)

                def unit_tm(tt, tk):
                    hT = hT2[tt % 2]
                    hreads = [('hT', tt % 2, c) for c in range(KC)]
                    t0 = tt * 512
                    b = nb()
                    for kc in range(KC):
                        S.op('pe', lambda e, kc=kc: e.matmul(ps[:, b, :], lhsT=hT[:, kc, tk * 128:(tk + 1) * 128], rhs=wres[:, kc, 512:1024],
                                                             start=(kc == 0), stop=(kc == KC - 1)),
                             reads=['wres'] + (hreads if kc == 0 else []), writes=[('ps', b)])
                    S.op('act', lambda e: e.copy(out=ob[:, tk, :], in_=ps[:, b, :]), reads=[('ps', b)], writes=[('ob', tk)])
                    S.op('sp', lambda e: e.dma_start(out=vsl_d[t0 + tk * 128:t0 + (tk + 1) * 128, :], in_=ob[:, tk, :]), reads=[('ob', tk)], dma='ob%d' % tk)

                normA(0)
                normB(0)
                for tt in range(NT):
                    if pss == 1:
                        S.op('sp', lambda e, tt=tt: e.dma_start(out=cs_t[:, tt % 2], in_=cs_full[:, :, tt * 512:(tt + 1) * 512]), writes=[('cs', tt % 2)], dma='cs%d' % (tt % 2))
                        units = [(unit_fm, ct) for ct in range(4)] + [(unit_tm, tk) for tk in range(4)]
                    else:
                        units = [(unit_fm, ct) for ct in range(8)]
                    for fn_, a_ in units[:4]:
                        fn_(tt, a_)
                    if tt + 1 < NT:
                        normA(tt + 1)
                        normB(tt + 1)
                    for fn_, a_ in units[4:]:
                        fn_(tt, a_)
                S.flush()

        with ExitStack() as st:
            kin = sbt(st, "kin", [128, 2, S_ALL], BF16)
            w1 = sbt(st, "w1", [128, 2, 32, 128], BF16)
            w2 = sbt(st, "w2", [128, 2, 128], BF16)
            pe_b = sbt(st, "pe_b", [128, 2, 32], BF16)
            pe2 = sbt(st, "pe2", [128, 2, 32, 2], BF16)
            cb = sbt(st, "cb", [128, 2], F32)
            xg = sbt(st, "xg", [128, 2, 512], F32)
            tg = sbt(st, "tg", [128, 2, 512], F32)
            ge = sbt(st, "ge", [128, 2, 512], BF16)
            for kv, (wa1, wa2, pea) in enumerate(((w_ck1, w_ck2, peT_k), (w_cv1, w_cv2, peT_v))):
                S.op('pool', lambda e, kv=kv, wa1=wa1: e.dma_start(out=w1[:, kv], in_=wa1.rearrange("(l d) h -> d l h", d=128)), writes=['w1'], dma='c')
                S.op('pool', lambda e, kv=kv, wa2=wa2: e.dma_start(out=w2[:, kv], in_=wa2), writes=['w1'], dma='c')
                S.op('pool', lambda e, kv=kv, pea=pea: e.dma_start(out=pe_b[:, kv], in_=pea), writes=['w1'], dma='c')
            for j2 in range(2):
                S.op('dve', lambda e, j2=j2: e.tensor_copy(out=pe2[:, :, :, j2], in_=pe_b[:]), reads=['w1'], writes=[('pe2', j2)])
            for kv in range(2):
                b = nb()
                for l in range(32):
                    S.op('pe', lambda e, l=l, kv=kv, b=b: e.matmul(ps[:, b, 0:2], lhsT=w1[:, kv, l, :], rhs=pe2[:, kv, l, :], start=(l == 0), stop=(l == 31)),
                         reads=['w1', ('pe2', 0), ('pe2', 1)], writes=[('ps', b)])
                S.op('dve', lambda e, kv=kv, b=b: e.tensor_copy(out=cb[:, kv:kv + 1], in_=ps[:, b, 0:1]), reads=[('ps', b)], writes=[('cb', kv)])
            NCMP = 511
            for g in range(4):
                for kv in range(2):
                    i = (g * 2 + kv) % 2
                    S.op('sp', lambda e, g=g, kv=kv, i=i: e.dma_start(out=kin[:, i, :], in_=kcT_d[kv * 4 + g]), writes=[('kin', i)], dma='kin%d' % i)
                    b = nb()
                    for l in range(32):
                        S.op('pe', lambda e, l=l, kv=kv, i=i, b=b: e.matmul(ps[:, b, :NCMP], lhsT=w1[:, kv, l, :], rhs=kin[:, i, l:l + 16 * (NCMP - 1) + 1:16],
                                                                      start=(l == 0), stop=(l == 31)),
                             reads=['w1', ('kin', i)], writes=[('ps', b)])
                    S.op('dve', lambda e, kv=kv, i=i, b=b: e.tensor_scalar(out=xg[:, i, :NCMP], in0=ps[:, b, :NCMP], scalar1=cb[:, kv:kv + 1], scalar2=None, op0=ALU.add),
                         reads=[('ps', b), ('cb', kv)], writes=[('xg', i)])
                    S.op('dve', lambda e, i=i: e.tensor_tensor(out=tg[:, i, :NCMP], in0=xg[:, i, :NCMP], in1=xg[:, i, :NCMP], op=ALU.mult),
                         reads=[('xg', i)], writes=[('tg', i)])
                    S.op('dve', lambda e, i=i: e.tensor_scalar(out=tg[:, i, :NCMP], in0=tg[:, i, :NCMP], scalar1=0.044715, scalar2=1.0, op0=ALU.mult, op1=ALU.add),
                         reads=[('tg', i)], writes=[('tg', i)])
                    S.op('dve', lambda e, i=i: e.tensor_tensor(out=tg[:, i, :NCMP], in0=tg[:, i, :NCMP], in1=xg[:, i, :NCMP], op=ALU.mult),
                         reads=[('tg', i), ('xg', i)], writes=[('tg', i)])
                    S.op('act', lambda e, i=i: e.activation(out=tg[:, i, :NCMP], in_=tg[:, i, :NCMP], func=AF.Sigmoid, scale=1.5957691216057308),
                         reads=[('tg', i)], writes=[('tg', i)])
                    S.op('dve', lambda e, i=i: e.tensor_tensor(out=ge[:, i, :NCMP], in0=tg[:, i, :NCMP], in1=xg[:, i, :NCMP], op=ALU.mult),
                         reads=[('tg', i), ('xg', i)], writes=[('ge', i)])
                    if kv == 0:
                        b2 = nb()
                        S.op('pe', lambda e, i=i, b2=b2: e.matmul(ps[:, b2, :NCMP], lhsT=w2[:, 0, :], rhs=ge[:, i, :NCMP], start=True, stop=True),
                             reads=['w1', ('ge', i)], writes=[('ps', b2)])
                        S.op('act', lambda e, g=g, b2=b2: e.copy(out=kc_c[:, g, :NCMP], in_=ps[:, b2, :NCMP]), reads=[('ps', b2)], writes=['kc_c'])
                    else:
                        for j in range(4):
                            m = 128 if j < 3 else 127
                            b2 = nb()
                            S.op('pe', lambda e, i=i, j=j, m=m, b2=b2: e.matmul(ps[:m, b2, :128], lhsT=ge[:, i, j * 128:j * 128 + m], rhs=w2[:, 1, :], start=True, stop=True),
                                 reads=['w1', ('ge', i)], writes=[('ps', b2)])
                            S.op('act', lambda e, g=g, j=j, m=m, b2=b2: e.copy(out=vc_c[:m, g, j, :], in_=ps[:m, b2, :128]), reads=[('ps', b2)], writes=['vc_c'])
            S.flush()

        for half in range(3):
            with ExitStack() as st:
                hT3 = [sbt(st, "hT3_%d" % i, [128, KC, 512], BF16) for i in range(2)]
                xs = sbt(st, "xs", [128, KC, 512], F32)
                sq = sbt(st, "sq", [128, 2, 512], BF16)
                rs = sbt(st, "rs", [128, 512], F32)
                wb = sbt(st, "wb", [128, 2, KC, 256], BF16)
                ob = sbt(st, "ob", [128, 4, 512], BF16)
                tmpq = sbt(st, "tmpq", [128, 2, 512], BF16)
                tmp1 = sbt(st, "tmp1", [128, 4, 512], F32)
                cs_t = sbt(st, "cs_t", [128, 2, 1024], F32)
                tb = half * 1024
                S.op('sp', lambda e, tb=tb: e.dma_start(out=cs_t[:], in_=cs_ext[:, :, tb:tb + 1024]), writes=['cs'], dma='cs')
                for i in range(2):
                    norm_tile(st, xs, hT3[i], xT_ext_v, tb + i * 512, G_MIXPRE, sq, rs)
                hr = [[('hT', id(hT3[i]), c) for c in range(KC)] for i in range(2)]
                okey = [0]

                def fm_cols(c0, ncols, tiles, sink):
                    for w0 in range(0, ncols, 256):
                        wn = min(256, ncols - w0)
                        s = load_w(wb, w_in, 0, KC, c0 + w0, wn)
                        for i in tiles:
                            for cs_ in range((wn + 127) // 128):
                                mrows = min(128, wn - cs_ * 128)
                                b = nb()
                                for kc in range(KC):
                                    S.op('pe', lambda e, kc=kc, s=s, cs_=cs_, i=i, b=b, mrows=mrows: e.matmul(
                                        ps[:mrows, b, :], lhsT=wb[:, s, kc, cs_ * 128:cs_ * 128 + mrows], rhs=hT3[i][:, kc, :],
                                        start=(kc == 0), stop=(kc == KC - 1)),
                                        reads=[('wb', s)] + (hr[i] if kc == 0 else []), writes=[('ps', b)])
                                sink((w0 + cs_ * 128) // 128, i, b, mrows)

                def tm_cols(c0, ncols, tiles, dst_d, dcol0):
                    for w0 in range(0, ncols, 256):
                        s = load_w(wb, w_in, 0, KC, c0 + w0, 256)
                        for i in tiles:
                            for tk in range(4):
                                b = nb()
                                for kc in range(KC):
                                    S.op('pe', lambda e, kc=kc, s=s, i=i, tk=tk, b=b: e.matmul(
                                        ps[:, b, :256], lhsT=hT3[i][:, kc, tk * 128:(tk + 1) * 128], rhs=wb[:, s, kc, :],
                                        start=(kc == 0), stop=(kc == KC - 1)),
                                        reads=[('wb', s)] + (hr[i] if kc == 0 else []), writes=[('ps', b)])
                                o = okey[0] % 4
                                okey[0] += 1
                                S.op('act', lambda e, b=b, o=o: e.copy(out=ob[:, o, :256], in_=ps[:, b, :256]), reads=[('ps', b)], writes=[('ob', o)])
                                r0 = tb + i * 512 + tk * 128
                                S.op('sp', lambda e, o=o, r0=r0, w0=w0: e.dma_start(out=dst_d[r0:r0 + 128, dcol0 + w0:dcol0 + w0 + 256], in_=ob[:, o, :256]),
                                     reads=[('ob', o)], dma='ob%d' % o)

                def sink_rope_dram(dst_d, ct_off=0):
                    def sink(ct, i, b, mrows):
                        o = okey[0] % 4
                        okey[0] += 1
                        rope_store(b, 512, cs_t, i * 512, lambda o=o: (ob[:, o, :], [('ob', o)]), tmpq, tmp1, okey[0])
                        t0 = tb + i * 512
                        S.op('sp', lambda e, ct=ct, o=o, t0=t0: e.dma_start(out=dst_d[ct_off + ct, :, t0:t0 + 512], in_=ob[:, o, :]),
                             reads=[('ob', o)], dma='ob%d' % o)
                    return sink

                if half == 0:
                    fm_cols(C_KA + 1024, 512, range(2), sink_rope_dram(kaT_d, 8))
                    tm_cols(C_VA + 1024, 512, range(2), va_d, 1024)
                elif half == 1:
                    fm_cols(C_KA, 1024, (1,), sink_rope_dram(kaT_d))
                    fm_cols(C_KA + 1024, 512, range(2), sink_rope_dram(kaT_d, 8))
                    fm_cols(C_KWN, 512, (1,), sink_rope_dram(kwnT_d))
                    tm_cols(C_VA, 1024, (1,), va_d, 0)
                    tm_cols(C_VA + 1024, 512, range(2), va_d, 1024)
                    tm_cols(C_VWN, 512, (1,), vwn_d, 0)
                else:
                    fm_cols(C_KA, 1536, range(2), sink_rope_dram(kaT_d))
                    fm_cols(C_KWN, 512, range(2), sink_rope_dram(kwnT_d))
                    tm_cols(C_VA, 1536, range(2), va_d, 0)
                    tm_cols(C_VWN, 512, range(2), vwn_d, 0)
                if half == 2:
                    own = (0, 1)

                    def sink_q(dst_d, extra=()):
                        def sink(ct, i, b, mrows):
                            t0 = i * 512
                            o = okey[0] % 4
                            okey[0] += 1
                            rope_store(b, 512, cs_t, i * 512, lambda o=o: (ob[:, o, :], [('ob', o)]), tmpq, tmp1, okey[0], extra)
                            S.op('sp', lambda e: e.dma_start(out=dst_d[ct, :, t0:t0 + 512], in_=ob[:, o, :]), reads=[('ob', o)], dma='ob%d' % o)
                        return sink
                    fm_cols(C_QA, 1536, own, sink_q(qaT_d))

                    def sink_qb(ct, i, b, mrows):
                        t0 = i * 512
                        o = okey[0] % 4
                        okey[0] += 1
                        S.op('act', lambda e: e.copy(out=ob[:, o, :], in_=ps[:, b, :]), reads=[('ps', b)], writes=[('ob', o)])
                        S.op('sp', lambda e: e.dma_start(out=qrT_d[ct, :, t0:t0 + 512], in_=ob[:, o, :]), reads=[('ob', o)], dma='ob%d' % o)
                        sink_q(qoT_d, [('ob', o)])(ct, i, b, mrows)
                    fm_cols(C_QB, 2048, own, sink_qb)

                    def sink_gn(ct, i, b, mrows):
                        t0 = i * 512
                        S.op('act', lambda e: e.activation(out=gsT[:, t0:t0 + 512], in_=ps[:48, b, :], func=AF.Sigmoid), reads=[('ps', b)], writes=['gsT'])
                    fm_cols(C_GN, 48, own, sink_gn)

                    def sink_gm(ct, i, b, mrows):
                        t0 = i * 512
                        o = okey[0] % 4
                        okey[0] += 1
                        S.op('act', lambda e: e.activation(out=ob[:, o, :], in_=ps[:, b, :], func=AF.Sigmoid), reads=[('ps', b)], writes=[('ob', o)])
                        S.op('sp', lambda e: e.dma_start(out=gmix_d[ct, :, t0:t0 + 512], in_=ob[:, o, :]), reads=[('ob', o)], dma='ob%d' % o)
                    fm_cols(C_GM, 8192, own, sink_gm)
                S.flush()

        acc_i = [0]

        def attn_chunks(chunks, nq, n_heads_cols, o_bank, d_bank):
            ncols = n_heads_cols
            nchunks = len(chunks)

            def emit_pv(ci, ch, pi):
                nk = ch['nk']
                for h in range(4):
                    v_ap, rd = ch['v_fn'](h)
                    S.op('pe', lambda e, v_ap=v_ap, h=h, nk=nk, pi=pi, ci=ci: e.matmul(
                        ps[:, o_bank, h * nq:(h + 1) * nq], lhsT=v_ap, rhs=PT[:nk, pi, h * nq:(h + 1) * nq],
                        start=(ci == 0 and h == 0), stop=(ci == nchunks - 1)), reads=rd + [('PT', pi)], writes=[('ps', o_bank)])
                S.op('pe', lambda e, nk=nk, pi=pi, ci=ci: e.matmul(ps[:, d_bank, :ncols], lhsT=cst[:nk, 2, :], rhs=PT[:nk, pi, :ncols],
                                                                start=(ci == 0), stop=(ci == nchunks - 1)),
                     reads=[('PT', pi), 'cst'], writes=[('ps', d_bank)])

            pend = None
            for ci, ch in enumerate(chunks):
                nk = ch['nk']
                b = nb()
                ops_ = ch['s_ops']
                for oi, (l_ap, r_ap, o_ap, rd) in enumerate(ops_):
                    S.op('pe', lambda e, l_ap=l_ap, r_ap=r_ap, o_ap=o_ap, b=b, st_=ch['starts'][oi], sp_=ch['stops'][oi]: e.matmul(
                        o_ap(b), lhsT=l_ap, rhs=r_ap, start=st_, stop=sp_), reads=rd, writes=[('ps', b)])
                pi = acc_i[0] % 3
                acc_i[0] += 1
                S.op('act', lambda e, b=b, nk=nk, pi=pi: e.activation(out=PT[:nk, pi, :ncols], in_=ps[:nk, b, :ncols], func=AF.Exp, scale=SCALE),
                     reads=[('ps', b)], writes=[('PT', pi)])
                if ch.get('keep') is not None:
                    ch['keep'](pi, nk)
                if os.environ.get('MK_OLDATTN'):
                    emit_pv(ci, ch, pi)
                    continue
                if pend is not None:
                    emit_pv(*pend)
                pend = (ci, ch, pi)
            if pend is not None:
                emit_pv(*pend)

        with ExitStack() as st:
            PT = sbt(st, "PT", [128, 3, 512], BF16)
            kaS = sbt(st, "kaS", [128, 4, EXT], BF16)
            vS = sbt(st, "vS", [128, 4, 512], BF16)
            bD = sbt(st, "bD", [128, 22, 512], BF16)
            numT = sbt(st, "numT", [128, 4, T], F32)
            denT = sbt(st, "denT", [128, 4, T], F32)
            qaT = sbt(st, "qaT", [128, 4, T], BF16)
            yaT = sbt(st, "yaT", [128, 4, T], BF16)
            S.op('sp', lambda e: e.dma_start(out=bD[:], in_=biasD), writes=['bD'], dma='c')
            vslot = [0]
            for grp, r in enumerate((1, 4, 16)):
                for h in range(4):
                    S.op('sp', lambda e, grp=grp, h=h: e.dma_start(out=kaS[:, h, :], in_=kaT_d[grp * 4 + h]), writes=['kaS'], dma='kaS')
                    S.op('sp', lambda e, grp=grp, h=h: e.dma_start(out=qaT[:, h, :], in_=qaT_d[grp * 4 + h]), writes=['qaT'], dma='qaT')
                nq = 128 if r < 16 else 64
                nblk = (T // r) // nq
                for rho in range(r):
                    for blk in range(nblk):
                        a0 = blk * nq
                        if grp == 0:
                            bidx = [blk * 2, blk * 2 + 1]
                        elif grp == 1:
                            bidx = [16 + blk * 2, 16 + blk * 2 + 1]
                        else:
                            bidx = [20, 21]
                        chunks = []
                        for ci, (ks, nk) in enumerate(((a0 - 128, 128), (a0, nq))):
                            e0 = HALO + r * ks + rho
                            vs = vslot[0] % 4
                            vslot[0] += 1
                            S.op('sp', lambda e, e0=e0, nk=nk, vs=vs, grp=grp, r=r: e.dma_start(
                                out=vS[:nk, vs, :], in_=va_d[e0:e0 + r * (nk - 1) + 1:r, grp * 512:(grp + 1) * 512]),
                                writes=[('vS', vs)], dma='vS%d' % vs)
                            q0 = rho + r * a0
                            s_ops = [(cst[:nk, 0, :nk], bD[:nk, bidx[ci], :4 * nq],
                                      (lambda b, nk=nk, nq=nq: ps[:nk, b, :4 * nq]), ['bD', 'cst'])]
                            for h in range(4):
                                s_ops.append((kaS[:, h, e0:e0 + r * (nk - 1) + 1:r],
                                              qaT[:, h, q0:q0 + r * (nq - 1) + 1:r],
                                              (lambda b, h=h, nk=nk, nq=nq: ps[:nk, b, h * nq:(h + 1) * nq]),
                                              ['kaS', 'qaT']))
                            chunks.append(dict(nk=nk, s_ops=s_ops, starts=[True] + [False] * 4, stops=[False] * 4 + [True],
                                               v_fn=(lambda h, vs=vs, nk=nk: (vS[:nk, vs, h * 128:(h + 1) * 128], [('vS', vs)]))))
                        ob_, db_ = (4, 5) if (acc_i[0] // 2) % 2 == 0 else (6, 7)
                        attn_chunks(chunks, nq, 4 * nq, ob_, db_)
                        q0 = rho + r * a0
                        dstn = numT[:, :, q0:q0 + r * (nq - 1) + 1:r]
                        dstd = denT[:, :, q0:q0 + r * (nq - 1) + 1:r]
                        srcn = ps[:, ob_, :4 * nq].rearrange("p (h q) -> p h q", h=4)
                        srcd = ps[:, db_, :4 * nq].rearrange("p (h q) -> p h q", h=4)
                        if grp == 0:
                            S.op('dve', lambda e, dstn=dstn, srcn=srcn: e.tensor_copy(out=dstn, in_=srcn), reads=[('ps', ob_)], writes=['numT'])
                            S.op('act', lambda e, dstd=dstd, srcd=srcd: e.copy(out=dstd, in_=srcd), reads=[('ps', db_)], writes=['denT'])
                        else:
                            S.op('dve', lambda e, dstn=dstn, srcn=srcn: e.tensor_tensor(out=dstn, in0=dstn, in1=srcn, op=ALU.add), reads=[('ps', ob_), 'numT'], writes=['numT'])
                            S.op('dve', lambda e, dstd=dstd, srcd=srcd: e.tensor_tensor(out=dstd, in0=dstd, in1=srcd, op=ALU.add), reads=[('ps', db_), 'denT'], writes=['denT'])
            S.op('dve', lambda e: e.reciprocal(out=denT[:], in_=denT[:]), reads=['denT'], writes=['denT'])
            S.op('dve', lambda e: e.tensor_tensor(out=yaT[:], in0=numT[:], in1=denT[:], op=ALU.mult), reads=['numT', 'denT'], writes=['yaT'])
            S.op('sp', lambda e: e.dma_start(out=ya_d.rearrange("h p t -> p h t"), in_=yaT[:]), reads=['yaT'], dma='c')
            S.flush()

        with ExitStack() as st:
            PT = sbt(st, "PT", [128, 3, 512], BF16)
            PK = sbt(st, "PK", [128, 4, 512], BF16)
            Ksel = sbt(st, "Ksel", [128, S_ALL], BF16)
            Vsel = sbt(st, "Vsel", [128, 64, 128], BF16)
            Kw = sbt(st, "Kw", [128, 1536], BF16)
            Vw = sbt(st, "Vw", [128, 12, 128], BF16)
            xs_sb = sbt(st, "xs_sb", [128, S_ALL], BF16)
            ov_sb = sbt(st, "ov_sb", [128, 4, 128], BF16)
            gs_sb = sbt(st, "gs_sb", [48, 48, 128], BF16)
            bC = sbt(st, "bC", [128, 4, 512], BF16)
            bW = sbt(st, "bW", [128, 5, 512], BF16)
            dg = sbt(st, "dg", [128, 8, 512], BF16)
            aF = sbt(st, "aF", [128, 128], F32)
            rden = sbt(st, "rden", [128, 512], F32)
            pn = sbt(st, "pn", [128, 2, 512], BF16)
            sc = sbt(st, "sc", [128, 128], F32)
            wk = sbt(st, "wk", [128, 128], F32)
            mx = sbt(st, "mx", [128, 16], F32)
            selb = sbt(st, "selb", [128, 128], BF16)
            selT = sbt(st, "selT", [128, 512], BF16)
            grep = sbt(st, "grep", [128, 512], F32)
            acc = sbt(st, "acc", [128, 512], F32)
            tmpo = sbt(st, "tmpo", [128, 512], F32)
            ybo = sbt(st, "ybo", [128, 512], BF16)
            qrT = sbt(st, "qrT", [128, 4, T], BF16)
            qoT = sbt(st, "qoT", [128, 4, T], BF16)
            S.op('sp', lambda e: e.dma_start(out=xs_sb[:], in_=xsel), writes=['k2'], dma='c')
            S.op('sp', lambda e: e.dma_start(out=ov_sb[:], in_=ovm), writes=['k2'], dma='c')
            S.op('sp', lambda e: e.dma_start(out=gs_sb[:], in_=gsel), writes=['k2'], dma='c')
            S.op('sp', lambda e: e.dma_start(out=dg[:], in_=diagb), writes=['k2'], dma='c')

            def finish_part(part, g, ob_, db_, first, QB0):
                S.op('dve', lambda e: e.tensor_scalar(out=rden[:], in0=ps[:, db_, :], scalar1=1e-30, scalar2=None, op0=ALU.max),
                     reads=[('ps', db_)], writes=['rden'])
                S.op('dve', lambda e: e.reciprocal(out=rden[:], in_=rden[:]), reads=['rden'], writes=['rden'])
                bg = nb()
                for h in range(4):
                    col = (g * 4 + h) * 3 + part
                    S.op('pe', lambda e, h=h, col=col: e.matmul(ps[:, bg, h * 128:(h + 1) * 128], lhsT=gs_sb[:, col, :], rhs=gsT[:, QB0:QB0 + 128], start=(h == 0), stop=True),
                         reads=['k2', 'gsT'], writes=[('ps', bg)])
                S.op('dve', lambda e: e.tensor_tensor(out=grep[:], in0=ps[:, bg, :], in1=rden[:], op=ALU.mult), reads=[('ps', bg), 'rden'], writes=['grep'])
                if first:
                    S.op('dve', lambda e: e.tensor_tensor(out=acc[:], in0=ps[:, ob_, :], in1=grep[:], op=ALU.mult), reads=[('ps', ob_), 'grep'], writes=['acc'])
                else:
                    S.op('dve', lambda e: e.tensor_tensor(out=tmpo[:], in0=ps[:, ob_, :], in1=grep[:], op=ALU.mult), reads=[('ps', ob_), 'grep'], writes=['tmpo'])
                    S.op('dve', lambda e: e.tensor_tensor(out=acc[:], in0=acc[:], in1=tmpo[:], op=ALU.add), reads=['acc', 'tmpo'], writes=['acc'])

            def nsa_block(g, qb):
                QB0 = qb * 128
                S.op('sp', lambda e, qb=qb: e.dma_start(out=bC[:], in_=biasC[qb]), writes=['bC'], dma='bC')
                S.op('sp', lambda e, qb=qb: e.dma_start(out=bW[:], in_=biasW[qb]), writes=['bW'], dma='bW')
                S.op('sp', lambda e, qb=qb: e.dma_start(out=aF[:], in_=addF[qb]), writes=['aF'], dma='aF')
                qr4 = qrT[:, :, QB0:QB0 + 128]
                qo4 = qoT[:, :, QB0:QB0 + 128]
                qrd = ['qrT']
                qod = ['qoT']
                full = lambda b: ps[:, b, :]
                chunks = []
                for j in range(4):
                    def keep(pi, nk, j=j):
                        S.op('pool', lambda e, pi=pi, j=j: e.tensor_copy(out=PK[:, j, :], in_=PT[:, pi, :]), reads=[('PT', pi)], writes=[('PK', j)])
                    chunks.append(dict(nk=128, starts=[True, False], stops=[False, True], keep=keep,
                                       s_ops=[(kc_c[:, g, j * 128:(j + 1) * 128], qr4, full, ['kc_c'] + qrd),
                                              (ident, bC[:, j, :], full, ['bC', 'cst'])],
                                       v_fn=(lambda h, j=j, g=g: (vc_c[:, g, j, :], ['vc_c']))))
                attn_chunks(chunks, 128, 512, 4, 5)
                finish_part(0, g, 4, 5, True, QB0)
                bi = nb()
                for j in range(4):
                    S.op('dve', lambda e, j=j: e.tensor_tensor(out=pn[:, j % 2, :], in0=PK[:, j, :], in1=rden[:], op=ALU.mult),
                         reads=[('PK', j), 'rden'], writes=[('pn', j % 2)])
                    for h in range(4):
                        S.op('pe', lambda e, j=j, h=h: e.matmul(ps[:, bi, :128], lhsT=pn[:, j % 2, h * 128:(h + 1) * 128], rhs=ov_sb[:, j, :],
                                                               start=(j == 0 and h == 0), stop=(j == 3 and h == 3)),
                             reads=[('pn', j % 2), 'k2'], writes=[('ps', bi)])
                S.op('dve', lambda e: e.tensor_tensor(out=sc[:], in0=ps[:, bi, :128], in1=aF[:], op=ALU.add), reads=[('ps', bi), 'aF'], writes=['sc'])
                S.op('dve', lambda e: e.max(out=mx[:, 0:8], in_=sc[:]), reads=['sc'], writes=['mx'])
                S.op('dve', lambda e: e.match_replace(out=wk[:], in_to_replace=mx[:, 0:8], in_values=sc[:], imm_value=-1e30), reads=['sc', 'mx'], writes=['wk'])
                S.op('dve', lambda e: e.max(out=mx[:, 8:16], in_=wk[:]), reads=['wk'], writes=['mx'])
                S.op('dve', lambda e: e.tensor_scalar(out=mx[:, 15:16], in0=mx[:, 15:16], scalar1=-1e29, scalar2=None, op0=ALU.max), reads=['mx'], writes=['mx'])
                S.op('dve', lambda e: e.tensor_scalar(out=selb[:], in0=sc[:], scalar1=mx[:, 15:16], scalar2=NEG, op0=ALU.is_lt, op1=ALU.mult),
                     reads=['sc', 'mx'], writes=['selb'])
                bt = nb()
                S.op('pe', lambda e: e.matmul(ps[:, bt, :128], lhsT=selb[:], rhs=ident, start=True, stop=True), reads=['selb', 'cst'], writes=[('ps', bt)])
                for h in range(4):
                    S.op('dve', (lambda e, h=h: e.tensor_copy(out=selT[:, h * 128:(h + 1) * 128], in_=ps[:, bt, :128])),
                         reads=[('ps', bt)], writes=[('selT', h)])
                selrd = [('selT', h) for h in range(4)]
                chunks = []
                nch = 57 + qb
                for j in range(nch):
                    s_ops = [(Ksel[:, j * 128:(j + 1) * 128], qo4, full, ['Ksel'] + qod),
                             (xs_sb[:, j * 128:(j + 1) * 128], selT[:], full, ['k2'] + selrd)]
                    if j % 8 == qb:
                        s_ops.append((ident, dg[:, j // 8, :], full, ['k2', 'cst']))
                    n = len(s_ops)
                    chunks.append(dict(nk=128, s_ops=s_ops, starts=[True] + [False] * (n - 1), stops=[False] * (n - 1) + [True],
                                       v_fn=(lambda h, j=j: (Vsel[:, j, :], ['Vsel']))))
                attn_chunks(chunks, 128, 512, 6, 7)
                finish_part(1, g, 6, 7, False, QB0)
                chunks = []
                for i in range(5):
                    kj = qb + i
                    chunks.append(dict(nk=128, starts=[True, False], stops=[False, True],
                                       s_ops=[(Kw[:, kj * 128:(kj + 1) * 128], qo4, full, ['Kw'] + qod),
                                              (ident, bW[:, i, :], full, ['bW', 'cst'])],
                                       v_fn=(lambda h, kj=kj: (Vw[:, kj, :], ['Vw']))))
                attn_chunks(chunks, 128, 512, 4, 5)
                finish_part(2, g, 4, 5, False, QB0)
                S.op('act', lambda e: e.copy(out=ybo[:], in_=acc[:]), reads=['acc'], writes=['ybo'])
                S.op('sp', lambda e: e.dma_start(out=yb_d[4 * g:4 * g + 4, :, QB0:QB0 + 128].rearrange("h p q -> p h q"), in_=ybo[:].rearrange("p (h q) -> p h q", h=4)), reads=['ybo'], dma='ybo')

            for g in range(4):
                S.op('sp', lambda e, g=g: e.dma_start(out=Ksel[:], in_=kslT_d[g]), writes=['Ksel'], dma='Ksel')
                for jj in range(8):
                    S.op('sp', lambda e, g=g, jj=jj: e.dma_start(out=Vsel[:, 8 * jj:8 * jj + 8, :], in_=vsl_d[1024 * jj:1024 * jj + 1024, g * 128:(g + 1) * 128].rearrange("(j k) d -> k j d", k=128)), writes=['Vsel'], dma='Vsel')
                S.op('sp', lambda e, g=g: e.dma_start(out=qrT[:], in_=qrT_d[4 * g:4 * g + 4].rearrange("h p t -> p h t")), writes=['qrT'], dma='qrT')
                S.op('sp', lambda e, g=g: e.dma_start(out=qoT[:], in_=qoT_d[4 * g:4 * g + 4].rearrange("h p t -> p h t")), writes=['qoT'], dma='qoT')
                S.op('sp', lambda e, g=g: e.dma_start(out=Kw[:], in_=kwnT_d[g, :, HALO - 512:EXT]), writes=['Kw'], dma='Kw')
                S.op('sp', lambda e, g=g: e.dma_start(out=Vw[:], in_=vwn_d[HALO - 512:EXT, g * 128:(g + 1) * 128].rearrange("(j k) d -> k j d", k=128)), writes=['Vw'], dma='Vw')
                for qb in range(8):
                    nsa_block(g, qb)
            S.flush()

        xo_v = xT_ext_v

        def epilogue(st, sums_banks, g_post, xin_fn, xout_d, g_next, hN, final_out=None):
            rsb = sbt(st, "rsb", [128, 2, 512], F32)
            hb = sbt(st, "hb", [128, 2, 512], BF16)
            pv = sbt(st, "pv", [128, 2, 512], F32)
            xv = sbt(st, "xv", [128, 2, 512], F32)
            sq2 = sbt(st, "sq2", [128, 2, 512], BF16)
            for tt in range(2):
                S.op('act', lambda e, tt=tt: e.activation(out=rsb[:, tt, :], in_=ps[:, sums_banks[tt], :], func=AF.Sqrt, scale=1.0 / D, bias=epsb[:]),
                     reads=[('ps', sums_banks[tt]), 'epsb'], writes=[('rsb', tt)])
                S.op('dve', lambda e, tt=tt: e.reciprocal(out=rsb[:, tt, :], in_=rsb[:, tt, :]), reads=[('rsb', tt)], writes=[('rsb', tt)])
            for tt in range(2):
                t0 = tt * 512
                for c in range(KC):
                    i = c % 2
                    S.op('sp', lambda e, c=c, i=i, t0=t0: e.dma_start(out=pv[:, i, :], in_=pre_d[c, :, t0:t0 + 512]), writes=[('pv', i)], dma='pv%d' % i)
                    S.op('sp', lambda e, c=c, i=i, t0=t0: e.dma_start(out=xv[:, i, :], in_=xin_fn(c, t0)), writes=[('xv', i)], dma='xv%d' % i)
                    S.op('dve', lambda e, c=c, i=i, tt=tt: e.scalar_tensor_tensor(out=pv[:, i, :], in0=pv[:, i, :], scalar=gcol(g_post, c), in1=rsb[:, tt, :], op0=ALU.mult, op1=ALU.mult),
                         reads=[('pv', i), ('rsb', tt), 'cst'], writes=[('pv', i)])
                    S.op('dve', lambda e, i=i: e.tensor_tensor(out=xv[:, i, :], in0=xv[:, i, :], in1=pv[:, i, :], op=ALU.add),
                         reads=[('pv', i), ('xv', i)], writes=[('xv', i)])
                    dst = final_out if final_out is not None else xout_d
                    S.op('sp', lambda e, c=c, i=i, t0=t0, dst=dst: e.dma_start(
                        out=(dst[c * 128:(c + 1) * 128, t0:t0 + 512] if final_out is not None else dst[c, :, t0:t0 + 512]), in_=xv[:, i, :]),
                        reads=[('xv', i)], dma='xo%d' % i)
                    if hN is not None:
                        S.op('act', lambda e, i=i: e.activation(out=sq2[:, i, :], in_=xv[:, i, :], func=AF.Square), reads=[('xv', i)], writes=[('sq2', i)])
                        S.op('pe', lambda e, c=c, i=i, tt=tt: e.matmul(ps[:, sums_banks[tt], :], lhsT=ones, rhs=sq2[:, i, :], start=(c == 0), stop=(c == KC - 1)),
                             reads=[('sq2', i), 'cst'] + ([('rsb', tt)] if c == 0 else []), writes=[('ps', sums_banks[tt])])
                        S.op('act', lambda e, c=c, i=i: e.activation(out=hb[:, i, :], in_=xv[:, i, :], func=AF.Copy, scale=gcol(g_next, c)),
                             reads=[('xv', i), 'cst'], writes=[('hb', i)])
                        S.op('sp', lambda e, c=c, i=i, t0=t0: e.dma_start(out=h_d[c, :, t0:t0 + 512], in_=hb[:, i, :]), reads=[('hb', i)], dma='hb%d' % i)
                if hN is not None:
                    S.op('act', lambda e, tt=tt: e.activation(out=rs2p[:, tt, :], in_=ps[:, sums_banks[tt], :], func=AF.Sqrt, scale=1.0 / D, bias=epsb[:]),
                         reads=[('ps', sums_banks[tt]), 'epsb'], writes=[('rs2', tt)])
                    S.op('dve', lambda e, tt=tt: e.reciprocal(out=rs2p[:, tt, :], in_=rs2p[:, tt, :]), reads=[('rs2', tt)], writes=[('rs2', tt)])

        def load_h(hT_):
            for q in range(4):
                S.op('sp', lambda e, q=q: e.dma_start(out=hT_[:, 8 * q:8 * q + 8, :], in_=h_d[8 * q:8 * q + 8].rearrange("c p t -> p c t")), writes=[('hld', q)], dma='hld')
            for c in range(KC):
                for tt in range(2):
                    S.op('dve' if (c + tt) % 2 else 'pool', lambda e, c=c, tt=tt: e.tensor_tensor(out=hT_[:, c, tt * 512:(tt + 1) * 512], in0=hT_[:, c, tt * 512:(tt + 1) * 512], in1=rs2p[:, tt, :], op=ALU.mult),
                         reads=[('hld', c // 8)], writes=[('hN', c, tt)])

        def pre_store(st_bufs, b, ct, tt, okey):
            po, sqp = st_bufs
            o = okey[0] % 2
            okey[0] += 1
            S.op('dve', lambda e: e.tensor_copy(out=po[:, o, :], in_=ps[:, b, :]), reads=[('ps', b)], writes=[('po', o)])
            S.op('act', lambda e: e.activation(out=sqp[:, o, :], in_=po[:, o, :], func=AF.Square), reads=[('po', o)], writes=[('sqp', o)])
            S.op('pe', lambda e: e.matmul(ps[:, 6 + tt, :], lhsT=ones, rhs=sqp[:, o, :], start=(ct == 0), stop=(ct == KC - 1)),
                 reads=[('sqp', o), 'cst'], writes=[('ps', 6 + tt)])
            S.op('sp', lambda e: e.dma_start(out=pre_d[ct, :, tt * 512:(tt + 1) * 512], in_=po[:, o, :]), reads=[('po', o)], dma='po%d' % o)

        if True:
            with ExitStack() as st:
                yaT = sbt(st, "yaT", [128, 4, T], BF16)
                ybT = sbt(st, "ybT", [128, 16, T], BF16)
                S.op('sp', lambda e: e.dma_start(out=yaT[:], in_=ya_d.rearrange("h p t -> p h t")), writes=['yaT'], dma='c')
                S.op('sp', lambda e: e.dma_start(out=ybT[:], in_=yb_d.rearrange("h p t -> p h t")), writes=['ybT'], dma='c')
                mT = sbt(st, "mT", [128, KC, T], BF16)
                wb = sbt(st, "wb", [128, 2, KC, 256], BF16)
                gm = sbt(st, "gm", [128, 2, 2, T], BF16)
                t1 = sbt(st, "t1", [128, 2, 512], F32)
                t2 = sbt(st, "t2", [128, 2, 512], F32)
                po = sbt(st, "po", [128, 2, 512], F32)
                sqp = sbt(st, "sqp", [128, 2, 512], BF16)
                k = [0]
                for w0 in range(0, D, 256):
                    sa = load_w(wb, w_a, 0, 4, w0, 256)
                    sb_ = load_w(wb, w_b, 0, 16, w0, 256)
                    for cs_ in range(2):
                        ct = w0 // 128 + cs_
                        gi = ct % 2
                        S.op('sp', lambda e, ct=ct, gi=gi: e.dma_start(out=gm[:, gi, 0, :], in_=gmix_d[ct]), writes=[('gm', gi)], dma='gm%d' % gi)
                        S.op('sp', lambda e, ct=ct, gi=gi: e.dma_start(out=gm[:, gi, 1, :], in_=gmix_d[32 + ct]), writes=[('gm', gi)], dma='gm%d' % gi)
                        for tt in range(2):
                            t0 = tt * 512
                            ba = nb(); bb = nb()
                            mm_fm(wb, sa, 4, cs_, lambda kc, t0=t0: yaT[:, kc, t0:t0 + 512], 512, ba, ['yaT'])
                            mm_fm(wb, sb_, 16, cs_, lambda kc, t0=t0: ybT[:, kc, t0:t0 + 512], 512, bb, ['ybT'])
                            o = k[0] % 2
                            k[0] += 1
                            S.op('dve', lambda e, ba=ba, o=o, gi=gi, t0=t0: e.tensor_tensor(out=t1[:, o, :], in0=ps[:, ba, :], in1=gm[:, gi, 0, t0:t0 + 512], op=ALU.mult),
                                 reads=[('ps', ba), ('gm', gi)], writes=[('t1', o)])
                            S.op('dve', lambda e, bb=bb, o=o, gi=gi, t0=t0: e.tensor_tensor(out=t2[:, o, :], in0=ps[:, bb, :], in1=gm[:, gi, 1, t0:t0 + 512], op=ALU.mult),
                                 reads=[('ps', bb), ('gm', gi)], writes=[('t2', o)])
                            S.op('dve', lambda e, o=o, ct=ct, t0=t0: e.tensor_tensor(out=mT[:, ct, t0:t0 + 512], in0=t1[:, o, :], in1=t2[:, o, :], op=ALU.add),
                                 reads=[('t1', o), ('t2', o)], writes=[('mT', ct)])
                mrd = [('mT', c) for c in range(KC)]
                ok = [0]
                for w0 in range(0, D, 256):
                    s = load_w(wb, w_out, 0, KC, w0, 256)
                    for cs_ in range(2):
                        ct = w0 // 128 + cs_
                        for tt in range(2):
                            t0 = tt * 512
                            b = nb()
                            mm_fm(wb, s, KC, cs_, lambda kc, t0=t0: mT[:, kc, t0:t0 + 512], 512, b, mrd)
                            pre_store((po, sqp), b, ct, tt, ok)
                S.flush()
            with ExitStack() as st:
                epilogue(st, (6, 7), G_MIXPOST, lambda c, t0: xT_ext_v[:, c, HALO + t0:HALO + t0 + 512], x1_d, G_FFNPRE, True)
                S.flush()

            with ExitStack() as st:
                hT2 = sbt(st, "hT2", [128, KC, T], BF16)
                load_h(hT2)
                wb = sbt(st, "wb", [128, 4, KC, 128], BF16)
                ao = sbt(st, "ao", [128, 2, T], BF16)
                sg = sbt(st, "sg", [128, 2, 512], F32)
                h2rd = [('hN', c, tt) for c in range(KC) for tt in range(2)]
                wk_ = [0]
                for ft in range(FKC):
                    slots = []
                    for half_, c0 in enumerate((ft * 128, DFF + ft * 128)):
                        s = wk_[0] % 4
                        wk_[0] += 1
                        src = w_gu[:, c0:c0 + 128].rearrange("(k p) c -> p k c", p=128)
                        S.op('pool', lambda e, s=s, src=src: e.dma_start(out=wb[:, s, :16, :], in_=src[:, :16, :]), writes=[('wb', s)], dma='wb%d' % s)
                        S.op('pool', lambda e, s=s, src=src: e.dma_start(out=wb[:, s, 16:, :], in_=src[:, 16:, :]), writes=[('wb', s)], dma='wb%d' % s)
                        slots.append(s)
                    ai = ft % 2
                    for tt in range(2):
                        t0 = tt * 512
                        bg = nb(); bu = nb()
                        for kc in range(KC):
                            S.op('pe', lambda e, kc=kc, bg=bg, s=slots[0], t0=t0: e.matmul(ps[:, bg, :], lhsT=wb[:, s, kc, :], rhs=hT2[:, kc, t0:t0 + 512], start=(kc == 0), stop=(kc == KC - 1)),
                                 reads=[('wb', slots[0])] + (h2rd if kc == 0 else []), writes=[('ps', bg)])
                        for kc in range(KC):
                            S.op('pe', lambda e, kc=kc, bu=bu, s=slots[1], t0=t0: e.matmul(ps[:, bu, :], lhsT=wb[:, s, kc, :], rhs=hT2[:, kc, t0:t0 + 512], start=(kc == 0), stop=(kc == KC - 1)),
                                 reads=[('wb', slots[1])], writes=[('ps', bu)])
                        S.op('act', lambda e, bg=bg, tt=tt: e.activation(out=sg[:, tt, :], in_=ps[:, bg, :], func=AF.Silu), reads=[('ps', bg)], writes=[('sg', tt)])
                        S.op('dve', lambda e, bu=bu, tt=tt, ai=ai, t0=t0: e.tensor_tensor(out=ao[:, ai, t0:t0 + 512], in0=sg[:, tt, :], in1=ps[:, bu, :], op=ALU.mult),
                             reads=[('sg', tt), ('ps', bu)], writes=[('ao', ai)])
                    S.op('sp', lambda e, ft=ft, ai=ai: e.dma_start(out=act_d[ft], in_=ao[:, ai, :]), reads=[('ao', ai)], dma='ao%d' % ai)
                S.flush()
        with ExitStack() as st:
            aT = sbt(st, "aT", [128, FKC, 512], BF16)
            wb = sbt(st, "wb", [128, 2, FKC, 128], BF16)
            po = sbt(st, "po", [128, 2, 512], F32)
            sqp = sbt(st, "sqp", [128, 2, 512], BF16)
            ok = [0]
            wk_ = [0]
            for tt in range(2):
                t0 = tt * 512
                S.op('sp', lambda e, t0=t0: e.dma_start(out=aT[:, :43, :], in_=act_d[:43, :, t0:t0 + 512].rearrange("f p t -> p f t")), writes=['aT'], dma='aT')
                S.op('sp', lambda e, t0=t0: e.dma_start(out=aT[:, 43:, :], in_=act_d[43:, :, t0:t0 + 512].rearrange("f p t -> p f t")), writes=['aT'], dma='aT')
                for ct in range(KC):
                    s = wk_[0] % 2
                    wk_[0] += 1
                    src = w_down[:, ct * 128:(ct + 1) * 128].rearrange("(k p) c -> p k c", p=128)
                    S.op('pool', lambda e, s=s, src=src: e.dma_start(out=wb[:, s, :43, :], in_=src[:, :43, :]), writes=[('wb', s)], dma='wb%d' % s)
                    S.op('pool', lambda e, s=s, src=src: e.dma_start(out=wb[:, s, 43:, :], in_=src[:, 43:, :]), writes=[('wb', s)], dma='wb%d' % s)
                    b = nb()
                    for kc in range(FKC):
                        S.op('pe', lambda e, kc=kc, s=s, b=b: e.matmul(ps[:, b, :], lhsT=wb[:, s, kc, :], rhs=aT[:, kc, :], start=(kc == 0), stop=(kc == FKC - 1)),
                             reads=[('wb', s), 'aT'], writes=[('ps', b)])
                    pre_store((po, sqp), b, ct, tt, ok)
            S.flush()
        if True:
            with ExitStack() as st:
                epilogue(st, (6, 7), G_FFNPOST, lambda c, t0: x1_d[c, :, t0:t0 + 512], x2_d, G_PLEPRE, True)
                S.flush()
            with ExitStack() as st:
                hT3 = sbt(st, "hT3", [128, KC, T], BF16)
                load_h(hT3)
                wb = sbt(st, "wb", [128, 2, KC, 256], BF16)
                wp = sbt(st, "wp", [128, 2, 2, 256], BF16)
                pb = sbt(st, "pb", [128, 2, T], BF16)
                sg = sbt(st, "sg", [128, 2, 512], F32)
                pl = sbt(st, "pl", [128, 2, 512], F32)
                po = sbt(st, "po", [128, 2, 512], F32)
                sqp = sbt(st, "sqp", [128, 2, 512], BF16)
                S.op('pool', lambda e: e.dma_start(out=pb[:], in_=pT.rearrange("(k p) t -> p k t", p=128)), writes=['pb'], dma='c')
                h3rd = [('hN', c, tt) for c in range(KC) for tt in range(2)]
                ok = [0]
                k = [0]
                for w0 in range(0, D, 256):
                    s = load_w(wb, w_pg, 0, KC, w0, 256)
                    S.op('pool', lambda e, s=s, w0=w0: e.dma_start(out=wp[:, s, :, :], in_=w_ple[:, w0:w0 + 256].rearrange("(k p) c -> p k c", p=128)),
                         writes=[('wp', s)], dma='wp%d' % s)
                    for cs_ in range(2):
                        ct = w0 // 128 + cs_
                        for tt in range(2):
                            t0 = tt * 512
                            bg = nb(); bp = nb()
                            mm_fm(wb, s, KC, cs_, lambda kc, t0=t0: hT3[:, kc, t0:t0 + 512], 512, bg, h3rd)
                            for kc in range(2):
                                S.op('pe', lambda e, kc=kc, s=s, cs_=cs_, bp=bp, t0=t0: e.matmul(ps[:, bp, :], lhsT=wp[:, s, kc, cs_ * 128:(cs_ + 1) * 128], rhs=pb[:, kc, t0:t0 + 512],
                                                                                         start=(kc == 0), stop=(kc == 1)),
                                     reads=[('wp', s), 'pb'], writes=[('ps', bp)])
                            o = k[0] % 2
                            k[0] += 1
                            S.op('act', lambda e, bg=bg, o=o: e.activation(out=sg[:, o, :], in_=ps[:, bg, :], func=AF.Sigmoid), reads=[('ps', bg)], writes=[('sg', o)])
                            S.op('dve', lambda e, bp=bp, o=o: e.tensor_tensor(out=pl[:, o, :], in0=sg[:, o, :], in1=ps[:, bp, :], op=ALU.mult),
                                 reads=[('sg', o), ('ps', bp)], writes=[('pl', o)])
                            o2 = ok[0] % 2
                            ok[0] += 1
                            S.op('act', lambda e, o=o, o2=o2: e.activation(out=sqp[:, o2, :], in_=pl[:, o, :], func=AF.Square), reads=[('pl', o)], writes=[('sqp', o2)])
                            S.op('pe', lambda e, o2=o2, ct=ct, tt=tt: e.matmul(ps[:, 6 + tt, :], lhsT=ones, rhs=sqp[:, o2, :], start=(ct == 0), stop=(ct == KC - 1)),
                                 reads=[('sqp', o2), 'cst'], writes=[('ps', 6 + tt)])
                            S.op('sp', lambda e, o=o, ct=ct, tt=tt: e.dma_start(out=pre_d[ct, :, tt * 512:(tt + 1) * 512], in_=pl[:, o, :]), reads=[('pl', o)], dma='po%d' % o)
                S.flush()
        with ExitStack() as st:
            epilogue(st, (6, 7), G_PLEPOST, lambda c, t0: x2_d[c, :, t0:t0 + 512], None, None, None, final_out=outT)
            S.flush()
    return nc


def _bias(valid, reps=4):
    b = np.where(valid, 0.0, NEG).astype(np.float32)
    return np.tile(b, (1, reps)).astype(ml_dtypes.bfloat16)


def _host_consts(core):
    bf = ml_dtypes.bfloat16
    start = core * T
    c = {}
    half = 64
    inv = (10000.0 ** (-np.arange(half, dtype=np.float32) / half)).astype(np.float32)

    def cs(pos):
        ang = pos.astype(np.float32)[None, :] * np.concatenate([inv, inv])[:, None]
        co = np.cos(ang).astype(np.float32)
        si = np.sin(ang).astype(np.float32)
        si[:half] *= -1.0
        return np.stack([co, si], axis=1).astype(np.float32)
    c["cs_full"] = cs(np.arange(S_ALL))
    c["cs_ext"] = cs(np.arange(start - HALO, start + T))
    ident = np.eye(128, dtype=np.float32)
    swp = np.zeros((128, 128), np.float32)
    for m in range(128):
        swp[(m + 64) % 128, m] = 1.0
    c["consts"] = np.stack([ident, swp, np.ones((128, 128), np.float32)], axis=1).astype(bf)
    keys = np.arange(S_ALL)
    c["xsel"] = (keys[None, :] // 64 == np.arange(128)[:, None]).astype(np.float32).astype(bf)
    n = np.arange(512)
    m = np.arange(128)
    ov = np.clip(np.minimum(n[:, None] * 16 + 32, m[None, :] * 64 + 64) - np.maximum(n[:, None] * 16, m[None, :] * 64), 0, None) / 32.0
    ov[511] = 0.0
    c["ovm"] = ov.reshape(4, 128, 128).transpose(1, 0, 2).astype(np.float32).astype(bf)
    gs = np.zeros((48, 48, 128), np.float32)
    for k in range(48):
        gs[k, k, :] = 1.0
    c["gsel"] = gs.astype(bf)
    bc = np.zeros((8, 128, 4, 512), bf)
    af = np.zeros((8, 128, 128), np.float32)
    bw = np.zeros((8, 128, 5, 512), bf)
    for qb in range(8):
        pos = start + qb * 128 + np.arange(128)
        for j in range(4):
            nn = j * 128 + np.arange(128)
            valid = (nn[:, None] * 16 + 31 <= pos[None, :]) & (nn[:, None] < 511)
            bc[qb, :, j, :] = _bias(valid)
        cur = pos // 64
        sblk = np.arange(128)
        forced = (sblk[None] == 0) | (sblk[None] == cur[:, None]) | (sblk[None] == cur[:, None] - 1)
        sval = sblk[None] * 64 <= pos[:, None]
        af[qb] = np.where(sval, 1000.0 * forced, -1e30).astype(np.float32)
        for i in range(5):
            kp = start + (qb - 4 + i) * 128 + np.arange(128)
            valid = (kp[:, None] <= pos[None, :]) & (pos[None, :] - kp[:, None] < 512) & (kp[:, None] >= 0)
            bw[qb, :, i, :] = _bias(valid)
    c["biasC"] = bc
    c["addF"] = af
    c["biasW"] = bw
    dg = np.zeros((128, 8, 512), bf)
    kk = np.arange(128)
    dg[:, core, :] = _bias(kk[:, None] <= kk[None, :])
    c["diagb"] = dg
    bd = np.zeros((128, 22, 512), bf)

    def dil(r, nq, a0, ks, nk):
        qpos = start + r * (a0 + np.arange(nq))
        kpos = start + r * (ks + np.arange(nk))
        valid = (kpos[:, None] >= 0) & (kpos[:, None] <= qpos[None, :]) & (qpos[None, :] - kpos[:, None] <= 128 * r)
        t = np.full((128, 4 * nq), NEG, np.float32)
        t[:nk] = np.tile(np.where(valid, 0.0, NEG), (1, 4))
        out = np.zeros((128, 512), np.float32)
        out[:, :4 * nq] = t
        return out.astype(bf)
    for blk in range(8):
        bd[:, blk * 2] = dil(1, 128, blk * 128, blk * 128 - 128, 128)
        bd[:, blk * 2 + 1] = dil(1, 128, blk * 128, blk * 128, 128)
    for blk in range(2):
        bd[:, 16 + blk * 2] = dil(4, 128, blk * 128, blk * 128 - 128, 128)
        bd[:, 16 + blk * 2 + 1] = dil(4, 128, blk * 128, blk * 128, 128)
    bd[:, 20] = dil(16, 64, 0, -128, 128)
    bd[:, 21] = dil(16, 64, 0, 0, 64)
    c["biasD"] = bd
    return c


_NC_CACHE = {}


def kernel(**inputs):
    f = lambda k: np.asarray(inputs[k], dtype=np.float32)
    x = f("x")[0]
    xT = np.ascontiguousarray(x.T)
    p = f("p")[0, 0]
    gl = lambda v: np.ascontiguousarray(v.reshape(32, 128).T)
    gains = np.concatenate([gl(f(k)[0]) for k in ("g_mix_pre", "g_mix_post", "g_ffn_pre", "g_ffn_post", "g_ple_pre", "g_ple_post")]
                           + [np.zeros((128, 64), np.float32)], axis=1)
    shared = {
        "xT_full": xT, "gains": np.ascontiguousarray(gains), "w_in": f("w_in")[0],
        "peT_k": np.ascontiguousarray(f("pe_ck")[0].T), "peT_v": np.ascontiguousarray(f("pe_cv")[0].T),
        "w_ck1": f("w_ck1")[0], "w_ck2": f("w_ck2")[0], "w_cv1": f("w_cv1")[0], "w_cv2": f("w_cv2")[0],
        "w_a": f("w_a")[0], "w_b": f("w_b")[0], "w_out": f("w_out")[0], "w_gu": f("w_gu")[0],
        "w_down": f("w_down")[0], "w_pg": f("w_ple_gate")[0], "w_ple": f("w_ple")[0],
    }
    in_maps = []
    for c in range(NCORES):
        start = c * T
        ext = np.zeros((D, EXT), np.float32)
        lo = max(0, start - HALO)
        ext[:, EXT - (start + T - lo):] = xT[:, lo:start + T]
        m = dict(shared)
        m["xT_ext"] = ext
        m["pT"] = np.ascontiguousarray(p[start:start + T].T)
        m.update(_host_consts(c))
        in_maps.append(m)
    if "nc" not in _NC_CACHE:
        _NC_CACHE["nc"] = build()
    _ncr = int(os.environ.get("MK_NCORES", NCORES))
    _c0 = int(os.environ.get("MK_CORE0", 0))
    if _ncr != NCORES:
        res = run_bass_kernel_spmd(_NC_CACHE["nc"], in_maps[_c0:_c0 + _ncr], core_ids=list(range(_ncr)))
        _NC_CACHE["res"] = res
        return None
    res = run_bass_kernel_spmd(_NC_CACHE["nc"], in_maps, core_ids=list(range(NCORES)))
    if _DBG:
        _NC_CACHE["res"] = res
    out = np.concatenate([np.asarray(r["outT"]).T for r in res.results], axis=0)
    return out.reshape(1, S_ALL, D).astype(np.float32)
```

```python
import numpy as np
import ml_dtypes
from contextlib import ExitStack
import concourse.bass as bass
import concourse.mybir as mybir
from concourse.bass_utils import run_bass_kernel_spmd

F32 = mybir.dt.float32
BF16 = mybir.dt.bfloat16
AF = mybir.ActivationFunctionType
ALU = mybir.AluOpType

NCORES = 8
S_ALL = 8192
D = 4096
KC = 32
T = 1024
HALO = 2048
EXT = HALO + T
DFF = 11008
FKC = 86
SCALE = 128 ** -0.5
EPS = 1e-6
NEG = -30000.0
C_QA, C_KA, C_VA, C_QB, C_KC, C_VC, C_KSL, C_VSL, C_KWN, C_VWN, C_GN, C_GM = (
    0, 1536, 3072, 4608, 6656, 7168, 7680, 8192, 8704, 9216, 9728, 9776)

ENGS = ("pe", "act", "dve", "pool", "sp")


import os
_STOP = int(os.environ.get("MK_STOP", "1000"))
_DBG = [x for x in os.environ.get("MK_DBG", "").split(",") if x]


class StopBuild(Exception):
    pass


class Sched:
    def __init__(self, nc, st):
        self.nc = nc
        self.esem = {e: st.enter_context(nc.semaphore("s_" + e)) for e in ENGS}
        self.dsem = {}
        self.st = st
        self.seq = {e: 0 for e in ENGS}
        self.nsig = {e: 0 for e in ENGS}
        self.known = {e: {} for e in ENGS}
        self.dcount = {}
        self._reset()

    def _reset(self):
        self.ops = {e: [] for e in ENGS}
        self.lastw = {}
        self.readers = {}
        self.signal = {e: set() for e in ENGS}
        self.dma_issuer = {}

    def _need(self, eng, seq, tok, waits):
        kind, src, val = tok
        if kind == 'E':
            if src == eng:
                if eng == 'pe' or seq - val > 2:
                    return
            k = ('E', src)
        else:
            k = ('D', src)
        if self.known[eng].get(k, -1) >= val:
            return
        if val > waits.get(k, -1):
            waits[k] = val

    def op(self, eng, fn, reads=(), writes=(), dma=None):
        if getattr(self, 'stopped', False):
            return
        seq = self.seq[eng]
        self.seq[eng] += 1
        waits = {}
        for r in reads:
            t = self.lastw.get(r)
            if t is not None:
                self._need(eng, seq, t, waits)
        for w in writes:
            t = self.lastw.get(w)
            if t is not None:
                self._need(eng, seq, t, waits)
            for t in self.readers.get(w, ()):
                self._need(eng, seq, t, waits)
        for k, v in waits.items():
            self.known[eng][k] = v
            if k[0] == 'E':
                self.signal[k[1]].add(v)
        if dma is not None:
            if dma not in self.dsem:
                self.dsem[dma] = self.st.enter_context(self.nc.semaphore("d_" + str(dma)))
            c = self.dcount.get(dma, 0) + 1
            self.dcount[dma] = c
            tok = ('D', dma, c)
            self.dma_issuer.setdefault(eng, {})[dma] = c
        else:
            tok = ('E', eng, seq)
        for r in reads:
            self.readers.setdefault(r, []).append(tok)
        for w in writes:
            self.lastw[w] = tok
            self.readers[w] = []
        self.ops[eng].append((seq, fn, waits, dma))

    def flush(self):
        nc = self.nc
        if getattr(self, 'stopped', False):
            self._reset()
            return
        last = {}
        for e in ENGS:
            cs = [o[0] for o in self.ops[e] if o[3] is None and o[1] is not None]
            if cs:
                last[e] = cs[-1]
                self.signal[e].add(cs[-1])
        for e in ENGS:
            waits = {}
            for e2, s in last.items():
                if e2 != e and self.known[e].get(('E', e2), -1) < s:
                    waits[('E', e2)] = s
                    self.known[e][('E', e2)] = s
            for c, v in self.dcount.items():
                if self.known[e].get(('D', c), -1) < v:
                    waits[('D', c)] = v
                    self.known[e][('D', c)] = v
            self.ops[e].append((None, None, waits, None))
        rank = {}
        for e in ENGS:
            srt = sorted(self.signal[e])
            rank[e] = {s: self.nsig[e] + i + 1 for i, s in enumerate(srt)}
            self.nsig[e] += len(srt)
        esem, dsem = self.esem, self.dsem

        def run(e, engobj):
            sig = self.signal[e]
            for (seq, fn, waits, dma) in self.ops[e]:
                for k, v in waits.items():
                    if k[0] == 'E':
                        engobj.wait_ge(esem[k[1]], rank[k[1]][v])
                    else:
                        engobj.wait_ge(dsem[k[1]], 16 * v)
                if fn is None:
                    continue
                ins = fn(engobj)
                if dma is not None:
                    ins.then_inc(dsem[dma], 16)
                elif seq in sig:
                    ins.then_inc(esem[e], 1)

        with nc.Block() as block:
            @block.tensor
            def _(eng):
                run('pe', eng)

            @block.scalar
            def _(eng):
                run('act', eng)

            @block.vector
            def _(eng):
                run('dve', eng)

            @block.gpsimd
            def _(eng):
                run('pool', eng)

            @block.sync
            def _(eng):
                run('sp', eng)
        self._reset()
        self.nflush = getattr(self, 'nflush', 0) + 1
        if self.nflush >= _STOP:
            self.stopped = True


def build():
    nc = bass.Bass("TRN2", target_bir_lowering=False)

    def din(name, shape, dt=F32):
        return nc.dram_tensor(name, list(shape), dt, kind="ExternalInput").ap()

    def dscr(name, shape, dt):
        return nc.dram_tensor(name, list(shape), dt, kind=("ExternalOutput" if name in _DBG else "Internal")).ap()

    xT_full = din("xT_full", [D, S_ALL])
    xT_ext = din("xT_ext", [D, EXT])
    pT = din("pT", [256, T])
    gains = din("gains", [128, 8 * 32])
    w_in = din("w_in", [D, 17968])
    peT_k = din("peT_k", [128, 32]); peT_v = din("peT_v", [128, 32])
    w_ck1 = din("w_ck1", [4096, 128]); w_ck2 = din("w_ck2", [128, 128])
    w_cv1 = din("w_cv1", [4096, 128]); w_cv2 = din("w_cv2", [128, 128])
    w_a = din("w_a", [512, D]); w_b = din("w_b", [2048, D]); w_out = din("w_out", [D, D])
    w_gu = din("w_gu", [D, 2 * DFF]); w_down = din("w_down", [DFF, D])
    w_pg = din("w_pg", [D, D]); w_ple = din("w_ple", [256, D])
    cs_full = din("cs_full", [128, 2, S_ALL])
    cs_ext = din("cs_ext", [128, 2, EXT])
    consts = din("consts", [128, 3, 128], BF16)
    xsel = din("xsel", [128, S_ALL], BF16)
    ovm = din("ovm", [128, 4, 128], BF16)
    gsel = din("gsel", [48, 48, 128], BF16)
    biasC = din("biasC", [8, 128, 4, 512], BF16)
    addF = din("addF", [8, 128, 128])
    diagb = din("diagb", [128, 8, 512], BF16)
    biasW = din("biasW", [8, 128, 5, 512], BF16)
    biasD = din("biasD", [128, 22, 512], BF16)
    outT = nc.dram_tensor("outT", [D, T], F32, kind="ExternalOutput").ap()

    kcT_d = dscr("kcT_d", [8, 128, S_ALL], BF16)
    kslT_d = dscr("kslT_d", [4, 128, S_ALL], BF16)
    vsl_d = dscr("vsl_d", [S_ALL, 512], BF16)
    kaT_d = dscr("kaT_d", [12, 128, EXT], BF16)
    va_d = dscr("va_d", [EXT, 1536], BF16)
    kwnT_d = dscr("kwnT_d", [4, 128, EXT], BF16)
    vwn_d = dscr("vwn_d", [EXT, 512], BF16)
    gmix_d = dscr("gmix_d", [64, 128, T], BF16)
    pre_d = dscr("pre_d", [32, 128, T], F32)
    x1_d = dscr("x1_d", [32, 128, T], F32)
    x2_d = dscr("x2_d", [32, 128, T], F32)
    act_d = dscr("act_d", [FKC, 128, T], BF16)
    qaT_d = dscr("qaT_d", [12, 128, T], BF16)
    qrT_d = dscr("qrT_d", [16, 128, T], BF16)
    qoT_d = dscr("qoT_d", [16, 128, T], BF16)
    h_d = dscr("h_d", [32, 128, T], BF16)
    ya_d = dscr("ya_d", [4, 128, T], BF16)
    yb_d = dscr("yb_d", [16, 128, T], BF16)

    with ExitStack() as top:
      try:
        S = Sched(nc, top)
        _build_body(nc, top, S, locals())
      except StopBuild:
        pass
    return nc


def _build_body(nc, top, S, L):
    globals().update({k: v for k, v in L.items() if k not in ('nc', 'top', 'S')})
    if True:
        _uid = [0]

        def sbt(st, name, shape, dt):
            _uid[0] += 1
            return st.enter_context(nc.sbuf_tensor("%s_%d" % (name, _uid[0]), list(shape), dt))
        ps = top.enter_context(nc.psum_tensor("ps", [128, 8, 512], F32))
        cst = sbt(top, "cst", [128, 3, 128], BF16)
        ident, swp, ones = cst[:, 0, :], cst[:, 1, :], cst[:, 2, :]
        gn = sbt(top, "gn", [128, 8 * 32], F32)
        kc_c = sbt(top, "kc_c", [128, 4, 512], BF16)
        vc_c = sbt(top, "vc_c", [128, 4, 4, 128], BF16)
        gsT = sbt(top, "gsT", [48, T], BF16)
        rs2p = sbt(top, "rs2p", [128, 2, 512], F32)

        S.op('sp', lambda e: e.dma_start(out=cst[:], in_=consts), writes=['cst'], dma='c')
        S.op('sp', lambda e: e.dma_start(out=gn[:], in_=gains), writes=['cst'], dma='c')
        epsb = sbt(top, "epsb", [128, 1], F32)
        S.op('pool', lambda e: e.memset(epsb[:], EPS), writes=['epsb'])
        S.op('pool', lambda e: e.memset(vc_c[:], 0.0), writes=['vc_c'])
        S.op('pool', lambda e: e.memset(kc_c[:], 0.0), writes=['kc_c'])
        G_MIXPRE, G_MIXPOST, G_FFNPRE, G_FFNPOST, G_PLEPRE, G_PLEPOST = range(6)

        def gcol(gi, c):
            return gn[:, gi * 32 + c: gi * 32 + c + 1]

        bank_rr = [0]

        def nb(n=4, base=0):
            b = base + bank_rr[0] % n
            bank_rr[0] += 1
            return b

        xT_full_v = xT_full.rearrange("(c p) t -> p c t", p=128)
        xT_ext_v = xT_ext.rearrange("(c p) t -> p c t", p=128)

        def norm_tile(st_, xs, hT, src_v, t0, gi, sq, rs):
            for q in range(4):
                S.op('sp', lambda e, q=q: e.dma_start(out=xs[:, 8 * q:8 * q + 8, :], in_=src_v[:, 8 * q:8 * q + 8, t0:t0 + 512]),
                     writes=[('xs', q)], dma='xs%d' % q)
            b = 7
            for c in range(KC):
                S.op('act', lambda e, c=c: e.activation(out=sq[:, c % 2, :], in_=xs[:, c, :], func=AF.Square),
                     reads=[('xs', c // 8)], writes=[('sq', c % 2)])
                S.op('pe', lambda e, c=c: e.matmul(ps[:, b, :], lhsT=ones, rhs=sq[:, c % 2, :], start=(c == 0), stop=(c == KC - 1)),
                     reads=[('sq', c % 2), 'cst'], writes=[('ps', b)])
            S.op('act', lambda e: e.activation(out=rs[:], in_=ps[:, b, :], func=AF.Sqrt, scale=1.0 / D, bias=epsb[:]),
                 reads=[('ps', b), 'epsb'], writes=['rs'])
            S.op('dve', lambda e: e.reciprocal(out=rs[:], in_=rs[:]), reads=['rs'], writes=['rs'])
            for c in range(KC):
                S.op('dve', lambda e, c=c: e.scalar_tensor_tensor(out=hT[:, c, :], in0=xs[:, c, :], scalar=gcol(gi, c), in1=rs[:], op0=ALU.mult, op1=ALU.mult),
                     reads=[('xs', c // 8), 'rs', 'cst'], writes=[('hT', id(hT), c)])

        wslot = [0]

        def load_w(wb, w_ap, r0, kc_n, c0, ncols):
            s = wslot[0] % 2
            wslot[0] += 1
            src = w_ap[r0:r0 + kc_n * 128, c0:c0 + ncols].rearrange("(k p) c -> p k c", p=128)
            half = max(1, kc_n // 2)
            for h0 in range(0, kc_n, half):
                h1 = min(kc_n, h0 + half)
                S.op('pool', lambda e, s=s, h0=h0, h1=h1: e.dma_start(out=wb[:, s, h0:h1, :ncols], in_=src[:, h0:h1, :]),
                     writes=[('wb', s)], dma='wb%d' % s)
            return s

        def mm_fm(wb, s, kc_n, csub, act_fn, n, b, extra_reads=()):
            for kc in range(kc_n):
                S.op('pe', lambda e, kc=kc: e.matmul(ps[:, b, :n], lhsT=wb[:, s, kc, csub * 128:(csub + 1) * 128], rhs=act_fn(kc),
                                                    start=(kc == 0), stop=(kc == kc_n - 1)),
                     reads=[('wb', s)] + list(extra_reads), writes=[('ps', b)])

        def rope_store(b, n, cs_t, t_off, dst_fn, tmpq, tmp1, key, extra_reads=()):
            i = key % 2
            S.op('dve', lambda e: e.tensor_copy(out=tmpq[:, i, :n], in_=ps[:, b, :n]), reads=[('ps', b)] + list(extra_reads), writes=[('tq', i)])
            b2 = nb()
            S.op('pe', lambda e: e.matmul(ps[:, b2, :n], lhsT=swp, rhs=tmpq[:, i, :n], start=True, stop=True),
                 reads=[('tq', i), 'cst'], writes=[('ps', b2)])
            S.op('dve', lambda e: e.tensor_tensor(out=tmp1[:, i, :n], in0=ps[:, b, :n], in1=cs_t[:, 0, t_off:t_off + n], op=ALU.mult),
                 reads=[('ps', b), 'cs'] + list(extra_reads), writes=[('t1', i)])
            S.op('dve', lambda e: e.tensor_tensor(out=tmp1[:, 2 + i, :n], in0=ps[:, b2, :n], in1=cs_t[:, 1, t_off:t_off + n], op=ALU.mult),
                 reads=[('ps', b2), 'cs'] + list(extra_reads), writes=[('t2', i)])
            dst, wkeys = dst_fn()
            S.op('dve', lambda e: e.tensor_tensor(out=dst, in0=tmp1[:, i, :n], in1=tmp1[:, 2 + i, :n], op=ALU.add),
                 reads=[('t1', i), ('t2', i)], writes=wkeys)

        for pss in range(2):
            with ExitStack() as st:
                wres = sbt(st, "wres", [128, KC, 1024], BF16)
                xsb = sbt(st, "xsb", [128, KC, 512], BF16)
                hT2 = [sbt(st, "hTa", [128, KC, 512], BF16), sbt(st, "hTb", [128, KC, 512], BF16)]
                sq = sbt(st, "sq", [128, 2, 512], BF16)
                rs = sbt(st, "rs", [128, 512], F32)
                ob = sbt(st, "ob", [128, 4, 512], BF16)
                tmpq = sbt(st, "tmpq", [128, 2, 512], BF16)
                tmp1 = sbt(st, "tmp1", [128, 4, 512], F32)
                cs_t = sbt(st, "cs_t", [128, 2, 2, 512], F32)
                c0 = C_KC if pss == 0 else C_KSL
                srcw = w_in[:, c0:c0 + 1024].rearrange("(k p) c -> p k c", p=128)
                for q in range(4):
                    S.op('pool', lambda e, q=q: e.dma_start(out=wres[:, 8 * q:8 * q + 8, :], in_=srcw[:, 8 * q:8 * q + 8, :]),
                         writes=['wres'], dma='wres')
                NT = S_ALL // 512

                def normA(tt):
                    t0 = tt * 512
                    for q in range(4):
                        S.op('pool', lambda e, q=q: e.dma_start(out=xsb[:, 8 * q:8 * q + 8, :], in_=xT_full_v[:, 8 * q:8 * q + 8, t0:t0 + 512]),
                             writes=[('xs', q)], dma='xs%d' % q)
                    for c in range(KC):
                        S.op('act', lambda e, c=c: e.activation(out=sq[:, c % 2, :], in_=xsb[:, c, :], func=AF.Square),
                             reads=[('xs', c // 8)], writes=[('sq', c % 2)])
                        S.op('pe', lambda e, c=c: e.matmul(ps[:, 7, :], lhsT=ones, rhs=sq[:, c % 2, :], start=(c == 0), stop=(c == KC - 1)),
                             reads=[('sq', c % 2), 'cst'], writes=[('ps', 7)])
                    S.op('act', lambda e: e.activation(out=rs[:], in_=ps[:, 7, :], func=AF.Sqrt, scale=1.0 / D, bias=epsb[:]),
                         reads=[('ps', 7), 'epsb'], writes=['rs'])
                    S.op('dve', lambda e: e.reciprocal(out=rs[:], in_=rs[:]), reads=['rs'], writes=['rs'])

                def normB(tt):
                    h = hT2[tt % 2]
                    for c in range(KC):
                        S.op('dve', lambda e, c=c: e.scalar_tensor_tensor(out=h[:, c, :], in0=xsb[:, c, :], scalar=gcol(G_MIXPRE, c), in1=rs[:], op0=ALU.mult, op1=ALU.mult),
                             reads=[('xs', c // 8), 'rs', 'cst'], writes=[('hT', tt % 2, c)])

                def unit_fm(tt, ct):
                    hT = hT2[tt % 2]
                    hreads = [('hT', tt % 2, c) for c in range(KC)]
                    t0 = tt * 512
                    b = nb()
                    for kc in range(KC):
                        S.op('pe', lambda e, kc=kc: e.matmul(ps[:, b, :], lhsT=wres[:, kc, ct * 128:(ct + 1) * 128], rhs=hT[:, kc, :],
                                                             start=(kc == 0), stop=(kc == KC - 1)),
                             reads=['wres'] + (hreads if kc == 0 else []), writes=[('ps', b)])
                    o = ct % 4
                    if pss == 0:
                        S.op('act', lambda e: e.copy(out=ob[:, o, :], in_=ps[:, b, :]), reads=[('ps', b)], writes=[('ob', o)])
                        S.op('sp', lambda e: e.dma_start(out=kcT_d[ct, :, t0:t0 + 512], in_=ob[:, o, :]), reads=[('ob', o)], dma='ob%d' % o)
                    else:
                        rope_store(b, 512, cs_t[:, tt % 2], 0, lambda: (ob[:, o, :], [('ob', o)]), tmpq, tmp1, ct, [('cs', tt % 2)])
                        S.op('sp', lambda e: e.dma_start(out=kslT_d[ct, :, t0:t0 + 512], in_=ob[:, o, :]), reads=[('ob', o)], dma='ob%d' % o)

                def unit_tm(tt, tk):
                    hT = hT2[tt % 2]
                    hreads = [('hT', tt % 2, c) for c in range(KC)]
                    t0 = tt * 512
                    b = nb()
                    for kc in range(KC):
                        S.op('pe', lambda e, kc=kc: e.matmul(ps[:, b, :], lhsT=hT[:, kc, tk * 128:(tk + 1) * 128], rhs=wres[:, kc, 512:1024],
                                                             start=(kc == 0), stop=(kc == KC - 1)),
                             reads=['wres'] + (hreads if kc == 0 else []), writes=[('ps', b)])
                    S.op('act', lambda e: e.copy(out=ob[:, tk, :], in_=ps[:, b, :]), reads=[('ps', b)], writes=[('ob', tk)])
                    S.op('sp', lambda e: e.dma_start(out=vsl_d[t0 + tk * 128:t0 + (tk + 1) * 128, :], in_=ob[:, tk, :]), reads=[('ob', tk)], dma='ob%d' % tk)

                normA(0)
                normB(0)
                for tt in range(NT):
                    if pss == 1:
                        S.op('sp', lambda e, tt=tt: e.dma_start(out=cs_t[:, tt % 2], in_=cs_full[:, :, tt * 512:(tt + 1) * 512]), writes=[('cs', tt % 2)], dma='cs%d' % (tt % 2))
                        units = [(unit_fm, ct) for ct in range(4)] + [(unit_tm, tk) for tk in range(4)]
                    else:
                        units = [(unit_fm, ct) for ct in range(8)]
                    for fn_, a_ in units[:4]:
                        fn_(tt, a_)
                    if tt + 1 < NT:
                        normA(tt + 1)
                        normB(tt + 1)
                    for fn_, a_ in units[4:]:
                        fn_(tt, a_)
                S.flush()

        with ExitStack() as st:
            kin = sbt(st, "kin", [128, 2, S_ALL], BF16)
            w1 = sbt(st, "w1", [128, 2, 32, 128], BF16)
            w2 = sbt(st, "w2", [128, 2, 128], BF16)
            pe_b = sbt(st, "pe_b", [128, 2, 32], BF16)
            pe2 = sbt(st, "pe2", [128, 2, 32, 2], BF16)
            cb = sbt(st, "cb", [128, 2], F32)
            xg = sbt(st, "xg", [128, 2, 512], F32)
            tg = sbt(st, "tg", [128, 2, 512], F32)
            ge = sbt(st, "ge", [128, 2, 512], BF16)
            for kv, (wa1, wa2, pea) in enumerate(((w_ck1, w_ck2, peT_k), (w_cv1, w_cv2, peT_v))):
                S.op('pool', lambda e, kv=kv, wa1=wa1: e.dma_start(out=w1[:, kv], in_=wa1.rearrange("(l d) h -> d l h", d=128)), writes=['w1'], dma='c')
                S.op('pool', lambda e, kv=kv, wa2=wa2: e.dma_start(out=w2[:, kv], in_=wa2), writes=['w1'], dma='c')
                S.op('pool', lambda e, kv=kv, pea=pea: e.dma_start(out=pe_b[:, kv], in_=pea), writes=['w1'], dma='c')
            for j2 in range(2):
                S.op('dve', lambda e, j2=j2: e.tensor_copy(out=pe2[:, :, :, j2], in_=pe_b[:]), reads=['w1'], writes=[('pe2', j2)])
            for kv in range(2):
                b = nb()
                for l in range(32):
                    S.op('pe', lambda e, l=l, kv=kv, b=b: e.matmul(ps[:, b, 0:2], lhsT=w1[:, kv, l, :], rhs=pe2[:, kv, l, :], start=(l == 0), stop=(l == 31)),
                         reads=['w1', ('pe2', 0), ('pe2', 1)], writes=[('ps', b)])
                S.op('dve', lambda e, kv=kv, b=b: e.tensor_copy(out=cb[:, kv:kv + 1], in_=ps[:, b, 0:1]), reads=[('ps', b)], writes=[('cb', kv)])
            NCMP = 511
            for g in range(4):
                for kv in range(2):
                    i = (g * 2 + kv) % 2
                    S.op('sp', lambda e, g=g, kv=kv, i=i: e.dma_start(out=kin[:, i, :], in_=kcT_d[kv * 4 + g]), writes=[('kin', i)], dma='kin%d' % i)
                    b = nb()
                    for l in range(32):
                        S.op('pe', lambda e, l=l, kv=kv, i=i, b=b: e.matmul(ps[:, b, :NCMP], lhsT=w1[:, kv, l, :], rhs=kin[:, i, l:l + 16 * (NCMP - 1) + 1:16],
                                                                      start=(l == 0), stop=(l == 31)),
                             reads=['w1', ('kin', i)], writes=[('ps', b)])
                    S.op('dve', lambda e, kv=kv, i=i, b=b: e.tensor_scalar(out=xg[:, i, :NCMP], in0=ps[:, b, :NCMP], scalar1=cb[:, kv:kv + 1], scalar2=None, op0=ALU.add),
                         reads=[('ps', b), ('cb', kv)], writes=[('xg', i)])
                    S.op('dve', lambda e, i=i: e.tensor_tensor(out=tg[:, i, :NCMP], in0=xg[:, i, :NCMP], in1=xg[:, i, :NCMP], op=ALU.mult),
                         reads=[('xg', i)], writes=[('tg', i)])
                    S.op('dve', lambda e, i=i: e.tensor_scalar(out=tg[:, i, :NCMP], in0=tg[:, i, :NCMP], scalar1=0.044715, scalar2=1.0, op0=ALU.mult, op1=ALU.add),
                         reads=[('tg', i)], writes=[('tg', i)])
                    S.op('dve', lambda e, i=i: e.tensor_tensor(out=tg[:, i, :NCMP], in0=tg[:, i, :NCMP], in1=xg[:, i, :NCMP], op=ALU.mult),
                         reads=[('tg', i), ('xg', i)], writes=[('tg', i)])
                    S.op('act', lambda e, i=i: e.activation(out=tg[:, i, :NCMP], in_=tg[:, i, :NCMP], func=AF.Sigmoid, scale=1.5957691216057308),
                         reads=[('tg', i)], writes=[('tg', i)])
                    S.op('dve', lambda e, i=i: e.tensor_tensor(out=ge[:, i, :NCMP], in0=tg[:, i, :NCMP], in1=xg[:, i, :NCMP], op=ALU.mult),
                         reads=[('tg', i), ('xg', i)], writes=[('ge', i)])
                    if kv == 0:
                        b2 = nb()
                        S.op('pe', lambda e, i=i, b2=b2: e.matmul(ps[:, b2, :NCMP], lhsT=w2[:, 0, :], rhs=ge[:, i, :NCMP], start=True, stop=True),
                             reads=['w1', ('ge', i)], writes=[('ps', b2)])
                        S.op('act', lambda e, g=g, b2=b2: e.copy(out=kc_c[:, g, :NCMP], in_=ps[:, b2, :NCMP]), reads=[('ps', b2)], writes=['kc_c'])
                    else:
                        for j in range(4):
                            m = 128 if j < 3 else 127
                            b2 = nb()
                            S.op('pe', lambda e, i=i, j=j, m=m, b2=b2: e.matmul(ps[:m, b2, :128], lhsT=ge[:, i, j * 128:j * 128 + m], rhs=w2[:, 1, :], start=True, stop=True),
                                 reads=['w1', ('ge', i)], writes=[('ps', b2)])
                            S.op('act', lambda e, g=g, j=j, m=m, b2=b2: e.copy(out=vc_c[:m, g, j, :], in_=ps[:m, b2, :128]), reads=[('ps', b2)], writes=['vc_c'])
            S.flush()

        for half in range(3):
            with ExitStack() as st:
                hT3 = [sbt(st, "hT3_%d" % i, [128, KC, 512], BF16) for i in range(2)]
                xs = sbt(st, "xs", [128, KC, 512], F32)
                sq = sbt(st, "sq", [128, 2, 512], BF16)
                rs = sbt(st, "rs", [128, 512], F32)
                wb = sbt(st, "wb", [128, 2, KC, 256], BF16)
                ob = sbt(st, "ob", [128, 4, 512], BF16)
                tmpq = sbt(st, "tmpq", [128, 2, 512], BF16)
                tmp1 = sbt(st, "tmp1", [128, 4, 512], F32)
                cs_t = sbt(st, "cs_t", [128, 2, 1024], F32)
                tb = half * 1024
                S.op('sp', lambda e, tb=tb: e.dma_start(out=cs_t[:], in_=cs_ext[:, :, tb:tb + 1024]), writes=['cs'], dma='cs')
                for i in range(2):
                    norm_tile(st, xs, hT3[i], xT_ext_v, tb + i * 512, G_MIXPRE, sq, rs)
                hr = [[('hT', id(hT3[i]), c) for c in range(KC)] for i in range(2)]
                okey = [0]

                def fm_cols(c0, ncols, tiles, sink):
                    for w0 in range(0, ncols, 256):
                        wn = min(256, ncols - w0)
                        s = load_w(wb, w_in, 0, KC, c0 + w0, wn)
                        for i in tiles:
                            for cs_ in range((wn + 127) // 128):
                                mrows = min(128, wn - cs_ * 128)
                                b = nb()
                                for kc in range(KC):
                                    S.op('pe', lambda e, kc=kc, s=s, cs_=cs_, i=i, b=b, mrows=mrows: e.matmul(
                                        ps[:mrows, b, :], lhsT=wb[:, s, kc, cs_ * 128:cs_ * 128 + mrows], rhs=hT3[i][:, kc, :],
                                        start=(kc == 0), stop=(kc == KC - 1)),
                                        reads=[('wb', s)] + (hr[i] if kc == 0 else []), writes=[('ps', b)])
                                sink((w0 + cs_ * 128) // 128, i, b, mrows)

                def tm_cols(c0, ncols, tiles, dst_d, dcol0):
                    for w0 in range(0, ncols, 256):
                        s = load_w(wb, w_in, 0, KC, c0 + w0, 256)
                        for i in tiles:
                            for tk in range(4):
                                b = nb()
                                for kc in range(KC):
                                    S.op('pe', lambda e, kc=kc, s=s, i=i, tk=tk, b=b: e.matmul(
                                        ps[:, b, :256], lhsT=hT3[i][:, kc, tk * 128:(tk + 1) * 128], rhs=wb[:, s, kc, :],
                                        start=(kc == 0), stop=(kc == KC - 1)),
                                        reads=[('wb', s)] + (hr[i] if kc == 0 else []), writes=[('ps', b)])
                                o = okey[0] % 4
                                okey[0] += 1
                                S.op('act', lambda e, b=b, o=o: e.copy(out=ob[:, o, :256], in_=ps[:, b, :256]), reads=[('ps', b)], writes=[('ob', o)])
                                r0 = tb + i * 512 + tk * 128
                                S.op('sp', lambda e, o=o, r0=r0, w0=w0: e.dma_start(out=dst_d[r0:r0 + 128, dcol0 + w0:dcol0 + w0 + 256], in_=ob[:, o, :256]),
                                     reads=[('ob', o)], dma='ob%d' % o)

                def sink_rope_dram(dst_d, ct_off=0):
                    def sink(ct, i, b, mrows):
                        o = okey[0] % 4
                        okey[0] += 1
                        rope_store(b, 512, cs_t, i * 512, lambda o=o: (ob[:, o, :], [('ob', o)]), tmpq, tmp1, okey[0])
                        t0 = tb + i * 512
                        S.op('sp', lambda e, ct=ct, o=o, t0=t0: e.dma_start(out=dst_d[ct_off + ct, :, t0:t0 + 512], in_=ob[:, o, :]),
                             reads=[('ob', o)], dma='ob%d' % o)
                    return sink

                if half == 0:
                    fm_cols(C_KA + 1024, 512, range(2), sink_rope_dram(kaT_d, 8))
                    tm_cols(C_VA + 1024, 512, range(2), va_d, 1024)
                elif half == 1:
                    fm_cols(C_KA, 1024, (1,), sink_rope_dram(kaT_d))
                    fm_cols(C_KA + 1024, 512, range(2), sink_rope_dram(kaT_d, 8))
                    fm_cols(C_KWN, 512, (1,), sink_rope_dram(kwnT_d))
                    tm_cols(C_VA, 1024, (1,), va_d, 0)
                    tm_cols(C_VA + 1024, 512, range(2), va_d, 1024)
                    tm_cols(C_VWN, 512, (1,), vwn_d, 0)
                else:
                    fm_cols(C_KA, 1536, range(2), sink_rope_dram(kaT_d))
                    fm_cols(C_KWN, 512, range(2), sink_rope_dram(kwnT_d))
                    tm_cols(C_VA, 1536, range(2), va_d, 0)
                    tm_cols(C_VWN, 512, range(2), vwn_d, 0)
                if half == 2:
                    own = (0, 1)

                    def sink_q(dst_d, extra=()):
                        def sink(ct, i, b, mrows):
                            t0 = i * 512
                            o = okey[0] % 4
                            okey[0] += 1
                            rope_store(b, 512, cs_t, i * 512, lambda o=o: (ob[:, o, :], [('ob', o)]), tmpq, tmp1, okey[0], extra)
                            S.op('sp', lambda e: e.dma_start(out=dst_d[ct, :, t0:t0 + 512], in_=ob[:, o, :]), reads=[('ob', o)], dma='ob%d' % o)
                        return sink
                    fm_cols(C_QA, 1536, own, sink_q(qaT_d))

                    def sink_qb(ct, i, b, mrows):
                        t0 = i * 512
                        o = okey[0] % 4
                        okey[0] += 1
                        S.op('act', lambda e: e.copy(out=ob[:, o, :], in_=ps[:, b, :]), reads=[('ps', b)], writes=[('ob', o)])
                        S.op('sp', lambda e: e.dma_start(out=qrT_d[ct, :, t0:t0 + 512], in_=ob[:, o, :]), reads=[('ob', o)], dma='ob%d' % o)
                        sink_q(qoT_d, [('ob', o)])(ct, i, b, mrows)
                    fm_cols(C_QB, 2048, own, sink_qb)

                    def sink_gn(ct, i, b, mrows):
                        t0 = i * 512
                        S.op('act', lambda e: e.activation(out=gsT[:, t0:t0 + 512], in_=ps[:48, b, :], func=AF.Sigmoid), reads=[('ps', b)], writes=['gsT'])
                    fm_cols(C_GN, 48, own, sink_gn)

                    def sink_gm(ct, i, b, mrows):
                        t0 = i * 512
                        o = okey[0] % 4
                        okey[0] += 1
                        S.op('act', lambda e: e.activation(out=ob[:, o, :], in_=ps[:, b, :], func=AF.Sigmoid), reads=[('ps', b)], writes=[('ob', o)])
                        S.op('sp', lambda e: e.dma_start(out=gmix_d[ct, :, t0:t0 + 512], in_=ob[:, o, :]), reads=[('ob', o)], dma='ob%d' % o)
                    fm_cols(C_GM, 8192, own, sink_gm)
                S.flush()

        acc_i = [0]

        def attn_chunks(chunks, nq, n_heads_cols, o_bank, d_bank):
            ncols = n_heads_cols
            nchunks = len(chunks)

            def emit_pv(ci, ch, pi):
                nk = ch['nk']
                for h in range(4):
                    v_ap, rd = ch['v_fn'](h)
                    S.op('pe', lambda e, v_ap=v_ap, h=h, nk=nk, pi=pi, ci=ci: e.matmul(
                        ps[:, o_bank, h * nq:(h + 1) * nq], lhsT=v_ap, rhs=PT[:nk, pi, h * nq:(h + 1) * nq],
                        start=(ci == 0 and h == 0), stop=(ci == nchunks - 1)), reads=rd + [('PT', pi)], writes=[('ps', o_bank)])
                S.op('pe', lambda e, nk=nk, pi=pi, ci=ci: e.matmul(ps[:, d_bank, :ncols], lhsT=cst[:nk, 2, :], rhs=PT[:nk, pi, :ncols],
                                                                start=(ci == 0), stop=(ci == nchunks - 1)),
                     reads=[('PT', pi), 'cst'], writes=[('ps', d_bank)])

            pend = None
            for ci, ch in enumerate(chunks):
                nk = ch['nk']
                b = nb()
                ops_ = ch['s_ops']
                for oi, (l_ap, r_ap, o_ap, rd) in enumerate(ops_):
                    S.op('pe', lambda e, l_ap=l_ap, r_ap=r_ap, o_ap=o_ap, b=b, st_=ch['starts'][oi], sp_=ch['stops'][oi]: e.matmul(
                        o_ap(b), lhsT=l_ap, rhs=r_ap, start=st_, stop=sp_), reads=rd, writes=[('ps', b)])
                pi = acc_i[0] % 3
                acc_i[0] += 1
                S.op('act', lambda e, b=b, nk=nk, pi=pi: e.activation(out=PT[:nk, pi, :ncols], in_=ps[:nk, b, :ncols], func=AF.Exp, scale=SCALE),
                     reads=[('ps', b)], writes=[('PT', pi)])
                if ch.get('keep') is not None:
                    ch['keep'](pi, nk)
                if os.environ.get('MK_OLDATTN'):
                    emit_pv(ci, ch, pi)
                    continue
                if pend is not None:
                    emit_pv(*pend)
                pend = (ci, ch, pi)
            if pend is not None:
                emit_pv(*pend)

        with ExitStack() as st:
            PT = sbt(st, "PT", [128, 3, 512], BF16)
            kaS = sbt(st, "kaS", [128, 4, EXT], BF16)
            vS = sbt(st, "vS", [128, 4, 512], BF16)
            bD = sbt(st, "bD", [128, 22, 512], BF16)
            numT = sbt(st, "numT", [128, 4, T], F32)
            denT = sbt(st, "denT", [128, 4, T], F32)
            qaT = sbt(st, "qaT", [128, 4, T], BF16)
            yaT = sbt(st, "yaT", [128, 4, T], BF16)
            S.op('sp', lambda e: e.dma_start(out=bD[:], in_=biasD), writes=['bD'], dma='c')
            vslot = [0]
            for grp, r in enumerate((1, 4, 16)):
                for h in range(4):
                    S.op('sp', lambda e, grp=grp, h=h: e.dma_start(out=kaS[:, h, :], in_=kaT_d[grp * 4 + h]), writes=['kaS'], dma='kaS')
                    S.op('sp', lambda e, grp=grp, h=h: e.dma_start(out=qaT[:, h, :], in_=qaT_d[grp * 4 + h]), writes=['qaT'], dma='qaT')
                nq = 128 if r < 16 else 64
                nblk = (T // r) // nq
                for rho in range(r):
                    for blk in range(nblk):
                        a0 = blk * nq
                        if grp == 0:
                            bidx = [blk * 2, blk * 2 + 1]
                        elif grp == 1:
                            bidx = [16 + blk * 2, 16 + blk * 2 + 1]
                        else:
                            bidx = [20, 21]
                        chunks = []
                        for ci, (ks, nk) in enumerate(((a0 - 128, 128), (a0, nq))):
                            e0 = HALO + r * ks + rho
                            vs = vslot[0] % 4
                            vslot[0] += 1
                            S.op('sp', lambda e, e0=e0, nk=nk, vs=vs, grp=grp, r=r: e.dma_start(
                                out=vS[:nk, vs, :], in_=va_d[e0:e0 + r * (nk - 1) + 1:r, grp * 512:(grp + 1) * 512]),
                                writes=[('vS', vs)], dma='vS%d' % vs)
                            q0 = rho + r * a0
                            s_ops = [(cst[:nk, 0, :nk], bD[:nk, bidx[ci], :4 * nq],
                                      (lambda b, nk=nk, nq=nq: ps[:nk, b, :4 * nq]), ['bD', 'cst'])]
                            for h in range(4):
                                s_ops.append((kaS[:, h, e0:e0 + r * (nk - 1) + 1:r],
                                              qaT[:, h, q0:q0 + r * (nq - 1) + 1:r],
                                              (lambda b, h=h, nk=nk, nq=nq: ps[:nk, b, h * nq:(h + 1) * nq]),
                                              ['kaS', 'qaT']))
                            chunks.append(dict(nk=nk, s_ops=s_ops, starts=[True] + [False] * 4, stops=[False] * 4 + [True],
                                               v_fn=(lambda h, vs=vs, nk=nk: (vS[:nk, vs, h * 128:(h + 1) * 128], [('vS', vs)]))))
                        ob_, db_ = (4, 5) if (acc_i[0] // 2) % 2 == 0 else (6, 7)
                        attn_chunks(chunks, nq, 4 * nq, ob_, db_)
                        q0 = rho + r * a0
                        dstn = numT[:, :, q0:q0 + r * (nq - 1) + 1:r]
                        dstd = denT[:, :, q0:q0 + r * (nq - 1) + 1:r]
                        srcn = ps[:, ob_, :4 * nq].rearrange("p (h q) -> p h q", h=4)
                        srcd = ps[:, db_, :4 * nq].rearrange("p (h q) -> p h q", h=4)
                        if grp == 0:
                            S.op('dve', lambda e, dstn=dstn, srcn=srcn: e.tensor_copy(out=dstn, in_=srcn), reads=[('ps', ob_)], writes=['numT'])
                            S.op('act', lambda e, dstd=dstd, srcd=srcd: e.copy(out=dstd, in_=srcd), reads=[('ps', db_)], writes=['denT'])
                        else:
                            S.op('dve', lambda e, dstn=dstn, srcn=srcn: e.tensor_tensor(out=dstn, in0=dstn, in1=srcn, op=ALU.add), reads=[('ps', ob_), 'numT'], writes=['numT'])
                            S.op('dve', lambda e, dstd=dstd, srcd=srcd: e.tensor_tensor(out=dstd, in0=dstd, in1=srcd, op=ALU.add), reads=[('ps', db_), 'denT'], writes=['denT'])
            S.op('dve', lambda e: e.reciprocal(out=denT[:], in_=denT[:]), reads=['denT'], writes=['denT'])
            S.op('dve', lambda e: e.tensor_tensor(out=yaT[:], in0=numT[:], in1=denT[:], op=ALU.mult), reads=['numT', 'denT'], writes=['yaT'])
            S.op('sp', lambda e: e.dma_start(out=ya_d.rearrange("h p t -> p h t"), in_=yaT[:]), reads=['yaT'], dma='c')
            S.flush()

        with ExitStack() as st:
            PT = sbt(st, "PT", [128, 3, 512], BF16)
            PK = sbt(st, "PK", [128, 4, 512], BF16)
            Ksel = sbt(st, "Ksel", [128, S_ALL], BF16)
            Vsel = sbt(st, "Vsel", [128, 64, 128], BF16)
            Kw = sbt(st, "Kw", [128, 1536], BF16)
            Vw = sbt(st, "Vw", [128, 12, 128], BF16)
            xs_sb = sbt(st, "xs_sb", [128, S_ALL], BF16)
            ov_sb = sbt(st, "ov_sb", [128, 4, 128], BF16)
            gs_sb = sbt(st, "gs_sb", [48, 48, 128], BF16)
            bC = sbt(st, "bC", [128, 4, 512], BF16)
            bW = sbt(st, "bW", [128, 5, 512], BF16)
            dg = sbt(st, "dg", [128, 8, 512], BF16)
            aF = sbt(st, "aF", [128, 128], F32)
            rden = sbt(st, "rden", [128, 512], F32)
            pn = sbt(st, "pn", [128, 2, 512], BF16)
            sc = sbt(st, "sc", [128, 128], F32)
            wk = sbt(st, "wk", [128, 128], F32)
            mx = sbt(st, "mx", [128, 16], F32)
            selb = sbt(st, "selb", [128, 128], BF16)
            selT = sbt(st, "selT", [128, 512], BF16)
            grep = sbt(st, "grep", [128, 512], F32)
            acc = sbt(st, "acc", [128, 512], F32)
            tmpo = sbt(st, "tmpo", [128, 512], F32)
            ybo = sbt(st, "ybo", [128, 512], BF16)
            qrT = sbt(st, "qrT", [128, 4, T], BF16)
            qoT = sbt(st, "qoT", [128, 4, T], BF16)
            S.op('sp', lambda e: e.dma_start(out=xs_sb[:], in_=xsel), writes=['k2'], dma='c')
            S.op('sp', lambda e: e.dma_start(out=ov_sb[:], in_=ovm), writes=['k2'], dma='c')
            S.op('sp', lambda e: e.dma_start(out=gs_sb[:], in_=gsel), writes=['k2'], dma='c')
            S.op('sp', lambda e: e.dma_start(out=dg[:], in_=diagb), writes=['k2'], dma='c')

            def finish_part(part, g, ob_, db_, first, QB0):
                S.op('dve', lambda e: e.tensor_scalar(out=rden[:], in0=ps[:, db_, :], scalar1=1e-30, scalar2=None, op0=ALU.max),
                     reads=[('ps', db_)], writes=['rden'])
                S.op('dve', lambda e: e.reciprocal(out=rden[:], in_=rden[:]), reads=['rden'], writes=['rden'])
                bg = nb()
                for h in range(4):
                    col = (g * 4 + h) * 3 + part
                    S.op('pe', lambda e, h=h, col=col: e.matmul(ps[:, bg, h * 128:(h + 1) * 128], lhsT=gs_sb[:, col, :], rhs=gsT[:, QB0:QB0 + 128], start=(h == 0), stop=True),
                         reads=['k2', 'gsT'], writes=[('ps', bg)])
                S.op('dve', lambda e: e.tensor_tensor(out=grep[:], in0=ps[:, bg, :], in1=rden[:], op=ALU.mult), reads=[('ps', bg), 'rden'], writes=['grep'])
                if first:
                    S.op('dve', lambda e: e.tensor_tensor(out=acc[:], in0=ps[:, ob_, :], in1=grep[:], op=ALU.mult), reads=[('ps', ob_), 'grep'], writes=['acc'])
                else:
                    S.op('dve', lambda e: e.tensor_tensor(out=tmpo[:], in0=ps[:, ob_, :], in1=grep[:], op=ALU.mult), reads=[('ps', ob_), 'grep'], writes=['tmpo'])
                    S.op('dve', lambda e: e.tensor_tensor(out=acc[:], in0=acc[:], in1=tmpo[:], op=ALU.add), reads=['acc', 'tmpo'], writes=['acc'])

            def nsa_block(g, qb):
                QB0 = qb * 128
                S.op('sp', lambda e, qb=qb: e.dma_start(out=bC[:], in_=biasC[qb]), writes=['bC'], dma='bC')
                S.op('sp', lambda e, qb=qb: e.dma_start(out=bW[:], in_=biasW[qb]), writes=['bW'], dma='bW')
                S.op('sp', lambda e, qb=qb: e.dma_start(out=aF[:], in_=addF[qb]), writes=['aF'], dma='aF')
                qr4 = qrT[:, :, QB0:QB0 + 128]
                qo4 = qoT[:, :, QB0:QB0 + 128]
                qrd = ['qrT']
                qod = ['qoT']
                full = lambda b: ps[:, b, :]
                chunks = []
                for j in range(4):
                    def keep(pi, nk, j=j):
                        S.op('pool', lambda e, pi=pi, j=j: e.tensor_copy(out=PK[:, j, :], in_=PT[:, pi, :]), reads=[('PT', pi)], writes=[('PK', j)])
                    chunks.append(dict(nk=128, starts=[True, False], stops=[False, True], keep=keep,
                                       s_ops=[(kc_c[:, g, j * 128:(j + 1) * 128], qr4, full, ['kc_c'] + qrd),
                                              (ident, bC[:, j, :], full, ['bC', 'cst'])],
                                       v_fn=(lambda h, j=j, g=g: (vc_c[:, g, j, :], ['vc_c']))))
                attn_chunks(chunks, 128, 512, 4, 5)
                finish_part(0, g, 4, 5, True, QB0)
                bi = nb()
                for j in range(4):
                    S.op('dve', lambda e, j=j: e.tensor_tensor(out=pn[:, j % 2, :], in0=PK[:, j, :], in1=rden[:], op=ALU.mult),
                         reads=[('PK', j), 'rden'], writes=[('pn', j % 2)])
                    for h in range(4):
                        S.op('pe', lambda e, j=j, h=h: e.matmul(ps[:, bi, :128], lhsT=pn[:, j % 2, h * 128:(h + 1) * 128], rhs=ov_sb[:, j, :],
                                                               start=(j == 0 and h == 0), stop=(j == 3 and h == 3)),
                             reads=[('pn', j % 2), 'k2'], writes=[('ps', bi)])
                S.op('dve', lambda e: e.tensor_tensor(out=sc[:], in0=ps[:, bi, :128], in1=aF[:], op=ALU.add), reads=[('ps', bi), 'aF'], writes=['sc'])
                S.op('dve', lambda e: e.max(out=mx[:, 0:8], in_=sc[:]), reads=['sc'], writes=['mx'])
                S.op('dve', lambda e: e.match_replace(out=wk[:], in_to_replace=mx[:, 0:8], in_values=sc[:], imm_value=-1e30), reads=['sc', 'mx'], writes=['wk'])
                S.op('dve', lambda e: e.max(out=mx[:, 8:16], in_=wk[:]), reads=['wk'], writes=['mx'])
                S.op('dve', lambda e: e.tensor_scalar(out=mx[:, 15:16], in0=mx[:, 15:16], scalar1=-1e29, scalar2=None, op0=ALU.max), reads=['mx'], writes=['mx'])
                S.op('dve', lambda e: e.tensor_scalar(out=selb[:], in0=sc[:], scalar1=mx[:, 15:16], scalar2=NEG, op0=ALU.is_lt, op1=ALU.mult),
                     reads=['sc', 'mx'], writes=['selb'])
                bt = nb()
                S.op('pe', lambda e: e.matmul(ps[:, bt, :128], lhsT=selb[:], rhs=ident, start=True, stop=True), reads=['selb', 'cst'], writes=[('ps', bt)])
                for h in range(4):
                    S.op('dve', (lambda e, h=h: e.tensor_copy(out=selT[:, h * 128:(h + 1) * 128], in_=ps[:, bt, :128])),
                         reads=[('ps', bt)], writes=[('selT', h)])
                selrd = [('selT', h) for h in range(4)]
                chunks = []
                nch = 57 + qb
                for j in range(nch):
                    s_ops = [(Ksel[:, j * 128:(j + 1) * 128], qo4, full, ['Ksel'] + qod),
                             (xs_sb[:, j * 128:(j + 1) * 128], selT[:], full, ['k2'] + selrd)]
                    if j % 8 == qb:
                        s_ops.append((ident, dg[:, j // 8, :], full, ['k2', 'cst']))
                    n = len(s_ops)
                    chunks.append(dict(nk=128, s_ops=s_ops, starts=[True] + [False] * (n - 1), stops=[False] * (n - 1) + [True],
                                       v_fn=(lambda h, j=j: (Vsel[:, j, :], ['Vsel']))))
                attn_chunks(chunks, 128, 512, 6, 7)
                finish_part(1, g, 6, 7, False, QB0)
                chunks = []
                for i in range(5):
                    kj = qb + i
                    chunks.append(dict(nk=128, starts=[True, False], stops=[False, True],
                                       s_ops=[(Kw[:, kj * 128:(kj + 1) * 128], qo4, full, ['Kw'] + qod),
                                              (ident, bW[:, i, :], full, ['bW', 'cst'])],
                                       v_fn=(lambda h, kj=kj: (Vw[:, kj, :], ['Vw']))))
                attn_chunks(chunks, 128, 512, 4, 5)
                finish_part(2, g, 4, 5, False, QB0)
                S.op('act', lambda e: e.copy(out=ybo[:], in_=acc[:]), reads=['acc'], writes=['ybo'])
                S.op('sp', lambda e: e.dma_start(out=yb_d[4 * g:4 * g + 4, :, QB0:QB0 + 128].rearrange("h p q -> p h q"), in_=ybo[:].rearrange("p (h q) -> p h q", h=4)), reads=['ybo'], dma='ybo')

            for g in range(4):
                S.op('sp', lambda e, g=g: e.dma_start(out=Ksel[:], in_=kslT_d[g]), writes=['Ksel'], dma='Ksel')
                for jj in range(8):
                    S.op('sp', lambda e, g=g, jj=jj: e.dma_start(out=Vsel[:, 8 * jj:8 * jj + 8, :], in_=vsl_d[1024 * jj:1024 * jj + 1024, g * 128:(g + 1) * 128].rearrange("(j k) d -> k j d", k=128)), writes=['Vsel'], dma='Vsel')
                S.op('sp', lambda e, g=g: e.dma_start(out=qrT[:], in_=qrT_d[4 * g:4 * g + 4].rearrange("h p t -> p h t")), writes=['qrT'], dma='qrT')
                S.op('sp', lambda e, g=g: e.dma_start(out=qoT[:], in_=qoT_d[4 * g:4 * g + 4].rearrange("h p t -> p h t")), writes=['qoT'], dma='qoT')
                S.op('sp', lambda e, g=g: e.dma_start(out=Kw[:], in_=kwnT_d[g, :, HALO - 512:EXT]), writes=['Kw'], dma='Kw')
                S.op('sp', lambda e, g=g: e.dma_start(out=Vw[:], in_=vwn_d[HALO - 512:EXT, g * 128:(g + 1) * 128].rearrange("(j k) d -> k j d", k=128)), writes=['Vw'], dma='Vw')
                for qb in range(8):
                    nsa_block(g, qb)
            S.flush()

        xo_v = xT_ext_v

        def epilogue(st, sums_banks, g_post, xin_fn, xout_d, g_next, hN, final_out=None):
            rsb = sbt(st, "rsb", [128, 2, 512], F32)
            hb = sbt(st, "hb", [128, 2, 512], BF16)
            pv = sbt(st, "pv", [128, 2, 512], F32)
            xv = sbt(st, "xv", [128, 2, 512], F32)
            sq2 = sbt(st, "sq2", [128, 2, 512], BF16)
            for tt in range(2):
                S.op('act', lambda e, tt=tt: e.activation(out=rsb[:, tt, :], in_=ps[:, sums_banks[tt], :], func=AF.Sqrt, scale=1.0 / D, bias=epsb[:]),
                     reads=[('ps', sums_banks[tt]), 'epsb'], writes=[('rsb', tt)])
                S.op('dve', lambda e, tt=tt: e.reciprocal(out=rsb[:, tt, :], in_=rsb[:, tt, :]), reads=[('rsb', tt)], writes=[('rsb', tt)])
            for tt in range(2):
                t0 = tt * 512
                for c in range(KC):
                    i = c % 2
                    S.op('sp', lambda e, c=c, i=i, t0=t0: e.dma_start(out=pv[:, i, :], in_=pre_d[c, :, t0:t0 + 512]), writes=[('pv', i)], dma='pv%d' % i)
                    S.op('sp', lambda e, c=c, i=i, t0=t0: e.dma_start(out=xv[:, i, :], in_=xin_fn(c, t0)), writes=[('xv', i)], dma='xv%d' % i)
                    S.op('dve', lambda e, c=c, i=i, tt=tt: e.scalar_tensor_tensor(out=pv[:, i, :], in0=pv[:, i, :], scalar=gcol(g_post, c), in1=rsb[:, tt, :], op0=ALU.mult, op1=ALU.mult),
                         reads=[('pv', i), ('rsb', tt), 'cst'], writes=[('pv', i)])
                    S.op('dve', lambda e, i=i: e.tensor_tensor(out=xv[:, i, :], in0=xv[:, i, :], in1=pv[:, i, :], op=ALU.add),
                         reads=[('pv', i), ('xv', i)], writes=[('xv', i)])
                    dst = final_out if final_out is not None else xout_d
                    S.op('sp', lambda e, c=c, i=i, t0=t0, dst=dst: e.dma_start(
                        out=(dst[c * 128:(c + 1) * 128, t0:t0 + 512] if final_out is not None else dst[c, :, t0:t0 + 512]), in_=xv[:, i, :]),
                        reads=[('xv', i)], dma='xo%d' % i)
                    if hN is not None:
                        S.op('act', lambda e, i=i: e.activation(out=sq2[:, i, :], in_=xv[:, i, :], func=AF.Square), reads=[('xv', i)], writes=[('sq2', i)])
                        S.op('pe', lambda e, c=c, i=i, tt=tt: e.matmul(ps[:, sums_banks[tt], :], lhsT=ones, rhs=sq2[:, i, :], start=(c == 0), stop=(c == KC - 1)),
                             reads=[('sq2', i), 'cst'] + ([('rsb', tt)] if c == 0 else []), writes=[('ps', sums_banks[tt])])
                        S.op('act', lambda e, c=c, i=i: e.activation(out=hb[:, i, :], in_=xv[:, i, :], func=AF.Copy, scale=gcol(g_next, c)),
                             reads=[('xv', i), 'cst'], writes=[('hb', i)])
                        S.op('sp', lambda e, c=c, i=i, t0=t0: e.dma_start(out=h_d[c, :, t0:t0 + 512], in_=hb[:, i, :]), reads=[('hb', i)], dma='hb%d' % i)
                if hN is not None:
                    S.op('act', lambda e, tt=tt: e.activation(out=rs2p[:, tt, :], in_=ps[:, sums_banks[tt], :], func=AF.Sqrt, scale=1.0 / D, bias=epsb[:]),
                         reads=[('ps', sums_banks[tt]), 'epsb'], writes=[('rs2', tt)])
                    S.op('dve', lambda e, tt=tt: e.reciprocal(out=rs2p[:, tt, :], in_=rs2p[:, tt, :]), reads=[('rs2', tt)], writes=[('rs2', tt)])

        def load_h(hT_):
            for q in range(4):
                S.op('sp', lambda e, q=q: e.dma_start(out=hT_[:, 8 * q:8 * q + 8, :], in_=h_d[8 * q:8 * q + 8].rearrange("c p t -> p c t")), writes=[('hld', q)], dma='hld')
            for c in range(KC):
                for tt in range(2):
                    S.op('dve' if (c + tt) % 2 else 'pool', lambda e, c=c, tt=tt: e.tensor_tensor(out=hT_[:, c, tt * 512:(tt + 1) * 512], in0=hT_[:, c, tt * 512:(tt + 1) * 512], in1=rs2p[:, tt, :], op=ALU.mult),
                         reads=[('hld', c // 8)], writes=[('hN', c, tt)])

        def pre_store(st_bufs, b, ct, tt, okey):
            po, sqp = st_bufs
            o = okey[0] % 2
            okey[0] += 1
            S.op('dve', lambda e: e.tensor_copy(out=po[:, o, :], in_=ps[:, b, :]), reads=[('ps', b)], writes=[('po', o)])
            S.op('act', lambda e: e.activation(out=sqp[:, o, :], in_=po[:, o, :], func=AF.Square), reads=[('po', o)], writes=[('sqp', o)])
            S.op('pe', lambda e: e.matmul(ps[:, 6 + tt, :], lhsT=ones, rhs=sqp[:, o, :], start=(ct == 0), stop=(ct == KC - 1)),
                 reads=[('sqp', o), 'cst'], writes=[('ps', 6 + tt)])
            S.op('sp', lambda e: e.dma_start(out=pre_d[ct, :, tt * 512:(tt + 1) * 512], in_=po[:, o, :]), reads=[('po', o)], dma='po%d' % o)

        if True:
            with ExitStack() as st:
                yaT = sbt(st, "yaT", [128, 4, T], BF16)
                ybT = sbt(st, "ybT", [128, 16, T], BF16)
                S.op('sp', lambda e: e.dma_start(out=yaT[:], in_=ya_d.rearrange("h p t -> p h t")), writes=['yaT'], dma='c')
                S.op('sp', lambda e: e.dma_start(out=ybT[:], in_=yb_d.rearrange("h p t -> p h t")), writes=['ybT'], dma='c')
                mT = sbt(st, "mT", [128, KC, T], BF16)
                wb = sbt(st, "wb", [128, 2, KC, 256], BF16)
                gm = sbt(st, "gm", [128, 2, 2, T], BF16)
                t1 = sbt(st, "t1", [128, 2, 512], F32)
                t2 = sbt(st, "t2", [128, 2, 512], F32)
                po = sbt(st, "po", [128, 2, 512], F32)
                sqp = sbt(st, "sqp", [128, 2, 512], BF16)
                k = [0]
                for w0 in range(0, D, 256):
                    sa = load_w(wb, w_a, 0, 4, w0, 256)
                    sb_ = load_w(wb, w_b, 0, 16, w0, 256)
                    for cs_ in range(2):
                        ct = w0 // 128 + cs_
                        gi = ct % 2
                        S.op('sp', lambda e, ct=ct, gi=gi: e.dma_start(out=gm[:, gi, 0, :], in_=gmix_d[ct]), writes=[('gm', gi)], dma='gm%d' % gi)
                        S.op('sp', lambda e, ct=ct, gi=gi: e.dma_start(out=gm[:, gi, 1, :], in_=gmix_d[32 + ct]), writes=[('gm', gi)], dma='gm%d' % gi)
                        for tt in range(2):
                            t0 = tt * 512
                            ba = nb(); bb = nb()
                            mm_fm(wb, sa, 4, cs_, lambda kc, t0=t0: yaT[:, kc, t0:t0 + 512], 512, ba, ['yaT'])
                            mm_fm(wb, sb_, 16, cs_, lambda kc, t0=t0: ybT[:, kc, t0:t0 + 512], 512, bb, ['ybT'])
                            o = k[0] % 2
                            k[0] += 1
                            S.op('dve', lambda e, ba=ba, o=o, gi=gi, t0=t0: e.tensor_tensor(out=t1[:, o, :], in0=ps[:, ba, :], in1=gm[:, gi, 0, t0:t0 + 512], op=ALU.mult),
                                 reads=[('ps', ba), ('gm', gi)], writes=[('t1', o)])
                            S.op('dve', lambda e, bb=bb, o=o, gi=gi, t0=t0: e.tensor_tensor(out=t2[:, o, :], in0=ps[:, bb, :], in1=gm[:, gi, 1, t0:t0 + 512], op=ALU.mult),
                                 reads=[('ps', bb), ('gm', gi)], writes=[('t2', o)])
                            S.op('dve', lambda e, o=o, ct=ct, t0=t0: e.tensor_tensor(out=mT[:, ct, t0:t0 + 512], in0=t1[:, o, :], in1=t2[:, o, :], op=ALU.add),
                                 reads=[('t1', o), ('t2', o)], writes=[('mT', ct)])
                mrd = [('mT', c) for c in range(KC)]
                ok = [0]
                for w0 in range(0, D, 256):
                    s = load_w(wb, w_out, 0, KC, w0, 256)
                    for cs_ in range(2):
                        ct = w0 // 128 + cs_
                        for tt in range(2):
                            t0 = tt * 512
                            b = nb()
                            mm_fm(wb, s, KC, cs_, lambda kc, t0=t0: mT[:, kc, t0:t0 + 512], 512, b, mrd)
                            pre_store((po, sqp), b, ct, tt, ok)
                S.flush()
            with ExitStack() as st:
                epilogue(st, (6, 7), G_MIXPOST, lambda c, t0: xT_ext_v[:, c, HALO + t0:HALO + t0 + 512], x1_d, G_FFNPRE, True)
                S.flush()

            with ExitStack() as st:
                hT2 = sbt(st, "hT2", [128, KC, T], BF16)
                load_h(hT2)
                wb = sbt(st, "wb", [128, 4, KC, 128], BF16)
                ao = sbt(st, "ao", [128, 2, T], BF16)
                sg = sbt(st, "sg", [128, 2, 512], F32)
                h2rd = [('hN', c, tt) for c in range(KC) for tt in range(2)]
                wk_ = [0]
                for ft in range(FKC):
                    slots = []
                    for half_, c0 in enumerate((ft * 128, DFF + ft * 128)):
                        s = wk_[0] % 4
                        wk_[0] += 1
                        src = w_gu[:, c0:c0 + 128].rearrange("(k p) c -> p k c", p=128)
                        S.op('pool', lambda e, s=s, src=src: e.dma_start(out=wb[:, s, :16, :], in_=src[:, :16, :]), writes=[('wb', s)], dma='wb%d' % s)
                        S.op('pool', lambda e, s=s, src=src: e.dma_start(out=wb[:, s, 16:, :], in_=src[:, 16:, :]), writes=[('wb', s)], dma='wb%d' % s)
                        slots.append(s)
                    ai = ft % 2
                    for tt in range(2):
                        t0 = tt * 512
                        bg = nb(); bu = nb()
                        for kc in range(KC):
                            S.op('pe', lambda e, kc=kc, bg=bg, s=slots[0], t0=t0: e.matmul(ps[:, bg, :], lhsT=wb[:, s, kc, :], rhs=hT2[:, kc, t0:t0 + 512], start=(kc == 0), stop=(kc == KC - 1)),
                                 reads=[('wb', slots[0])] + (h2rd if kc == 0 else []), writes=[('ps', bg)])
                        for kc in range(KC):
                            S.op('pe', lambda e, kc=kc, bu=bu, s=slots[1], t0=t0: e.matmul(ps[:, bu, :], lhsT=wb[:, s, kc, :], rhs=hT2[:, kc, t0:t0 + 512], start=(kc == 0), stop=(kc == KC - 1)),
                                 reads=[('wb', slots[1])], writes=[('ps', bu)])
                        S.op('act', lambda e, bg=bg, tt=tt: e.activation(out=sg[:, tt, :], in_=ps[:, bg, :], func=AF.Silu), reads=[('ps', bg)], writes=[('sg', tt)])
                        S.op('dve', lambda e, bu=bu, tt=tt, ai=ai, t0=t0: e.tensor_tensor(out=ao[:, ai, t0:t0 + 512], in0=sg[:, tt, :], in1=ps[:, bu, :], op=ALU.mult),
                             reads=[('sg', tt), ('ps', bu)], writes=[('ao', ai)])
                    S.op('sp', lambda e, ft=ft, ai=ai: e.dma_start(out=act_d[ft], in_=ao[:, ai, :]), reads=[('ao', ai)], dma='ao%d' % ai)
                S.flush()
        with ExitStack() as st:
            HK = FKC // 2
            aT = sbt(st, "aT", [128, HK, T], BF16)
            wb = sbt(st, "wb", [128, 2, HK, 128], BF16)
            po = sbt(st, "po", [128, 2, 512], F32)
            pr = sbt(st, "pr", [128, 2, 512], F32)
            sqp = sbt(st, "sqp", [128, 2, 512], BF16)
            ok = [0]
            wk_ = [0]
            for hf in range(2):
                k0 = hf * HK
                for q in range(2):
                    f0, f1 = (0, 22) if q == 0 else (22, HK)
                    S.op('sp', lambda e, f0=f0, f1=f1, k0=k0: e.dma_start(out=aT[:, f0:f1, :], in_=act_d[k0 + f0:k0 + f1].rearrange("f p t -> p f t")), writes=['aT'], dma='aT')
                for ct in range(KC):
                    s_ = wk_[0] % 2
                    wk_[0] += 1
                    src = w_down[k0 * 128:(k0 + HK) * 128, ct * 128:(ct + 1) * 128].rearrange("(k p) c -> p k c", p=128)
                    S.op('pool', lambda e, s_=s_, src=src: e.dma_start(out=wb[:, s_, :22, :], in_=src[:, :22, :]), writes=[('wb', s_)], dma='wb%d' % s_)
                    S.op('pool', lambda e, s_=s_, src=src: e.dma_start(out=wb[:, s_, 22:, :], in_=src[:, 22:, :]), writes=[('wb', s_)], dma='wb%d' % s_)
                    for tt in range(2):
                        t0 = tt * 512
                        bk = nb()
                        for kc in range(HK):
                            S.op('pe', lambda e, kc=kc, s_=s_, bk=bk, t0=t0: e.matmul(ps[:, bk, :], lhsT=wb[:, s_, kc, :], rhs=aT[:, kc, t0:t0 + 512], start=(kc == 0), stop=(kc == HK - 1)),
                                 reads=[('wb', s_), 'aT'], writes=[('ps', bk)])
                        o = ok[0] % 2
                        ok[0] += 1
                        if hf == 0:
                            S.op('dve', lambda e, bk=bk, o=o: e.tensor_copy(out=po[:, o, :], in_=ps[:, bk, :]), reads=[('ps', bk)], writes=[('po', o)])
                            S.op('sp', lambda e, o=o, ct=ct, t0=t0: e.dma_start(out=pre_d[ct, :, t0:t0 + 512], in_=po[:, o, :]), reads=[('po', o)], writes=[('pre', ct, tt)], dma='po%d' % o)
                        else:
                            S.op('sp', lambda e, o=o, ct=ct, t0=t0: e.dma_start(out=pr[:, o, :], in_=pre_d[ct, :, t0:t0 + 512]), reads=[('pre', ct, tt)], writes=[('pr', o)], dma='pr%d' % o)
                            S.op('dve', lambda e, bk=bk, o=o: e.tensor_tensor(out=po[:, o, :], in0=ps[:, bk, :], in1=pr[:, o, :], op=ALU.add), reads=[('ps', bk), ('pr', o)], writes=[('po', o)])
                            S.op('act', lambda e, o=o: e.activation(out=sqp[:, o, :], in_=po[:, o, :], func=AF.Square), reads=[('po', o)], writes=[('sqp', o)])
                            S.op('pe', lambda e, o=o, ct=ct, tt=tt: e.matmul(ps[:, 6 + tt, :], lhsT=ones, rhs=sqp[:, o, :], start=(ct == 0), stop=(ct == KC - 1)),
                                 reads=[('sqp', o), 'cst'], writes=[('ps', 6 + tt)])
                            S.op('sp', lambda e, o=o, ct=ct, t0=t0: e.dma_start(out=pre_d[ct, :, t0:t0 + 512], in_=po[:, o, :]), reads=[('po', o)], writes=[('pre', ct, tt)], dma='po%d' % o)
            S.flush()
        if True:
            with ExitStack() as st:
                epilogue(st, (6, 7), G_FFNPOST, lambda c, t0: x1_d[c, :, t0:t0 + 512], x2_d, G_PLEPRE, True)
                S.flush()
            with ExitStack() as st:
                hT3 = sbt(st, "hT3", [128, KC, T], BF16)
                load_h(hT3)
                wb = sbt(st, "wb", [128, 2, KC, 256], BF16)
                wp = sbt(st, "wp", [128, 2, 2, 256], BF16)
                pb = sbt(st, "pb", [128, 2, T], BF16)
                sg = sbt(st, "sg", [128, 2, 512], F32)
                pl = sbt(st, "pl", [128, 2, 512], F32)
                po = sbt(st, "po", [128, 2, 512], F32)
                sqp = sbt(st, "sqp", [128, 2, 512], BF16)
                S.op('pool', lambda e: e.dma_start(out=pb[:], in_=pT.rearrange("(k p) t -> p k t", p=128)), writes=['pb'], dma='c')
                h3rd = [('hN', c, tt) for c in range(KC) for tt in range(2)]
                ok = [0]
                k = [0]
                for w0 in range(0, D, 256):
                    s = load_w(wb, w_pg, 0, KC, w0, 256)
                    S.op('pool', lambda e, s=s, w0=w0: e.dma_start(out=wp[:, s, :, :], in_=w_ple[:, w0:w0 + 256].rearrange("(k p) c -> p k c", p=128)),
                         writes=[('wp', s)], dma='wp%d' % s)
                    for cs_ in range(2):
                        ct = w0 // 128 + cs_
                        for tt in range(2):
                            t0 = tt * 512
                            bg = nb(); bp = nb()
                            mm_fm(wb, s, KC, cs_, lambda kc, t0=t0: hT3[:, kc, t0:t0 + 512], 512, bg, h3rd)
                            for kc in range(2):
                                S.op('pe', lambda e, kc=kc, s=s, cs_=cs_, bp=bp, t0=t0: e.matmul(ps[:, bp, :], lhsT=wp[:, s, kc, cs_ * 128:(cs_ + 1) * 128], rhs=pb[:, kc, t0:t0 + 512],
                                                                                         start=(kc == 0), stop=(kc == 1)),
                                     reads=[('wp', s), 'pb'], writes=[('ps', bp)])
                            o = k[0] % 2
                            k[0] += 1
                            S.op('act', lambda e, bg=bg, o=o: e.activation(out=sg[:, o, :], in_=ps[:, bg, :], func=AF.Sigmoid), reads=[('ps', bg)], writes=[('sg', o)])
                            S.op('dve', lambda e, bp=bp, o=o: e.tensor_tensor(out=pl[:, o, :], in0=sg[:, o, :], in1=ps[:, bp, :], op=ALU.mult),
                                 reads=[('sg', o), ('ps', bp)], writes=[('pl', o)])
                            o2 = ok[0] % 2
                            ok[0] += 1
                            S.op('act', lambda e, o=o, o2=o2: e.activation(out=sqp[:, o2, :], in_=pl[:, o, :], func=AF.Square), reads=[('pl', o)], writes=[('sqp', o2)])
                            S.op('pe', lambda e, o2=o2, ct=ct, tt=tt: e.matmul(ps[:, 6 + tt, :], lhsT=ones, rhs=sqp[:, o2, :], start=(ct == 0), stop=(ct == KC - 1)),
                                 reads=[('sqp', o2), 'cst'], writes=[('ps', 6 + tt)])
                            S.op('sp', lambda e, o=o, ct=ct, tt=tt: e.dma_start(out=pre_d[ct, :, tt * 512:(tt + 1) * 512], in_=pl[:, o, :]), reads=[('pl', o)], dma='po%d' % o)
                S.flush()
        with ExitStack() as st:
            epilogue(st, (6, 7), G_PLEPOST, lambda c, t0: x2_d[c, :, t0:t0 + 512], None, None, None, final_out=outT)
            S.flush()
    return nc


def _bias(valid, reps=4):
    b = np.where(valid, 0.0, NEG).astype(np.float32)
    return np.tile(b, (1, reps)).astype(ml_dtypes.bfloat16)


def _host_consts(core):
    bf = ml_dtypes.bfloat16
    start = core * T
    c = {}
    half = 64
    inv = (10000.0 ** (-np.arange(half, dtype=np.float32) / half)).astype(np.float32)

    def cs(pos):
        ang = pos.astype(np.float32)[None, :] * np.concatenate([inv, inv])[:, None]
        co = np.cos(ang).astype(np.float32)
        si = np.sin(ang).astype(np.float32)
        si[:half] *= -1.0
        return np.stack([co, si], axis=1).astype(np.float32)
    c["cs_full"] = cs(np.arange(S_ALL))
    c["cs_ext"] = cs(np.arange(start - HALO, start + T))
    ident = np.eye(128, dtype=np.float32)
    swp = np.zeros((128, 128), np.float32)
    for m in range(128):
        swp[(m + 64) % 128, m] = 1.0
    c["consts"] = np.stack([ident, swp, np.ones((128, 128), np.float32)], axis=1).astype(bf)
    keys = np.arange(S_ALL)
    c["xsel"] = (keys[None, :] // 64 == np.arange(128)[:, None]).astype(np.float32).astype(bf)
    n = np.arange(512)
    m = np.arange(128)
    ov = np.clip(np.minimum(n[:, None] * 16 + 32, m[None, :] * 64 + 64) - np.maximum(n[:, None] * 16, m[None, :] * 64), 0, None) / 32.0
    ov[511] = 0.0
    c["ovm"] = ov.reshape(4, 128, 128).transpose(1, 0, 2).astype(np.float32).astype(bf)
    gs = np.zeros((48, 48, 128), np.float32)
    for k in range(48):
        gs[k, k, :] = 1.0
    c["gsel"] = gs.astype(bf)
    bc = np.zeros((8, 128, 4, 512), bf)
    af = np.zeros((8, 128, 128), np.float32)
    bw = np.zeros((8, 128, 5, 512), bf)
    for qb in range(8):
        pos = start + qb * 128 + np.arange(128)
        for j in range(4):
            nn = j * 128 + np.arange(128)
            valid = (nn[:, None] * 16 + 31 <= pos[None, :]) & (nn[:, None] < 511)
            bc[qb, :, j, :] = _bias(valid)
        cur = pos // 64
        sblk = np.arange(128)
        forced = (sblk[None] == 0) | (sblk[None] == cur[:, None]) | (sblk[None] == cur[:, None] - 1)
        sval = sblk[None] * 64 <= pos[:, None]
        af[qb] = np.where(sval, 1000.0 * forced, -1e30).astype(np.float32)
        for i in range(5):
            kp = start + (qb - 4 + i) * 128 + np.arange(128)
            valid = (kp[:, None] <= pos[None, :]) & (pos[None, :] - kp[:, None] < 512) & (kp[:, None] >= 0)
            bw[qb, :, i, :] = _bias(valid)
    c["biasC"] = bc
    c["addF"] = af
    c["biasW"] = bw
    dg = np.zeros((128, 8, 512), bf)
    kk = np.arange(128)
    dg[:, core, :] = _bias(kk[:, None] <= kk[None, :])
    c["diagb"] = dg
    bd = np.zeros((128, 22, 512), bf)

    def dil(r, nq, a0, ks, nk):
        qpos = start + r * (a0 + np.arange(nq))
        kpos = start + r * (ks + np.arange(nk))
        valid = (kpos[:, None] >= 0) & (kpos[:, None] <= qpos[None, :]) & (qpos[None, :] - kpos[:, None] <= 128 * r)
        t = np.full((128, 4 * nq), NEG, np.float32)
        t[:nk] = np.tile(np.where(valid, 0.0, NEG), (1, 4))
        out = np.zeros((128, 512), np.float32)
        out[:, :4 * nq] = t
        return out.astype(bf)
    for blk in range(8):
        bd[:, blk * 2] = dil(1, 128, blk * 128, blk * 128 - 128, 128)
        bd[:, blk * 2 + 1] = dil(1, 128, blk * 128, blk * 128, 128)
    for blk in range(2):
        bd[:, 16 + blk * 2] = dil(4, 128, blk * 128, blk * 128 - 128, 128)
        bd[:, 16 + blk * 2 + 1] = dil(4, 128, blk * 128, blk * 128, 128)
    bd[:, 20] = dil(16, 64, 0, -128, 128)
    bd[:, 21] = dil(16, 64, 0, 0, 64)
    c["biasD"] = bd
    return c


_NC_CACHE = {}


def kernel(**inputs):
    f = lambda k: np.asarray(inputs[k], dtype=np.float32)
    x = f("x")[0]
    xT = np.ascontiguousarray(x.T)
    p = f("p")[0, 0]
    gl = lambda v: np.ascontiguousarray(v.reshape(32, 128).T)
    gains = np.concatenate([gl(f(k)[0]) for k in ("g_mix_pre", "g_mix_post", "g_ffn_pre", "g_ffn_post", "g_ple_pre", "g_ple_post")]
                           + [np.zeros((128, 64), np.float32)], axis=1)
    shared = {
        "xT_full": xT, "gains": np.ascontiguousarray(gains), "w_in": f("w_in")[0],
        "peT_k": np.ascontiguousarray(f("pe_ck")[0].T), "peT_v": np.ascontiguousarray(f("pe_cv")[0].T),
        "w_ck1": f("w_ck1")[0], "w_ck2": f("w_ck2")[0], "w_cv1": f("w_cv1")[0], "w_cv2": f("w_cv2")[0],
        "w_a": f("w_a")[0], "w_b": f("w_b")[0], "w_out": f("w_out")[0], "w_gu": f("w_gu")[0],
        "w_down": f("w_down")[0], "w_pg": f("w_ple_gate")[0], "w_ple": f("w_ple")[0],
    }
    in_maps = []
    for c in range(NCORES):
        start = c * T
        ext = np.zeros((D, EXT), np.float32)
        lo = max(0, start - HALO)
        ext[:, EXT - (start + T - lo):] = xT[:, lo:start + T]
        m = dict(shared)
        m["xT_ext"] = ext
        m["pT"] = np.ascontiguousarray(p[start:start + T].T)
        m.update(_host_consts(c))
        in_maps.append(m)
    if "nc" not in _NC_CACHE:
        _NC_CACHE["nc"] = build()
    _ncr = int(os.environ.get("MK_NCORES", NCORES))
    _c0 = int(os.environ.get("MK_CORE0", 0))
    if _ncr != NCORES:
        res = run_bass_kernel_spmd(_NC_CACHE["nc"], in_maps[_c0:_c0 + _ncr], core_ids=list(range(_ncr)))
        _NC_CACHE["res"] = res
        return None
    res = run_bass_kernel_spmd(_NC_CACHE["nc"], in_maps, core_ids=list(range(NCORES)))
    if _DBG:
        _NC_CACHE["res"] = res
    out = np.concatenate([np.asarray(r["outT"]).T for r in res.results], axis=0)
    return out.reshape(1, S_ALL, D).astype(np.float32)
```

```python
import numpy as np
import ml_dtypes
from contextlib import ExitStack
import concourse.bass as bass
import concourse.mybir as mybir
from concourse.bass_utils import run_bass_kernel_spmd

F32 = mybir.dt.float32
BF16 = mybir.dt.bfloat16
AF = mybir.ActivationFunctionType
ALU = mybir.AluOpType

NCORES = 8
S_ALL = 8192
D = 4096
KC = 32
T = 1024
HALO = 2048
EXT = HALO + T
DFF = 11008
FKC = 86
SCALE = 128 ** -0.5
EPS = 1e-6
NEG = -30000.0
C_QA, C_KA, C_VA, C_QB, C_KC, C_VC, C_KSL, C_VSL, C_KWN, C_VWN, C_GN, C_GM = (
    0, 1536, 3072, 4608, 6656, 7168, 7680, 8192, 8704, 9216, 9728, 9776)

ENGS = ("pe", "act", "dve", "pool", "sp")


import os
_STOP = int(os.environ.get("MK_STOP", "1000"))
_DBG = [x for x in os.environ.get("MK_DBG", "").split(",") if x]


class StopBuild(Exception):
    pass


class Sched:
    def __init__(self, nc, st):
        self.nc = nc
        self.esem = {e: st.enter_context(nc.semaphore("s_" + e)) for e in ENGS}
        self.dsem = {}
        self.st = st
        self.seq = {e: 0 for e in ENGS}
        self.nsig = {e: 0 for e in ENGS}
        self.known = {e: {} for e in ENGS}
        self.dcount = {}
        self._reset()

    def _reset(self):
        self.ops = {e: [] for e in ENGS}
        self.lastw = {}
        self.readers = {}
        self.signal = {e: set() for e in ENGS}
        self.dma_issuer = {}

    def _need(self, eng, seq, tok, waits):
        kind, src, val = tok
        if kind == 'E':
            if src == eng:
                if eng == 'pe' or seq - val > 2:
                    return
            k = ('E', src)
        else:
            k = ('D', src)
        if self.known[eng].get(k, -1) >= val:
            return
        if val > waits.get(k, -1):
            waits[k] = val

    def op(self, eng, fn, reads=(), writes=(), dma=None):
        if getattr(self, 'stopped', False):
            return
        seq = self.seq[eng]
        self.seq[eng] += 1
        waits = {}
        for r in reads:
            t = self.lastw.get(r)
            if t is not None:
                self._need(eng, seq, t, waits)
        for w in writes:
            t = self.lastw.get(w)
            if t is not None:
                self._need(eng, seq, t, waits)
            for t in self.readers.get(w, ()):
                self._need(eng, seq, t, waits)
        for k, v in waits.items():
            self.known[eng][k] = v
            if k[0] == 'E':
                self.signal[k[1]].add(v)
        if dma is not None:
            if dma not in self.dsem:
                self.dsem[dma] = self.st.enter_context(self.nc.semaphore("d_" + str(dma)))
            c = self.dcount.get(dma, 0) + 1
            self.dcount[dma] = c
            tok = ('D', dma, c)
            self.dma_issuer.setdefault(eng, {})[dma] = c
        else:
            tok = ('E', eng, seq)
        for r in reads:
            self.readers.setdefault(r, []).append(tok)
        for w in writes:
            self.lastw[w] = tok
            self.readers[w] = []
        self.ops[eng].append((seq, fn, waits, dma))

    def flush(self):
        nc = self.nc
        if getattr(self, 'stopped', False):
            self._reset()
            return
        last = {}
        for e in ENGS:
            cs = [o[0] for o in self.ops[e] if o[3] is None and o[1] is not None]
            if cs:
                last[e] = cs[-1]
                self.signal[e].add(cs[-1])
        for e in ENGS:
            waits = {}
            for e2, s in last.items():
                if e2 != e and self.known[e].get(('E', e2), -1) < s:
                    waits[('E', e2)] = s
                    self.known[e][('E', e2)] = s
            for c, v in self.dcount.items():
                if self.known[e].get(('D', c), -1) < v:
                    waits[('D', c)] = v
                    self.known[e][('D', c)] = v
            self.ops[e].append((None, None, waits, None))
        rank = {}
        for e in ENGS:
            srt = sorted(self.signal[e])
            rank[e] = {s: self.nsig[e] + i + 1 for i, s in enumerate(srt)}
            self.nsig[e] += len(srt)
        esem, dsem = self.esem, self.dsem

        def run(e, engobj):
            sig = self.signal[e]
            for (seq, fn, waits, dma) in self.ops[e]:
                for k, v in waits.items():
                    if k[0] == 'E':
                        engobj.wait_ge(esem[k[1]], rank[k[1]][v])
                    else:
                        engobj.wait_ge(dsem[k[1]], 16 * v)
                if fn is None:
                    continue
                ins = fn(engobj)
                if dma is not None:
                    ins.then_inc(dsem[dma], 16)
                elif seq in sig:
                    ins.then_inc(esem[e], 1)

        with nc.Block() as block:
            @block.tensor
            def _(eng):
                run('pe', eng)

            @block.scalar
            def _(eng):
                run('act', eng)

            @block.vector
            def _(eng):
                run('dve', eng)

            @block.gpsimd
            def _(eng):
                run('pool', eng)

            @block.sync
            def _(eng):
                run('sp', eng)
        self._reset()
        self.nflush = getattr(self, 'nflush', 0) + 1
        if self.nflush >= _STOP:
            self.stopped = True


def build():
    nc = bass.Bass("TRN2", target_bir_lowering=False)

    def din(name, shape, dt=F32):
        return nc.dram_tensor(name, list(shape), dt, kind="ExternalInput").ap()

    def dscr(name, shape, dt):
        return nc.dram_tensor(name, list(shape), dt, kind=("ExternalOutput" if name in _DBG else "Internal")).ap()

    xT_full = din("xT_full", [D, S_ALL])
    xT_ext = din("xT_ext", [D, EXT])
    pT = din("pT", [256, T])
    gains = din("gains", [128, 8 * 32])
    w_in = din("w_in", [D, 17968])
    peT_k = din("peT_k", [128, 32]); peT_v = din("peT_v", [128, 32])
    w_ck1 = din("w_ck1", [4096, 128]); w_ck2 = din("w_ck2", [128, 128])
    w_cv1 = din("w_cv1", [4096, 128]); w_cv2 = din("w_cv2", [128, 128])
    w_a = din("w_a", [512, D]); w_b = din("w_b", [2048, D]); w_out = din("w_out", [D, D])
    w_gu = din("w_gu", [D, 2 * DFF]); w_down = din("w_down", [DFF, D])
    w_pg = din("w_pg", [D, D]); w_ple = din("w_ple", [256, D])
    cs_full = din("cs_full", [128, 2, S_ALL])
    cs_ext = din("cs_ext", [128, 2, EXT])
    consts = din("consts", [128, 3, 128], BF16)
    xsel = din("xsel", [128, S_ALL], BF16)
    ovm = din("ovm", [128, 4, 128], BF16)
    gsel = din("gsel", [48, 48, 128], BF16)
    biasC = din("biasC", [8, 128, 4, 512], BF16)
    addF = din("addF", [8, 128, 128])
    diagb = din("diagb", [128, 8, 512], BF16)
    biasW = din("biasW", [8, 128, 5, 512], BF16)
    biasD = din("biasD", [128, 22, 512], BF16)
    outT = nc.dram_tensor("outT", [D, T], F32, kind="ExternalOutput").ap()

    kcT_d = dscr("kcT_d", [8, 128, S_ALL], BF16)
    kslT_d = dscr("kslT_d", [4, 128, S_ALL], BF16)
    vsl_d = dscr("vsl_d", [S_ALL, 512], BF16)
    kaT_d = dscr("kaT_d", [12, 128, EXT], BF16)
    va_d = dscr("va_d", [EXT, 1536], BF16)
    kwnT_d = dscr("kwnT_d", [4, 128, EXT], BF16)
    vwn_d = dscr("vwn_d", [EXT, 512], BF16)
    gmix_d = dscr("gmix_d", [64, 128, T], BF16)
    pre_d = dscr("pre_d", [32, 128, T], F32)
    x1_d = dscr("x1_d", [32, 128, T], F32)
    x2_d = dscr("x2_d", [32, 128, T], F32)
    act_d = dscr("act_d", [FKC, 128, T], BF16)
    qaT_d = dscr("qaT_d", [12, 128, T], BF16)
    qrT_d = dscr("qrT_d", [16, 128, T], BF16)
    qoT_d = dscr("qoT_d", [16, 128, T], BF16)
    h_d = dscr("h_d", [32, 128, T], BF16)
    rstd_d = dscr("rstd_d", [S_ALL // 512, 128, 512], F32)
    ya_d = dscr("ya_d", [4, 128, T], BF16)
    yb_d = dscr("yb_d", [16, 128, T], BF16)

    with ExitStack() as top:
      try:
        S = Sched(nc, top)
        _build_body(nc, top, S, locals())
      except StopBuild:
        pass
    return nc


def _build_body(nc, top, S, L):
    globals().update({k: v for k, v in L.items() if k not in ('nc', 'top', 'S')})
    if True:
        _uid = [0]

        def sbt(st, name, shape, dt):
            _uid[0] += 1
            return st.enter_context(nc.sbuf_tensor("%s_%d" % (name, _uid[0]), list(shape), dt))
        ps = top.enter_context(nc.psum_tensor("ps", [128, 8, 512], F32))
        cst = sbt(top, "cst", [128, 3, 128], BF16)
        ident, swp, ones = cst[:, 0, :], cst[:, 1, :], cst[:, 2, :]
        gn = sbt(top, "gn", [128, 8 * 32], F32)
        kc_c = sbt(top, "kc_c", [128, 4, 512], BF16)
        vc_c = sbt(top, "vc_c", [128, 4, 4, 128], BF16)
        gsT = sbt(top, "gsT", [48, T], BF16)
        rs2p = sbt(top, "rs2p", [128, 2, 512], F32)

        S.op('sp', lambda e: e.dma_start(out=cst[:], in_=consts), writes=['cst'], dma='c')
        S.op('sp', lambda e: e.dma_start(out=gn[:], in_=gains), writes=['cst'], dma='c')
        epsb = sbt(top, "epsb", [128, 1], F32)
        S.op('pool', lambda e: e.memset(epsb[:], EPS), writes=['epsb'])
        S.op('pool', lambda e: e.memset(vc_c[:], 0.0), writes=['vc_c'])
        S.op('pool', lambda e: e.memset(kc_c[:], 0.0), writes=['kc_c'])
        G_MIXPRE, G_MIXPOST, G_FFNPRE, G_FFNPOST, G_PLEPRE, G_PLEPOST = range(6)

        def gcol(gi, c):
            return gn[:, gi * 32 + c: gi * 32 + c + 1]

        bank_rr = [0]

        def nb(n=4, base=0):
            b = base + bank_rr[0] % n
            bank_rr[0] += 1
            return b

        xT_full_v = xT_full.rearrange("(c p) t -> p c t", p=128)
        xT_ext_v = xT_ext.rearrange("(c p) t -> p c t", p=128)

        def norm_tile(st_, xs, hT, src_v, t0, gi, sq, rs):
            for q in range(4):
                S.op('sp', lambda e, q=q: e.dma_start(out=xs[:, 8 * q:8 * q + 8, :], in_=src_v[:, 8 * q:8 * q + 8, t0:t0 + 512]),
                     writes=[('xs', q)], dma='xs%d' % q)
            b = 7
            for c in range(KC):
                S.op('act', lambda e, c=c: e.activation(out=sq[:, c % 2, :], in_=xs[:, c, :], func=AF.Square),
                     reads=[('xs', c // 8)], writes=[('sq', c % 2)])
                S.op('pe', lambda e, c=c: e.matmul(ps[:, b, :], lhsT=ones, rhs=sq[:, c % 2, :], start=(c == 0), stop=(c == KC - 1)),
                     reads=[('sq', c % 2), 'cst'], writes=[('ps', b)])
            S.op('act', lambda e: e.activation(out=rs[:], in_=ps[:, b, :], func=AF.Sqrt, scale=1.0 / D, bias=epsb[:]),
                 reads=[('ps', b), 'epsb'], writes=['rs'])
            S.op('dve', lambda e: e.reciprocal(out=rs[:], in_=rs[:]), reads=['rs'], writes=['rs'])
            for c in range(KC):
                S.op('dve', lambda e, c=c: e.scalar_tensor_tensor(out=hT[:, c, :], in0=xs[:, c, :], scalar=gcol(gi, c), in1=rs[:], op0=ALU.mult, op1=ALU.mult),
                     reads=[('xs', c // 8), 'rs', 'cst'], writes=[('hT', id(hT), c)])

        wslot = [0]

        def load_w(wb, w_ap, r0, kc_n, c0, ncols):
            s = wslot[0] % 2
            wslot[0] += 1
            src = w_ap[r0:r0 + kc_n * 128, c0:c0 + ncols].rearrange("(k p) c -> p k c", p=128)
            half = max(1, kc_n // 2)
            for h0 in range(0, kc_n, half):
                h1 = min(kc_n, h0 + half)
                S.op('pool', lambda e, s=s, h0=h0, h1=h1: e.dma_start(out=wb[:, s, h0:h1, :ncols], in_=src[:, h0:h1, :]),
                     writes=[('wb', s)], dma='wb%d' % s)
            return s

        def mm_fm(wb, s, kc_n, csub, act_fn, n, b, extra_reads=()):
            for kc in range(kc_n):
                S.op('pe', lambda e, kc=kc: e.matmul(ps[:, b, :n], lhsT=wb[:, s, kc, csub * 128:(csub + 1) * 128], rhs=act_fn(kc),
                                                    start=(kc == 0), stop=(kc == kc_n - 1)),
                     reads=[('wb', s)] + list(extra_reads), writes=[('ps', b)])

        def rope_store(b, n, cs_t, t_off, dst_fn, tmpq, tmp1, key, extra_reads=()):
            i = key % 2
            S.op('dve', lambda e: e.tensor_copy(out=tmpq[:, i, :n], in_=ps[:, b, :n]), reads=[('ps', b)] + list(extra_reads), writes=[('tq', i)])
            b2 = nb()
            S.op('pe', lambda e: e.matmul(ps[:, b2, :n], lhsT=swp, rhs=tmpq[:, i, :n], start=True, stop=True),
                 reads=[('tq', i), 'cst'], writes=[('ps', b2)])
            S.op('dve', lambda e: e.tensor_tensor(out=tmp1[:, i, :n], in0=ps[:, b, :n], in1=cs_t[:, 0, t_off:t_off + n], op=ALU.mult),
                 reads=[('ps', b), 'cs'] + list(extra_reads), writes=[('t1', i)])
            S.op('dve', lambda e: e.tensor_tensor(out=tmp1[:, 2 + i, :n], in0=ps[:, b2, :n], in1=cs_t[:, 1, t_off:t_off + n], op=ALU.mult),
                 reads=[('ps', b2), 'cs'] + list(extra_reads), writes=[('t2', i)])
            dst, wkeys = dst_fn()
            S.op('dve', lambda e: e.tensor_tensor(out=dst, in0=tmp1[:, i, :n], in1=tmp1[:, 2 + i, :n], op=ALU.add),
                 reads=[('t1', i), ('t2', i)], writes=wkeys)

        for pss in range(2):
            with ExitStack() as st:
                wres = sbt(st, "wres", [128, KC, 1024], BF16)
                xsb = sbt(st, "xsb", [128, KC, 512], BF16)
                hT2 = [sbt(st, "hTa", [128, KC, 512], BF16), sbt(st, "hTb", [128, KC, 512], BF16)]
                sq = sbt(st, "sq", [128, 2, 512], BF16)
                rs = sbt(st, "rs", [128, 512], F32)
                ob = sbt(st, "ob", [128, 4, 512], BF16)
                tmpq = sbt(st, "tmpq", [128, 2, 512], BF16)
                tmp1 = sbt(st, "tmp1", [128, 4, 512], F32)
                cs_t = sbt(st, "cs_t", [128, 2, 2, 512], F32)
                c0 = C_KC if pss == 0 else C_KSL
                srcw = w_in[:, c0:c0 + 1024].rearrange("(k p) c -> p k c", p=128)
                for q in range(4):
                    S.op('pool', lambda e, q=q: e.dma_start(out=wres[:, 8 * q:8 * q + 8, :], in_=srcw[:, 8 * q:8 * q + 8, :]),
                         writes=['wres'], dma='wres')
                NT = S_ALL // 512

                def normA(tt):
                    t0 = tt * 512
                    for q in range(4):
                        S.op('pool', lambda e, q=q: e.dma_start(out=xsb[:, 8 * q:8 * q + 8, :], in_=xT_full_v[:, 8 * q:8 * q + 8, t0:t0 + 512]),
                             writes=[('xs', q)], dma='xs%d' % q)
                    if pss == 1:
                        S.op('sp', lambda e: e.dma_start(out=rs[:], in_=rstd_d[tt]), writes=['rs'], dma='rsi')
                        return
                    for c in range(KC):
                        S.op('act', lambda e, c=c: e.activation(out=sq[:, c % 2, :], in_=xsb[:, c, :], func=AF.Square),
                             reads=[('xs', c // 8)], writes=[('sq', c % 2)])
                        S.op('pe', lambda e, c=c: e.matmul(ps[:, 7, :], lhsT=ones, rhs=sq[:, c % 2, :], start=(c == 0), stop=(c == KC - 1)),
                             reads=[('sq', c % 2), 'cst'], writes=[('ps', 7)])
                    S.op('act', lambda e: e.activation(out=rs[:], in_=ps[:, 7, :], func=AF.Sqrt, scale=1.0 / D, bias=epsb[:]),
                         reads=[('ps', 7), 'epsb'], writes=['rs'])
                    S.op('dve', lambda e: e.reciprocal(out=rs[:], in_=rs[:]), reads=['rs'], writes=['rs'])
                    S.op('sp', lambda e: e.dma_start(out=rstd_d[tt], in_=rs[:]), reads=['rs'], dma='rso')

                def normB(tt):
                    h = hT2[tt % 2]
                    for c in range(KC):
                        S.op('dve', lambda e, c=c: e.scalar_tensor_tensor(out=h[:, c, :], in0=xsb[:, c, :], scalar=gcol(G_MIXPRE, c), in1=rs[:], op0=ALU.mult, op1=ALU.mult),
                             reads=[('xs', c // 8), 'rs', 'cst'], writes=[('hT', tt % 2, c)])

                def unit_fm(tt, ct):
                    hT = hT2[tt % 2]
                    hreads = [('hT', tt % 2, c) for c in range(KC)]
                    t0 = tt * 512
                    b = nb()
                    for kc in range(KC):
                        S.op('pe', lambda e, kc=kc: e.matmul(ps[:, b, :], lhsT=wres[:, kc, ct * 128:(ct + 1) * 128], rhs=hT[:, kc, :],
                                                             start=(kc == 0), stop=(kc == KC - 1)),
                             reads=['wres'] + (hreads if kc == 0 else []), writes=[('ps', b)])
                    o = ct % 4
                    if pss == 0:
                        S.op('act', lambda e: e.copy(out=ob[:, o, :], in_=ps[:, b, :]), reads=[('ps', b)], writes=[('ob', o)])
                        S.op('sp', lambda e: e.dma_start(out=kcT_d[ct, :, t0:t0 + 512], in_=ob[:, o, :]), reads=[('ob', o)], dma='ob%d' % o)
                    else:
                        rope_store(b, 512, cs_t[:, tt % 2], 0, lambda: (ob[:, o, :], [('ob', o)]), tmpq, tmp1, ct, [('cs', tt % 2)])
                        S.op('sp', lambda e: e.dma_start(out=kslT_d[ct, :, t0:t0 + 512], in_=ob[:, o, :]), reads=[('ob', o)], dma='ob%d' % o)

                def unit_tm(tt, tk):
                    hT = hT2[tt % 2]
                    hreads = [('hT', tt % 2, c) for c in range(KC)]
                    t0 = tt * 512
                    b = nb()
                    for kc in range(KC):
                        S.op('pe', lambda e, kc=kc: e.matmul(ps[:, b, :], lhsT=hT[:, kc, tk * 128:(tk + 1) * 128], rhs=wres[:, kc, 512:1024],
                                                             start=(kc == 0), stop=(kc == KC - 1)),
                             reads=['wres'] + (hreads if kc == 0 else []), writes=[('ps', b)])
                    S.op('act', lambda e: e.copy(out=ob[:, tk, :], in_=ps[:, b, :]), reads=[('ps', b)], writes=[('ob', tk)])
                    S.op('sp', lambda e: e.dma_start(out=vsl_d[t0 + tk * 128:t0 + (tk + 1) * 128, :], in_=ob[:, tk, :]), reads=[('ob', tk)], dma='ob%d' % tk)

                normA(0)
                normB(0)
                for tt in range(NT):
                    if pss == 1:
                        S.op('sp', lambda e, tt=tt: e.dma_start(out=cs_t[:, tt % 2], in_=cs_full[:, :, tt * 512:(tt + 1) * 512]), writes=[('cs', tt % 2)], dma='cs%d' % (tt % 2))
                        units = [(unit_fm, ct) for ct in range(4)] + [(unit_tm, tk) for tk in range(4)]
                    else:
                        units = [(unit_fm, ct) for ct in range(8)]
                    for fn_, a_ in units[:4]:
                        fn_(tt, a_)
                    if tt + 1 < NT:
                        normA(tt + 1)
                        normB(tt + 1)
                    for fn_, a_ in units[4:]:
                        fn_(tt, a_)
                S.flush()

        with ExitStack() as st:
            kin = sbt(st, "kin", [128, 2, S_ALL], BF16)
            w1 = sbt(st, "w1", [128, 2, 32, 128], BF16)
            w2 = sbt(st, "w2", [128, 2, 128], BF16)
            pe_b = sbt(st, "pe_b", [128, 2, 32], BF16)
            pe2 = sbt(st, "pe2", [128, 2, 32, 2], BF16)
            cb = sbt(st, "cb", [128, 2], F32)
            xg = sbt(st, "xg", [128, 2, 512], F32)
            tg = sbt(st, "tg", [128, 2, 512], F32)
            ge = sbt(st, "ge", [128, 2, 512], BF16)
            for kv, (wa1, wa2, pea) in enumerate(((w_ck1, w_ck2, peT_k), (w_cv1, w_cv2, peT_v))):
                S.op('pool', lambda e, kv=kv, wa1=wa1: e.dma_start(out=w1[:, kv], in_=wa1.rearrange("(l d) h -> d l h", d=128)), writes=['w1'], dma='c')
                S.op('pool', lambda e, kv=kv, wa2=wa2: e.dma_start(out=w2[:, kv], in_=wa2), writes=['w1'], dma='c')
                S.op('pool', lambda e, kv=kv, pea=pea: e.dma_start(out=pe_b[:, kv], in_=pea), writes=['w1'], dma='c')
            for j2 in range(2):
                S.op('dve', lambda e, j2=j2: e.tensor_copy(out=pe2[:, :, :, j2], in_=pe_b[:]), reads=['w1'], writes=[('pe2', j2)])
            for kv in range(2):
                b = nb()
                for l in range(32):
                    S.op('pe', lambda e, l=l, kv=kv, b=b: e.matmul(ps[:, b, 0:2], lhsT=w1[:, kv, l, :], rhs=pe2[:, kv, l, :], start=(l == 0), stop=(l == 31)),
                         reads=['w1', ('pe2', 0), ('pe2', 1)], writes=[('ps', b)])
                S.op('dve', lambda e, kv=kv, b=b: e.tensor_copy(out=cb[:, kv:kv + 1], in_=ps[:, b, 0:1]), reads=[('ps', b)], writes=[('cb', kv)])
            NCMP = 511
            for g in range(4):
                for kv in range(2):
                    i = (g * 2 + kv) % 2
                    S.op('sp', lambda e, g=g, kv=kv, i=i: e.dma_start(out=kin[:, i, :], in_=kcT_d[kv * 4 + g]), writes=[('kin', i)], dma='kin%d' % i)
                    b = nb()
                    for l in range(32):
                        S.op('pe', lambda e, l=l, kv=kv, i=i, b=b: e.matmul(ps[:, b, :NCMP], lhsT=w1[:, kv, l, :], rhs=kin[:, i, l:l + 16 * (NCMP - 1) + 1:16],
                                                                      start=(l == 0), stop=(l == 31)),
                             reads=['w1', ('kin', i)], writes=[('ps', b)])
                    S.op('dve', lambda e, kv=kv, i=i, b=b: e.tensor_scalar(out=xg[:, i, :NCMP], in0=ps[:, b, :NCMP], scalar1=cb[:, kv:kv + 1], scalar2=None, op0=ALU.add),
                         reads=[('ps', b), ('cb', kv)], writes=[('xg', i)])
                    S.op('dve', lambda e, i=i: e.tensor_tensor(out=tg[:, i, :NCMP], in0=xg[:, i, :NCMP], in1=xg[:, i, :NCMP], op=ALU.mult),
                         reads=[('xg', i)], writes=[('tg', i)])
                    S.op('dve', lambda e, i=i: e.tensor_scalar(out=tg[:, i, :NCMP], in0=tg[:, i, :NCMP], scalar1=0.044715, scalar2=1.0, op0=ALU.mult, op1=ALU.add),
                         reads=[('tg', i)], writes=[('tg', i)])
                    S.op('dve', lambda e, i=i: e.tensor_tensor(out=tg[:, i, :NCMP], in0=tg[:, i, :NCMP], in1=xg[:, i, :NCMP], op=ALU.mult),
                         reads=[('tg', i), ('xg', i)], writes=[('tg', i)])
                    S.op('act', lambda e, i=i: e.activation(out=tg[:, i, :NCMP], in_=tg[:, i, :NCMP], func=AF.Sigmoid, scale=1.5957691216057308),
                         reads=[('tg', i)], writes=[('tg', i)])
                    S.op('dve', lambda e, i=i: e.tensor_tensor(out=ge[:, i, :NCMP], in0=tg[:, i, :NCMP], in1=xg[:, i, :NCMP], op=ALU.mult),
                         reads=[('tg', i), ('xg', i)], writes=[('ge', i)])
                    if kv == 0:
                        b2 = nb()
                        S.op('pe', lambda e, i=i, b2=b2: e.matmul(ps[:, b2, :NCMP], lhsT=w2[:, 0, :], rhs=ge[:, i, :NCMP], start=True, stop=True),
                             reads=['w1', ('ge', i)], writes=[('ps', b2)])
                        S.op('act', lambda e, g=g, b2=b2: e.copy(out=kc_c[:, g, :NCMP], in_=ps[:, b2, :NCMP]), reads=[('ps', b2)], writes=['kc_c'])
                    else:
                        for j in range(4):
                            m = 128 if j < 3 else 127
                            b2 = nb()
                            S.op('pe', lambda e, i=i, j=j, m=m, b2=b2: e.matmul(ps[:m, b2, :128], lhsT=ge[:, i, j * 128:j * 128 + m], rhs=w2[:, 1, :], start=True, stop=True),
                                 reads=['w1', ('ge', i)], writes=[('ps', b2)])
                            S.op('act', lambda e, g=g, j=j, m=m, b2=b2: e.copy(out=vc_c[:m, g, j, :], in_=ps[:m, b2, :128]), reads=[('ps', b2)], writes=['vc_c'])
            S.flush()

        for half in range(3):
            with ExitStack() as st:
                hT3 = [sbt(st, "hT3_%d" % i, [128, KC, 512], BF16) for i in range(2)]
                xs = sbt(st, "xs", [128, KC, 512], F32)
                sq = sbt(st, "sq", [128, 2, 512], BF16)
                rs = sbt(st, "rs", [128, 512], F32)
                wb = sbt(st, "wb", [128, 2, KC, 256], BF16)
                ob = sbt(st, "ob", [128, 4, 512], BF16)
                tmpq = sbt(st, "tmpq", [128, 2, 512], BF16)
                tmp1 = sbt(st, "tmp1", [128, 4, 512], F32)
                cs_t = sbt(st, "cs_t", [128, 2, 1024], F32)
                tb = half * 1024
                S.op('sp', lambda e, tb=tb: e.dma_start(out=cs_t[:], in_=cs_ext[:, :, tb:tb + 1024]), writes=['cs'], dma='cs')
                for i in range(2):
                    norm_tile(st, xs, hT3[i], xT_ext_v, tb + i * 512, G_MIXPRE, sq, rs)
                hr = [[('hT', id(hT3[i]), c) for c in range(KC)] for i in range(2)]
                okey = [0]

                def fm_cols(c0, ncols, tiles, sink):
                    for w0 in range(0, ncols, 256):
                        wn = min(256, ncols - w0)
                        s = load_w(wb, w_in, 0, KC, c0 + w0, wn)
                        for i in tiles:
                            for cs_ in range((wn + 127) // 128):
                                mrows = min(128, wn - cs_ * 128)
                                b = nb()
                                for kc in range(KC):
                                    S.op('pe', lambda e, kc=kc, s=s, cs_=cs_, i=i, b=b, mrows=mrows: e.matmul(
                                        ps[:mrows, b, :], lhsT=wb[:, s, kc, cs_ * 128:cs_ * 128 + mrows], rhs=hT3[i][:, kc, :],
                                        start=(kc == 0), stop=(kc == KC - 1)),
                                        reads=[('wb', s)] + (hr[i] if kc == 0 else []), writes=[('ps', b)])
                                sink((w0 + cs_ * 128) // 128, i, b, mrows)

                def tm_cols(c0, ncols, tiles, dst_d, dcol0):
                    for w0 in range(0, ncols, 256):
                        s = load_w(wb, w_in, 0, KC, c0 + w0, 256)
                        for i in tiles:
                            for tk in range(4):
                                b = nb()
                                for kc in range(KC):
                                    S.op('pe', lambda e, kc=kc, s=s, i=i, tk=tk, b=b: e.matmul(
                                        ps[:, b, :256], lhsT=hT3[i][:, kc, tk * 128:(tk + 1) * 128], rhs=wb[:, s, kc, :],
                                        start=(kc == 0), stop=(kc == KC - 1)),
                                        reads=[('wb', s)] + (hr[i] if kc == 0 else []), writes=[('ps', b)])
                                o = okey[0] % 4
                                okey[0] += 1
                                S.op('act', lambda e, b=b, o=o: e.copy(out=ob[:, o, :256], in_=ps[:, b, :256]), reads=[('ps', b)], writes=[('ob', o)])
                                r0 = tb + i * 512 + tk * 128
                                S.op('sp', lambda e, o=o, r0=r0, w0=w0: e.dma_start(out=dst_d[r0:r0 + 128, dcol0 + w0:dcol0 + w0 + 256], in_=ob[:, o, :256]),
                                     reads=[('ob', o)], dma='ob%d' % o)

                def sink_rope_dram(dst_d, ct_off=0):
                    def sink(ct, i, b, mrows):
                        o = okey[0] % 4
                        okey[0] += 1
                        rope_store(b, 512, cs_t, i * 512, lambda o=o: (ob[:, o, :], [('ob', o)]), tmpq, tmp1, okey[0])
                        t0 = tb + i * 512
                        S.op('sp', lambda e, ct=ct, o=o, t0=t0: e.dma_start(out=dst_d[ct_off + ct, :, t0:t0 + 512], in_=ob[:, o, :]),
                             reads=[('ob', o)], dma='ob%d' % o)
                    return sink

                if half == 0:
                    fm_cols(C_KA + 1024, 512, range(2), sink_rope_dram(kaT_d, 8))
                    tm_cols(C_VA + 1024, 512, range(2), va_d, 1024)
                elif half == 1:
                    fm_cols(C_KA, 1024, (1,), sink_rope_dram(kaT_d))
                    fm_cols(C_KA + 1024, 512, range(2), sink_rope_dram(kaT_d, 8))
                    fm_cols(C_KWN, 512, (1,), sink_rope_dram(kwnT_d))
                    tm_cols(C_VA, 1024, (1,), va_d, 0)
                    tm_cols(C_VA + 1024, 512, range(2), va_d, 1024)
                    tm_cols(C_VWN, 512, (1,), vwn_d, 0)
                else:
                    fm_cols(C_KA, 1536, range(2), sink_rope_dram(kaT_d))
                    fm_cols(C_KWN, 512, range(2), sink_rope_dram(kwnT_d))
                    tm_cols(C_VA, 1536, range(2), va_d, 0)
                    tm_cols(C_VWN, 512, range(2), vwn_d, 0)
                if half == 2:
                    own = (0, 1)

                    def sink_q(dst_d, extra=()):
                        def sink(ct, i, b, mrows):
                            t0 = i * 512
                            o = okey[0] % 4
                            okey[0] += 1
                            rope_store(b, 512, cs_t, i * 512, lambda o=o: (ob[:, o, :], [('ob', o)]), tmpq, tmp1, okey[0], extra)
                            S.op('sp', lambda e: e.dma_start(out=dst_d[ct, :, t0:t0 + 512], in_=ob[:, o, :]), reads=[('ob', o)], dma='ob%d' % o)
                        return sink
                    fm_cols(C_QA, 1536, own, sink_q(qaT_d))

                    def sink_qb(ct, i, b, mrows):
                        t0 = i * 512
                        o = okey[0] % 4
                        okey[0] += 1
                        S.op('act', lambda e: e.copy(out=ob[:, o, :], in_=ps[:, b, :]), reads=[('ps', b)], writes=[('ob', o)])
                        S.op('sp', lambda e: e.dma_start(out=qrT_d[ct, :, t0:t0 + 512], in_=ob[:, o, :]), reads=[('ob', o)], dma='ob%d' % o)
                        sink_q(qoT_d, [('ob', o)])(ct, i, b, mrows)
                    fm_cols(C_QB, 2048, own, sink_qb)

                    def sink_gn(ct, i, b, mrows):
                        t0 = i * 512
                        S.op('act', lambda e: e.activation(out=gsT[:, t0:t0 + 512], in_=ps[:48, b, :], func=AF.Sigmoid), reads=[('ps', b)], writes=['gsT'])
                    fm_cols(C_GN, 48, own, sink_gn)

                    def sink_gm(ct, i, b, mrows):
                        t0 = i * 512
                        o = okey[0] % 4
                        okey[0] += 1
                        S.op('act', lambda e: e.activation(out=ob[:, o, :], in_=ps[:, b, :], func=AF.Sigmoid), reads=[('ps', b)], writes=[('ob', o)])
                        S.op('sp', lambda e: e.dma_start(out=gmix_d[ct, :, t0:t0 + 512], in_=ob[:, o, :]), reads=[('ob', o)], dma='ob%d' % o)
                    fm_cols(C_GM, 8192, own, sink_gm)
                S.flush()

        acc_i = [0]

        def attn_chunks(chunks, nq, n_heads_cols, o_bank, d_bank):
            ncols = n_heads_cols
            nchunks = len(chunks)

            def emit_pv(ci, ch, pi):
                nk = ch['nk']
                for h in range(4):
                    v_ap, rd = ch['v_fn'](h)
                    S.op('pe', lambda e, v_ap=v_ap, h=h, nk=nk, pi=pi, ci=ci: e.matmul(
                        ps[:, o_bank, h * nq:(h + 1) * nq], lhsT=v_ap, rhs=PT[:nk, pi, h * nq:(h + 1) * nq],
                        start=(ci == 0 and h == 0), stop=(ci == nchunks - 1)), reads=rd + [('PT', pi)], writes=[('ps', o_bank)])
                S.op('pe', lambda e, nk=nk, pi=pi, ci=ci: e.matmul(ps[:, d_bank, :ncols], lhsT=cst[:nk, 2, :], rhs=PT[:nk, pi, :ncols],
                                                                start=(ci == 0), stop=(ci == nchunks - 1)),
                     reads=[('PT', pi), 'cst'], writes=[('ps', d_bank)])

            pend = None
            for ci, ch in enumerate(chunks):
                nk = ch['nk']
                b = nb()
                ops_ = ch['s_ops']
                for oi, (l_ap, r_ap, o_ap, rd) in enumerate(ops_):
                    S.op('pe', lambda e, l_ap=l_ap, r_ap=r_ap, o_ap=o_ap, b=b, st_=ch['starts'][oi], sp_=ch['stops'][oi]: e.matmul(
                        o_ap(b), lhsT=l_ap, rhs=r_ap, start=st_, stop=sp_), reads=rd, writes=[('ps', b)])
                pi = acc_i[0] % 3
                acc_i[0] += 1
                S.op('act', lambda e, b=b, nk=nk, pi=pi: e.activation(out=PT[:nk, pi, :ncols], in_=ps[:nk, b, :ncols], func=AF.Exp, scale=SCALE),
                     reads=[('ps', b)], writes=[('PT', pi)])
                if ch.get('keep') is not None:
                    ch['keep'](pi, nk)
                if os.environ.get('MK_OLDATTN'):
                    emit_pv(ci, ch, pi)
                    continue
                if pend is not None:
                    emit_pv(*pend)
                pend = (ci, ch, pi)
            if pend is not None:
                emit_pv(*pend)

        with ExitStack() as st:
            PT = sbt(st, "PT", [128, 3, 512], BF16)
            kaS = sbt(st, "kaS", [128, 4, EXT], BF16)
            vS = sbt(st, "vS", [128, 4, 512], BF16)
            bD = sbt(st, "bD", [128, 22, 512], BF16)
            numT = sbt(st, "numT", [128, 4, T], F32)
            denT = sbt(st, "denT", [128, 4, T], F32)
            qaT = sbt(st, "qaT", [128, 4, T], BF16)
            yaT = sbt(st, "yaT", [128, 4, T], BF16)
            S.op('sp', lambda e: e.dma_start(out=bD[:], in_=biasD), writes=['bD'], dma='c')
            vslot = [0]
            for grp, r in enumerate((1, 4, 16)):
                for h in range(4):
                    S.op('sp', lambda e, grp=grp, h=h: e.dma_start(out=kaS[:, h, :], in_=kaT_d[grp * 4 + h]), writes=['kaS'], dma='kaS')
                    S.op('sp', lambda e, grp=grp, h=h: e.dma_start(out=qaT[:, h, :], in_=qaT_d[grp * 4 + h]), writes=['qaT'], dma='qaT')
                nq = 128 if r < 16 else 64
                nblk = (T // r) // nq
                for rho in range(r):
                    for blk in range(nblk):
                        a0 = blk * nq
                        if grp == 0:
                            bidx = [blk * 2, blk * 2 + 1]
                        elif grp == 1:
                            bidx = [16 + blk * 2, 16 + blk * 2 + 1]
                        else:
                            bidx = [20, 21]
                        chunks = []
                        for ci, (ks, nk) in enumerate(((a0 - 128, 128), (a0, nq))):
                            e0 = HALO + r * ks + rho
                            vs = vslot[0] % 4
                            vslot[0] += 1
                            S.op('sp', lambda e, e0=e0, nk=nk, vs=vs, grp=grp, r=r: e.dma_start(
                                out=vS[:nk, vs, :], in_=va_d[e0:e0 + r * (nk - 1) + 1:r, grp * 512:(grp + 1) * 512]),
                                writes=[('vS', vs)], dma='vS%d' % vs)
                            q0 = rho + r * a0
                            s_ops = [(cst[:nk, 0, :nk], bD[:nk, bidx[ci], :4 * nq],
                                      (lambda b, nk=nk, nq=nq: ps[:nk, b, :4 * nq]), ['bD', 'cst'])]
                            for h in range(4):
                                s_ops.append((kaS[:, h, e0:e0 + r * (nk - 1) + 1:r],
                                              qaT[:, h, q0:q0 + r * (nq - 1) + 1:r],
                                              (lambda b, h=h, nk=nk, nq=nq: ps[:nk, b, h * nq:(h + 1) * nq]),
                                              ['kaS', 'qaT']))
                            chunks.append(dict(nk=nk, s_ops=s_ops, starts=[True] + [False] * 4, stops=[False] * 4 + [True],
                                               v_fn=(lambda h, vs=vs, nk=nk: (vS[:nk, vs, h * 128:(h + 1) * 128], [('vS', vs)]))))
                        ob_, db_ = (4, 5) if (acc_i[0] // 2) % 2 == 0 else (6, 7)
                        attn_chunks(chunks, nq, 4 * nq, ob_, db_)
                        q0 = rho + r * a0
                        dstn = numT[:, :, q0:q0 + r * (nq - 1) + 1:r]
                        dstd = denT[:, :, q0:q0 + r * (nq - 1) + 1:r]
                        srcn = ps[:, ob_, :4 * nq].rearrange("p (h q) -> p h q", h=4)
                        srcd = ps[:, db_, :4 * nq].rearrange("p (h q) -> p h q", h=4)
                        if grp == 0:
                            S.op('dve', lambda e, dstn=dstn, srcn=srcn: e.tensor_copy(out=dstn, in_=srcn), reads=[('ps', ob_)], writes=['numT'])
                            S.op('act', lambda e, dstd=dstd, srcd=srcd: e.copy(out=dstd, in_=srcd), reads=[('ps', db_)], writes=['denT'])
                        else:
                            S.op('dve', lambda e, dstn=dstn, srcn=srcn: e.tensor_tensor(out=dstn, in0=dstn, in1=srcn, op=ALU.add), reads=[('ps', ob_), 'numT'], writes=['numT'])
                            S.op('dve', lambda e, dstd=dstd, srcd=srcd: e.tensor_tensor(out=dstd, in0=dstd, in1=srcd, op=ALU.add), reads=[('ps', db_), 'denT'], writes=['denT'])
            S.op('dve', lambda e: e.reciprocal(out=denT[:], in_=denT[:]), reads=['denT'], writes=['denT'])
            S.op('dve', lambda e: e.tensor_tensor(out=yaT[:], in0=numT[:], in1=denT[:], op=ALU.mult), reads=['numT', 'denT'], writes=['yaT'])
            S.op('sp', lambda e: e.dma_start(out=ya_d.rearrange("h p t -> p h t"), in_=yaT[:]), reads=['yaT'], dma='c')
            S.flush()

        with ExitStack() as st:
            PT = sbt(st, "PT", [128, 3, 512], BF16)
            PK = sbt(st, "PK", [128, 4, 512], BF16)
            Ksel = sbt(st, "Ksel", [128, S_ALL], BF16)
            Vsel = sbt(st, "Vsel", [128, 64, 128], BF16)
            Kw = sbt(st, "Kw", [128, 1536], BF16)
            Vw = sbt(st, "Vw", [128, 12, 128], BF16)
            xs_sb = sbt(st, "xs_sb", [128, S_ALL], BF16)
            ov_sb = sbt(st, "ov_sb", [128, 4, 128], BF16)
            gs_sb = sbt(st, "gs_sb", [48, 48, 128], BF16)
            bC = sbt(st, "bC", [128, 4, 512], BF16)
            bW = sbt(st, "bW", [128, 5, 512], BF16)
            dg = sbt(st, "dg", [128, 8, 512], BF16)
            aF = sbt(st, "aF", [128, 128], F32)
            rden = sbt(st, "rden", [128, 512], F32)
            pn = sbt(st, "pn", [128, 2, 512], BF16)
            sc = sbt(st, "sc", [128, 128], F32)
            wk = sbt(st, "wk", [128, 128], F32)
            mx = sbt(st, "mx", [128, 16], F32)
            selb = sbt(st, "selb", [128, 128], BF16)
            selT = sbt(st, "selT", [128, 512], BF16)
            grep = sbt(st, "grep", [128, 512], F32)
            acc = sbt(st, "acc", [128, 512], F32)
            tmpo = sbt(st, "tmpo", [128, 512], F32)
            ybo = sbt(st, "ybo", [128, 512], BF16)
            qrT = sbt(st, "qrT", [128, 4, T], BF16)
            qoT = sbt(st, "qoT", [128, 4, T], BF16)
            S.op('sp', lambda e: e.dma_start(out=xs_sb[:], in_=xsel), writes=['k2'], dma='c')
            S.op('sp', lambda e: e.dma_start(out=ov_sb[:], in_=ovm), writes=['k2'], dma='c')
            S.op('sp', lambda e: e.dma_start(out=gs_sb[:], in_=gsel), writes=['k2'], dma='c')
            S.op('sp', lambda e: e.dma_start(out=dg[:], in_=diagb), writes=['k2'], dma='c')

            def finish_part(part, g, ob_, db_, first, QB0):
                S.op('dve', lambda e: e.tensor_scalar(out=rden[:], in0=ps[:, db_, :], scalar1=1e-30, scalar2=None, op0=ALU.max),
                     reads=[('ps', db_)], writes=['rden'])
                S.op('dve', lambda e: e.reciprocal(out=rden[:], in_=rden[:]), reads=['rden'], writes=['rden'])
                bg = nb()
                for h in range(4):
                    col = (g * 4 + h) * 3 + part
                    S.op('pe', lambda e, h=h, col=col: e.matmul(ps[:, bg, h * 128:(h + 1) * 128], lhsT=gs_sb[:, col, :], rhs=gsT[:, QB0:QB0 + 128], start=(h == 0), stop=True),
                         reads=['k2', 'gsT'], writes=[('ps', bg)])
                S.op('dve', lambda e: e.tensor_tensor(out=grep[:], in0=ps[:, bg, :], in1=rden[:], op=ALU.mult), reads=[('ps', bg), 'rden'], writes=['grep'])
                if first:
                    S.op('dve', lambda e: e.tensor_tensor(out=acc[:], in0=ps[:, ob_, :], in1=grep[:], op=ALU.mult), reads=[('ps', ob_), 'grep'], writes=['acc'])
                else:
                    S.op('dve', lambda e: e.tensor_tensor(out=tmpo[:], in0=ps[:, ob_, :], in1=grep[:], op=ALU.mult), reads=[('ps', ob_), 'grep'], writes=['tmpo'])
                    S.op('dve', lambda e: e.tensor_tensor(out=acc[:], in0=acc[:], in1=tmpo[:], op=ALU.add), reads=['acc', 'tmpo'], writes=['acc'])

            def nsa_block(g, qb):
                QB0 = qb * 128
                S.op('sp', lambda e, qb=qb: e.dma_start(out=bC[:], in_=biasC[qb]), writes=['bC'], dma='bC')
                S.op('sp', lambda e, qb=qb: e.dma_start(out=bW[:], in_=biasW[qb]), writes=['bW'], dma='bW')
                S.op('sp', lambda e, qb=qb: e.dma_start(out=aF[:], in_=addF[qb]), writes=['aF'], dma='aF')
                qr4 = qrT[:, :, QB0:QB0 + 128]
                qo4 = qoT[:, :, QB0:QB0 + 128]
                qrd = ['qrT']
                qod = ['qoT']
                full = lambda b: ps[:, b, :]
                chunks = []
                for j in range(4):
                    def keep(pi, nk, j=j):
                        S.op('pool', lambda e, pi=pi, j=j: e.tensor_copy(out=PK[:, j, :], in_=PT[:, pi, :]), reads=[('PT', pi)], writes=[('PK', j)])
                    chunks.append(dict(nk=128, starts=[True, False], stops=[False, True], keep=keep,
                                       s_ops=[(kc_c[:, g, j * 128:(j + 1) * 128], qr4, full, ['kc_c'] + qrd),
                                              (ident, bC[:, j, :], full, ['bC', 'cst'])],
                                       v_fn=(lambda h, j=j, g=g: (vc_c[:, g, j, :], ['vc_c']))))
                attn_chunks(chunks, 128, 512, 4, 5)
                finish_part(0, g, 4, 5, True, QB0)
                bi = nb()
                for j in range(4):
                    S.op('dve', lambda e, j=j: e.tensor_tensor(out=pn[:, j % 2, :], in0=PK[:, j, :], in1=rden[:], op=ALU.mult),
                         reads=[('PK', j), 'rden'], writes=[('pn', j % 2)])
                    for h in range(4):
                        S.op('pe', lambda e, j=j, h=h: e.matmul(ps[:, bi, :128], lhsT=pn[:, j % 2, h * 128:(h + 1) * 128], rhs=ov_sb[:, j, :],
                                                               start=(j == 0 and h == 0), stop=(j == 3 and h == 3)),
                             reads=[('pn', j % 2), 'k2'], writes=[('ps', bi)])
                S.op('dve', lambda e: e.tensor_tensor(out=sc[:], in0=ps[:, bi, :128], in1=aF[:], op=ALU.add), reads=[('ps', bi), 'aF'], writes=['sc'])
                S.op('dve', lambda e: e.max(out=mx[:, 0:8], in_=sc[:]), reads=['sc'], writes=['mx'])
                S.op('dve', lambda e: e.match_replace(out=wk[:], in_to_replace=mx[:, 0:8], in_values=sc[:], imm_value=-1e30), reads=['sc', 'mx'], writes=['wk'])
                S.op('dve', lambda e: e.max(out=mx[:, 8:16], in_=wk[:]), reads=['wk'], writes=['mx'])
                S.op('dve', lambda e: e.tensor_scalar(out=mx[:, 15:16], in0=mx[:, 15:16], scalar1=-1e29, scalar2=None, op0=ALU.max), reads=['mx'], writes=['mx'])
                S.op('dve', lambda e: e.tensor_scalar(out=selb[:], in0=sc[:], scalar1=mx[:, 15:16], scalar2=NEG, op0=ALU.is_lt, op1=ALU.mult),
                     reads=['sc', 'mx'], writes=['selb'])
                bt = nb()
                S.op('pe', lambda e: e.matmul(ps[:, bt, :128], lhsT=selb[:], rhs=ident, start=True, stop=True), reads=['selb', 'cst'], writes=[('ps', bt)])
                for h in range(4):
                    S.op('dve', (lambda e, h=h: e.tensor_copy(out=selT[:, h * 128:(h + 1) * 128], in_=ps[:, bt, :128])),
                         reads=[('ps', bt)], writes=[('selT', h)])
                selrd = [('selT', h) for h in range(4)]
                chunks = []
                nch = 57 + qb
                for j in range(nch):
                    s_ops = [(Ksel[:, j * 128:(j + 1) * 128], qo4, full, ['Ksel'] + qod),
                             (xs_sb[:, j * 128:(j + 1) * 128], selT[:], full, ['k2'] + selrd)]
                    if j % 8 == qb:
                        s_ops.append((ident, dg[:, j // 8, :], full, ['k2', 'cst']))
                    n = len(s_ops)
                    chunks.append(dict(nk=128, s_ops=s_ops, starts=[True] + [False] * (n - 1), stops=[False] * (n - 1) + [True],
                                       v_fn=(lambda h, j=j: (Vsel[:, j, :], ['Vsel']))))
                attn_chunks(chunks, 128, 512, 6, 7)
                finish_part(1, g, 6, 7, False, QB0)
                chunks = []
                for i in range(5):
                    kj = qb + i
                    chunks.append(dict(nk=128, starts=[True, False], stops=[False, True],
                                       s_ops=[(Kw[:, kj * 128:(kj + 1) * 128], qo4, full, ['Kw'] + qod),
                                              (ident, bW[:, i, :], full, ['bW', 'cst'])],
                                       v_fn=(lambda h, kj=kj: (Vw[:, kj, :], ['Vw']))))
                attn_chunks(chunks, 128, 512, 4, 5)
                finish_part(2, g, 4, 5, False, QB0)
                S.op('act', lambda e: e.copy(out=ybo[:], in_=acc[:]), reads=['acc'], writes=['ybo'])
                S.op('sp', lambda e: e.dma_start(out=yb_d[4 * g:4 * g + 4, :, QB0:QB0 + 128].rearrange("h p q -> p h q"), in_=ybo[:].rearrange("p (h q) -> p h q", h=4)), reads=['ybo'], dma='ybo')

            for g in range(4):
                S.op('sp', lambda e, g=g: e.dma_start(out=Ksel[:], in_=kslT_d[g]), writes=['Ksel'], dma='Ksel')
                for jj in range(8):
                    S.op('sp', lambda e, g=g, jj=jj: e.dma_start(out=Vsel[:, 8 * jj:8 * jj + 8, :], in_=vsl_d[1024 * jj:1024 * jj + 1024, g * 128:(g + 1) * 128].rearrange("(j k) d -> k j d", k=128)), writes=['Vsel'], dma='Vsel')
                S.op('sp', lambda e, g=g: e.dma_start(out=qrT[:], in_=qrT_d[4 * g:4 * g + 4].rearrange("h p t -> p h t")), writes=['qrT'], dma='qrT')
                S.op('sp', lambda e, g=g: e.dma_start(out=qoT[:], in_=qoT_d[4 * g:4 * g + 4].rearrange("h p t -> p h t")), writes=['qoT'], dma='qoT')
                S.op('sp', lambda e, g=g: e.dma_start(out=Kw[:], in_=kwnT_d[g, :, HALO - 512:EXT]), writes=['Kw'], dma='Kw')
                S.op('sp', lambda e, g=g: e.dma_start(out=Vw[:], in_=vwn_d[HALO - 512:EXT, g * 128:(g + 1) * 128].rearrange("(j k) d -> k j d", k=128)), writes=['Vw'], dma='Vw')
                for qb in range(8):
                    nsa_block(g, qb)
            S.flush()

        xo_v = xT_ext_v

        def epilogue(st, sums_banks, g_post, xin_fn, xout_d, g_next, hN, final_out=None):
            rsb = sbt(st, "rsb", [128, 2, 512], F32)
            hb = sbt(st, "hb", [128, 2, 512], BF16)
            pv = sbt(st, "pv", [128, 2, 512], F32)
            xv = sbt(st, "xv", [128, 2, 512], F32)
            sq2 = sbt(st, "sq2", [128, 2, 512], BF16)
            for tt in range(2):
                S.op('act', lambda e, tt=tt: e.activation(out=rsb[:, tt, :], in_=ps[:, sums_banks[tt], :], func=AF.Sqrt, scale=1.0 / D, bias=epsb[:]),
                     reads=[('ps', sums_banks[tt]), 'epsb'], writes=[('rsb', tt)])
                S.op('dve', lambda e, tt=tt: e.reciprocal(out=rsb[:, tt, :], in_=rsb[:, tt, :]), reads=[('rsb', tt)], writes=[('rsb', tt)])
            for tt in range(2):
                t0 = tt * 512
                for c in range(KC):
                    i = c % 2
                    S.op('sp', lambda e, c=c, i=i, t0=t0: e.dma_start(out=pv[:, i, :], in_=pre_d[c, :, t0:t0 + 512]), writes=[('pv', i)], dma='pv%d' % i)
                    S.op('sp', lambda e, c=c, i=i, t0=t0: e.dma_start(out=xv[:, i, :], in_=xin_fn(c, t0)), writes=[('xv', i)], dma='xv%d' % i)
                    S.op('dve', lambda e, c=c, i=i, tt=tt: e.scalar_tensor_tensor(out=pv[:, i, :], in0=pv[:, i, :], scalar=gcol(g_post, c), in1=rsb[:, tt, :], op0=ALU.mult, op1=ALU.mult),
                         reads=[('pv', i), ('rsb', tt), 'cst'], writes=[('pv', i)])
                    S.op('dve', lambda e, i=i: e.tensor_tensor(out=xv[:, i, :], in0=xv[:, i, :], in1=pv[:, i, :], op=ALU.add),
                         reads=[('pv', i), ('xv', i)], writes=[('xv', i)])
                    dst = final_out if final_out is not None else xout_d
                    S.op('sp', lambda e, c=c, i=i, t0=t0, dst=dst: e.dma_start(
                        out=(dst[c * 128:(c + 1) * 128, t0:t0 + 512] if final_out is not None else dst[c, :, t0:t0 + 512]), in_=xv[:, i, :]),
                        reads=[('xv', i)], dma='xo%d' % i)
                    if hN is not None:
                        S.op('act', lambda e, i=i: e.activation(out=sq2[:, i, :], in_=xv[:, i, :], func=AF.Square), reads=[('xv', i)], writes=[('sq2', i)])
                        S.op('pe', lambda e, c=c, i=i, tt=tt: e.matmul(ps[:, sums_banks[tt], :], lhsT=ones, rhs=sq2[:, i, :], start=(c == 0), stop=(c == KC - 1)),
                             reads=[('sq2', i), 'cst'] + ([('rsb', tt)] if c == 0 else []), writes=[('ps', sums_banks[tt])])
                        S.op('act', lambda e, c=c, i=i: e.activation(out=hb[:, i, :], in_=xv[:, i, :], func=AF.Copy, scale=gcol(g_next, c)),
                             reads=[('xv', i), 'cst'], writes=[('hb', i)])
                        S.op('sp', lambda e, c=c, i=i, t0=t0: e.dma_start(out=h_d[c, :, t0:t0 + 512], in_=hb[:, i, :]), reads=[('hb', i)], dma='hb%d' % i)
                if hN is not None:
                    S.op('act', lambda e, tt=tt: e.activation(out=rs2p[:, tt, :], in_=ps[:, sums_banks[tt], :], func=AF.Sqrt, scale=1.0 / D, bias=epsb[:]),
                         reads=[('ps', sums_banks[tt]), 'epsb'], writes=[('rs2', tt)])
                    S.op('dve', lambda e, tt=tt: e.reciprocal(out=rs2p[:, tt, :], in_=rs2p[:, tt, :]), reads=[('rs2', tt)], writes=[('rs2', tt)])

        def load_h(hT_):
            for q in range(4):
                S.op('sp', lambda e, q=q: e.dma_start(out=hT_[:, 8 * q:8 * q + 8, :], in_=h_d[8 * q:8 * q + 8].rearrange("c p t -> p c t")), writes=[('hld', q)], dma='hld')
            for c in range(KC):
                for tt in range(2):
                    S.op('dve' if (c + tt) % 2 else 'pool', lambda e, c=c, tt=tt: e.tensor_tensor(out=hT_[:, c, tt * 512:(tt + 1) * 512], in0=hT_[:, c, tt * 512:(tt + 1) * 512], in1=rs2p[:, tt, :], op=ALU.mult),
                         reads=[('hld', c // 8)], writes=[('hN', c, tt)])

        def pre_store(st_bufs, b, ct, tt, okey):
            po, sqp = st_bufs
            o = okey[0] % 2
            okey[0] += 1
            S.op('dve', lambda e: e.tensor_copy(out=po[:, o, :], in_=ps[:, b, :]), reads=[('ps', b)], writes=[('po', o)])
            S.op('act', lambda e: e.activation(out=sqp[:, o, :], in_=po[:, o, :], func=AF.Square), reads=[('po', o)], writes=[('sqp', o)])
            S.op('pe', lambda e: e.matmul(ps[:, 6 + tt, :], lhsT=ones, rhs=sqp[:, o, :], start=(ct == 0), stop=(ct == KC - 1)),
                 reads=[('sqp', o), 'cst'], writes=[('ps', 6 + tt)])
            S.op('sp', lambda e: e.dma_start(out=pre_d[ct, :, tt * 512:(tt + 1) * 512], in_=po[:, o, :]), reads=[('po', o)], dma='po%d' % o)

        if True:
            with ExitStack() as st:
                yaT = sbt(st, "yaT", [128, 4, T], BF16)
                ybT = sbt(st, "ybT", [128, 16, T], BF16)
                S.op('sp', lambda e: e.dma_start(out=yaT[:], in_=ya_d.rearrange("h p t -> p h t")), writes=['yaT'], dma='c')
                S.op('sp', lambda e: e.dma_start(out=ybT[:], in_=yb_d.rearrange("h p t -> p h t")), writes=['ybT'], dma='c')
                mT = sbt(st, "mT", [128, KC, T], BF16)
                wb = sbt(st, "wb", [128, 2, KC, 256], BF16)
                gm = sbt(st, "gm", [128, 2, 2, T], BF16)
                t1 = sbt(st, "t1", [128, 2, 512], F32)
                t2 = sbt(st, "t2", [128, 2, 512], F32)
                po = sbt(st, "po", [128, 2, 512], F32)
                sqp = sbt(st, "sqp", [128, 2, 512], BF16)
                k = [0]
                for w0 in range(0, D, 256):
                    sa = load_w(wb, w_a, 0, 4, w0, 256)
                    sb_ = load_w(wb, w_b, 0, 16, w0, 256)
                    for cs_ in range(2):
                        ct = w0 // 128 + cs_
                        gi = ct % 2
                        S.op('sp', lambda e, ct=ct, gi=gi: e.dma_start(out=gm[:, gi, 0, :], in_=gmix_d[ct]), writes=[('gm', gi)], dma='gm%d' % gi)
                        S.op('sp', lambda e, ct=ct, gi=gi: e.dma_start(out=gm[:, gi, 1, :], in_=gmix_d[32 + ct]), writes=[('gm', gi)], dma='gm%d' % gi)
                        for tt in range(2):
                            t0 = tt * 512
                            ba = nb(); bb = nb()
                            mm_fm(wb, sa, 4, cs_, lambda kc, t0=t0: yaT[:, kc, t0:t0 + 512], 512, ba, ['yaT'])
                            mm_fm(wb, sb_, 16, cs_, lambda kc, t0=t0: ybT[:, kc, t0:t0 + 512], 512, bb, ['ybT'])
                            o = k[0] % 2
                            k[0] += 1
                            S.op('dve', lambda e, ba=ba, o=o, gi=gi, t0=t0: e.tensor_tensor(out=t1[:, o, :], in0=ps[:, ba, :], in1=gm[:, gi, 0, t0:t0 + 512], op=ALU.mult),
                                 reads=[('ps', ba), ('gm', gi)], writes=[('t1', o)])
                            S.op('dve', lambda e, bb=bb, o=o, gi=gi, t0=t0: e.tensor_tensor(out=t2[:, o, :], in0=ps[:, bb, :], in1=gm[:, gi, 1, t0:t0 + 512], op=ALU.mult),
                                 reads=[('ps', bb), ('gm', gi)], writes=[('t2', o)])
                            S.op('dve', lambda e, o=o, ct=ct, t0=t0: e.tensor_tensor(out=mT[:, ct, t0:t0 + 512], in0=t1[:, o, :], in1=t2[:, o, :], op=ALU.add),
                                 reads=[('t1', o), ('t2', o)], writes=[('mT', ct)])
                mrd = [('mT', c) for c in range(KC)]
                ok = [0]
                for w0 in range(0, D, 256):
                    s = load_w(wb, w_out, 0, KC, w0, 256)
                    for cs_ in range(2):
                        ct = w0 // 128 + cs_
                        for tt in range(2):
                            t0 = tt * 512
                            b = nb()
                            mm_fm(wb, s, KC, cs_, lambda kc, t0=t0: mT[:, kc, t0:t0 + 512], 512, b, mrd)
                            pre_store((po, sqp), b, ct, tt, ok)
                S.flush()
            with ExitStack() as st:
                epilogue(st, (6, 7), G_MIXPOST, lambda c, t0: xT_ext_v[:, c, HALO + t0:HALO + t0 + 512], x1_d, G_FFNPRE, True)
                S.flush()

            with ExitStack() as st:
                hT2 = sbt(st, "hT2", [128, KC, T], BF16)
                load_h(hT2)
                wb = sbt(st, "wb", [128, 4, KC, 128], BF16)
                ao = sbt(st, "ao", [128, 2, T], BF16)
                sg = sbt(st, "sg", [128, 2, 512], F32)
                h2rd = [('hN', c, tt) for c in range(KC) for tt in range(2)]
                wk_ = [0]
                for ft in range(FKC):
                    slots = []
                    for half_, c0 in enumerate((ft * 128, DFF + ft * 128)):
                        s = wk_[0] % 4
                        wk_[0] += 1
                        src = w_gu[:, c0:c0 + 128].rearrange("(k p) c -> p k c", p=128)
                        S.op('pool', lambda e, s=s, src=src: e.dma_start(out=wb[:, s, :16, :], in_=src[:, :16, :]), writes=[('wb', s)], dma='wb%d' % s)
                        S.op('pool', lambda e, s=s, src=src: e.dma_start(out=wb[:, s, 16:, :], in_=src[:, 16:, :]), writes=[('wb', s)], dma='wb%d' % s)
                        slots.append(s)
                    ai = ft % 2
                    for tt in range(2):
                        t0 = tt * 512
                        bg = nb(); bu = nb()
                        for kc in range(KC):
                            S.op('pe', lambda e, kc=kc, bg=bg, s=slots[0], t0=t0: e.matmul(ps[:, bg, :], lhsT=wb[:, s, kc, :], rhs=hT2[:, kc, t0:t0 + 512], start=(kc == 0), stop=(kc == KC - 1)),
                                 reads=[('wb', slots[0])] + (h2rd if kc == 0 else []), writes=[('ps', bg)])
                        for kc in range(KC):
                            S.op('pe', lambda e, kc=kc, bu=bu, s=slots[1], t0=t0: e.matmul(ps[:, bu, :], lhsT=wb[:, s, kc, :], rhs=hT2[:, kc, t0:t0 + 512], start=(kc == 0), stop=(kc == KC - 1)),
                                 reads=[('wb', slots[1])], writes=[('ps', bu)])
                        S.op('act', lambda e, bg=bg, tt=tt: e.activation(out=sg[:, tt, :], in_=ps[:, bg, :], func=AF.Silu), reads=[('ps', bg)], writes=[('sg', tt)])
                        S.op('dve', lambda e, bu=bu, tt=tt, ai=ai, t0=t0: e.tensor_tensor(out=ao[:, ai, t0:t0 + 512], in0=sg[:, tt, :], in1=ps[:, bu, :], op=ALU.mult),
                             reads=[('sg', tt), ('ps', bu)], writes=[('ao', ai)])
                    S.op('sp', lambda e, ft=ft, ai=ai: e.dma_start(out=act_d[ft], in_=ao[:, ai, :]), reads=[('ao', ai)], dma='ao%d' % ai)
                S.flush()
        with ExitStack() as st:
            HK = FKC // 2
            aT = sbt(st, "aT", [128, HK, T], BF16)
            wb = sbt(st, "wb", [128, 2, HK, 128], BF16)
            po = sbt(st, "po", [128, 2, 512], F32)
            pr = sbt(st, "pr", [128, 2, 512], F32)
            sqp = sbt(st, "sqp", [128, 2, 512], BF16)
            ok = [0]
            wk_ = [0]
            for hf in range(2):
                k0 = hf * HK
                for q in range(2):
                    f0, f1 = (0, 22) if q == 0 else (22, HK)
                    S.op('sp', lambda e, f0=f0, f1=f1, k0=k0: e.dma_start(out=aT[:, f0:f1, :], in_=act_d[k0 + f0:k0 + f1].rearrange("f p t -> p f t")), writes=['aT'], dma='aT')
                for ct in range(KC):
                    s_ = wk_[0] % 2
                    wk_[0] += 1
                    src = w_down[k0 * 128:(k0 + HK) * 128, ct * 128:(ct + 1) * 128].rearrange("(k p) c -> p k c", p=128)
                    S.op('pool', lambda e, s_=s_, src=src: e.dma_start(out=wb[:, s_, :22, :], in_=src[:, :22, :]), writes=[('wb', s_)], dma='wb%d' % s_)
                    S.op('pool', lambda e, s_=s_, src=src: e.dma_start(out=wb[:, s_, 22:, :], in_=src[:, 22:, :]), writes=[('wb', s_)], dma='wb%d' % s_)
                    for tt in range(2):
                        t0 = tt * 512
                        bk = nb()
                        for kc in range(HK):
                            S.op('pe', lambda e, kc=kc, s_=s_, bk=bk, t0=t0: e.matmul(ps[:, bk, :], lhsT=wb[:, s_, kc, :], rhs=aT[:, kc, t0:t0 + 512], start=(kc == 0), stop=(kc == HK - 1)),
                                 reads=[('wb', s_), 'aT'], writes=[('ps', bk)])
                        o = ok[0] % 2
                        ok[0] += 1
                        if hf == 0:
                            S.op('dve', lambda e, bk=bk, o=o: e.tensor_copy(out=po[:, o, :], in_=ps[:, bk, :]), reads=[('ps', bk)], writes=[('po', o)])
                            S.op('sp', lambda e, o=o, ct=ct, t0=t0: e.dma_start(out=pre_d[ct, :, t0:t0 + 512], in_=po[:, o, :]), reads=[('po', o)], writes=[('pre', ct, tt)], dma='po%d' % o)
                        else:
                            S.op('sp', lambda e, o=o, ct=ct, t0=t0: e.dma_start(out=pr[:, o, :], in_=pre_d[ct, :, t0:t0 + 512]), reads=[('pre', ct, tt)], writes=[('pr', o)], dma='pr%d' % o)
                            S.op('dve', lambda e, bk=bk, o=o: e.tensor_tensor(out=po[:, o, :], in0=ps[:, bk, :], in1=pr[:, o, :], op=ALU.add), reads=[('ps', bk), ('pr', o)], writes=[('po', o)])
                            S.op('act', lambda e, o=o: e.activation(out=sqp[:, o, :], in_=po[:, o, :], func=AF.Square), reads=[('po', o)], writes=[('sqp', o)])
                            S.op('pe', lambda e, o=o, ct=ct, tt=tt: e.matmul(ps[:, 6 + tt, :], lhsT=ones, rhs=sqp[:, o, :], start=(ct == 0), stop=(ct == KC - 1)),
                                 reads=[('sqp', o), 'cst'], writes=[('ps', 6 + tt)])
                            S.op('sp', lambda e, o=o, ct=ct, t0=t0: e.dma_start(out=pre_d[ct, :, t0:t0 + 512], in_=po[:, o, :]), reads=[('po', o)], writes=[('pre', ct, tt)], dma='po%d' % o)
            S.flush()
        if True:
            with ExitStack() as st:
                epilogue(st, (6, 7), G_FFNPOST, lambda c, t0: x1_d[c, :, t0:t0 + 512], x2_d, G_PLEPRE, True)
                S.flush()
            with ExitStack() as st:
                hT3 = sbt(st, "hT3", [128, KC, T], BF16)
                load_h(hT3)
                wb = sbt(st, "wb", [128, 2, KC, 256], BF16)
                wp = sbt(st, "wp", [128, 2, 2, 256], BF16)
                pb = sbt(st, "pb", [128, 2, T], BF16)
                sg = sbt(st, "sg", [128, 2, 512], F32)
                pl = sbt(st, "pl", [128, 2, 512], F32)
                po = sbt(st, "po", [128, 2, 512], F32)
                sqp = sbt(st, "sqp", [128, 2, 512], BF16)
                S.op('pool', lambda e: e.dma_start(out=pb[:], in_=pT.rearrange("(k p) t -> p k t", p=128)), writes=['pb'], dma='c')
                h3rd = [('hN', c, tt) for c in range(KC) for tt in range(2)]
                ok = [0]
                k = [0]
                for w0 in range(0, D, 256):
                    s = load_w(wb, w_pg, 0, KC, w0, 256)
                    S.op('pool', lambda e, s=s, w0=w0: e.dma_start(out=wp[:, s, :, :], in_=w_ple[:, w0:w0 + 256].rearrange("(k p) c -> p k c", p=128)),
                         writes=[('wp', s)], dma='wp%d' % s)
                    for cs_ in range(2):
                        ct = w0 // 128 + cs_
                        for tt in range(2):
                            t0 = tt * 512
                            bg = nb(); bp = nb()
                            mm_fm(wb, s, KC, cs_, lambda kc, t0=t0: hT3[:, kc, t0:t0 + 512], 512, bg, h3rd)
                            for kc in range(2):
                                S.op('pe', lambda e, kc=kc, s=s, cs_=cs_, bp=bp, t0=t0: e.matmul(ps[:, bp, :], lhsT=wp[:, s, kc, cs_ * 128:(cs_ + 1) * 128], rhs=pb[:, kc, t0:t0 + 512],
                                                                                         start=(kc == 0), stop=(kc == 1)),
                                     reads=[('wp', s), 'pb'], writes=[('ps', bp)])
                            o = k[0] % 2
                            k[0] += 1
                            S.op('act', lambda e, bg=bg, o=o: e.activation(out=sg[:, o, :], in_=ps[:, bg, :], func=AF.Sigmoid), reads=[('ps', bg)], writes=[('sg', o)])
                            S.op('dve', lambda e, bp=bp, o=o: e.tensor_tensor(out=pl[:, o, :], in0=sg[:, o, :], in1=ps[:, bp, :], op=ALU.mult),
                                 reads=[('sg', o), ('ps', bp)], writes=[('pl', o)])
                            o2 = ok[0] % 2
                            ok[0] += 1
                            S.op('act', lambda e, o=o, o2=o2: e.activation(out=sqp[:, o2, :], in_=pl[:, o, :], func=AF.Square), reads=[('pl', o)], writes=[('sqp', o2)])
                            S.op('pe', lambda e, o2=o2, ct=ct, tt=tt: e.matmul(ps[:, 6 + tt, :], lhsT=ones, rhs=sqp[:, o2, :], start=(ct == 0), stop=(ct == KC - 1)),
                                 reads=[('sqp', o2), 'cst'], writes=[('ps', 6 + tt)])
                            S.op('sp', lambda e, o=o, ct=ct, tt=tt: e.dma_start(out=pre_d[ct, :, tt * 512:(tt + 1) * 512], in_=pl[:, o, :]), reads=[('pl', o)], dma='po%d' % o)
                S.flush()
        with ExitStack() as st:
            epilogue(st, (6, 7), G_PLEPOST, lambda c, t0: x2_d[c, :, t0:t0 + 512], None, None, None, final_out=outT)
            S.flush()
    return nc


def _bias(valid, reps=4):
    b = np.where(valid, 0.0, NEG).astype(np.float32)
    return np.tile(b, (1, reps)).astype(ml_dtypes.bfloat16)


def _host_consts(core):
    bf = ml_dtypes.bfloat16
    start = core * T
    c = {}
    half = 64
    inv = (10000.0 ** (-np.arange(half, dtype=np.float32) / half)).astype(np.float32)

    def cs(pos):
        ang = pos.astype(np.float32)[None, :] * np.concatenate([inv, inv])[:, None]
        co = np.cos(ang).astype(np.float32)
        si = np.sin(ang).astype(np.float32)
        si[:half] *= -1.0
        return np.stack([co, si], axis=1).astype(np.float32)
    c["cs_full"] = cs(np.arange(S_ALL))
    c["cs_ext"] = cs(np.arange(start - HALO, start + T))
    ident = np.eye(128, dtype=np.float32)
    swp = np.zeros((128, 128), np.float32)
    for m in range(128):
        swp[(m + 64) % 128, m] = 1.0
    c["consts"] = np.stack([ident, swp, np.ones((128, 128), np.float32)], axis=1).astype(bf)
    keys = np.arange(S_ALL)
    c["xsel"] = (keys[None, :] // 64 == np.arange(128)[:, None]).astype(np.float32).astype(bf)
    n = np.arange(512)
    m = np.arange(128)
    ov = np.clip(np.minimum(n[:, None] * 16 + 32, m[None, :] * 64 + 64) - np.maximum(n[:, None] * 16, m[None, :] * 64), 0, None) / 32.0
    ov[511] = 0.0
    c["ovm"] = ov.reshape(4, 128, 128).transpose(1, 0, 2).astype(np.float32).astype(bf)
    gs = np.zeros((48, 48, 128), np.float32)
    for k in range(48):
        gs[k, k, :] = 1.0
    c["gsel"] = gs.astype(bf)
    bc = np.zeros((8, 128, 4, 512), bf)
    af = np.zeros((8, 128, 128), np.float32)
    bw = np.zeros((8, 128, 5, 512), bf)
    for qb in range(8):
        pos = start + qb * 128 + np.arange(128)
        for j in range(4):
            nn = j * 128 + np.arange(128)
            valid = (nn[:, None] * 16 + 31 <= pos[None, :]) & (nn[:, None] < 511)
            bc[qb, :, j, :] = _bias(valid)
        cur = pos // 64
        sblk = np.arange(128)
        forced = (sblk[None] == 0) | (sblk[None] == cur[:, None]) | (sblk[None] == cur[:, None] - 1)
        sval = sblk[None] * 64 <= pos[:, None]
        af[qb] = np.where(sval, 1000.0 * forced, -1e30).astype(np.float32)
        for i in range(5):
            kp = start + (qb - 4 + i) * 128 + np.arange(128)
            valid = (kp[:, None] <= pos[None, :]) & (pos[None, :] - kp[:, None] < 512) & (kp[:, None] >= 0)
            bw[qb, :, i, :] = _bias(valid)
    c["biasC"] = bc
    c["addF"] = af
    c["biasW"] = bw
    dg = np.zeros((128, 8, 512), bf)
    kk = np.arange(128)
    dg[:, core, :] = _bias(kk[:, None] <= kk[None, :])
    c["diagb"] = dg
    bd = np.zeros((128, 22, 512), bf)

    def dil(r, nq, a0, ks, nk):
        qpos = start + r * (a0 + np.arange(nq))
        kpos = start + r * (ks + np.arange(nk))
        valid = (kpos[:, None] >= 0) & (kpos[:, None] <= qpos[None, :]) & (qpos[None, :] - kpos[:, None] <= 128 * r)
        t = np.full((128, 4 * nq), NEG, np.float32)
        t[:nk] = np.tile(np.where(valid, 0.0, NEG), (1, 4))
        out = np.zeros((128, 512), np.float32)
        out[:, :4 * nq] = t
        return out.astype(bf)
    for blk in range(8):
        bd[:, blk * 2] = dil(1, 128, blk * 128, blk * 128 - 128, 128)
        bd[:, blk * 2 + 1] = dil(1, 128, blk * 128, blk * 128, 128)
    for blk in range(2):
        bd[:, 16 + blk * 2] = dil(4, 128, blk * 128, blk * 128 - 128, 128)
        bd[:, 16 + blk * 2 + 1] = dil(4, 128, blk * 128, blk * 128, 128)
    bd[:, 20] = dil(16, 64, 0, -128, 128)
    bd[:, 21] = dil(16, 64, 0, 0, 64)
    c["biasD"] = bd
    return c


_NC_CACHE = {}


def kernel(**inputs):
    f = lambda k: np.asarray(inputs[k], dtype=np.float32)
    x = f("x")[0]
    xT = np.ascontiguousarray(x.T)
    p = f("p")[0, 0]
    gl = lambda v: np.ascontiguousarray(v.reshape(32, 128).T)
    gains = np.concatenate([gl(f(k)[0]) for k in ("g_mix_pre", "g_mix_post", "g_ffn_pre", "g_ffn_post", "g_ple_pre", "g_ple_post")]
                           + [np.zeros((128, 64), np.float32)], axis=1)
    shared = {
        "xT_full": xT, "gains": np.ascontiguousarray(gains), "w_in": f("w_in")[0],
        "peT_k": np.ascontiguousarray(f("pe_ck")[0].T), "peT_v": np.ascontiguousarray(f("pe_cv")[0].T),
        "w_ck1": f("w_ck1")[0], "w_ck2": f("w_ck2")[0], "w_cv1": f("w_cv1")[0], "w_cv2": f("w_cv2")[0],
        "w_a": f("w_a")[0], "w_b": f("w_b")[0], "w_out": f("w_out")[0], "w_gu": f("w_gu")[0],
        "w_down": f("w_down")[0], "w_pg": f("w_ple_gate")[0], "w_ple": f("w_ple")[0],
    }
    in_maps = []
    for c in range(NCORES):
        start = c * T
        ext = np.zeros((D, EXT), np.float32)
        lo = max(0, start - HALO)
        ext[:, EXT - (start + T - lo):] = xT[:, lo:start + T]
        m = dict(shared)
        m["xT_ext"] = ext
        m["pT"] = np.ascontiguousarray(p[start:start + T].T)
        m.update(_host_consts(c))
        in_maps.append(m)
    if "nc" not in _NC_CACHE:
        _NC_CACHE["nc"] = build()
    _ncr = int(os.environ.get("MK_NCORES", NCORES))
    _c0 = int(os.environ.get("MK_CORE0", 0))
    if _ncr != NCORES:
        res = run_bass_kernel_spmd(_NC_CACHE["nc"], in_maps[_c0:_c0 + _ncr], core_ids=list(range(_ncr)))
        _NC_CACHE["res"] = res
        return None
    res = run_bass_kernel_spmd(_NC_CACHE["nc"], in_maps, core_ids=list(range(NCORES)))
    if _DBG:
        _NC_CACHE["res"] = res
    out = np.concatenate([np.asarray(r["outT"]).T for r in res.results], axis=0)
    return out.reshape(1, S_ALL, D).astype(np.float32)
```
